# Optimizing a Trainium2 kernel written in Bass

```python
import math
import jax
import jax.numpy as jnp
from jax import lax
import numpy as np

D_MODEL = 2048
BATCH = 2
SEQ = 8192
DEPTH = 1

N_META = 16
POOL_WIDTH = D_MODEL // 2
POOL_WINDOWS = (2, 4, 8, 16)
POOL_GROUPS = len(POOL_WINDOWS)
POOL_GROUP_WIDTH = POOL_WIDTH // POOL_GROUPS
SSM_WIDTH = D_MODEL // 2
SSM_GROUP_CH = 16
SSM_GROUPS = SSM_WIDTH // SSM_GROUP_CH
SSM_STATE = 64
DT_MIN = 1e-3
DT_MAX = 1e-1
GATE_OFFSET = POOL_WIDTH + SSM_WIDTH
IN_WIDTH = GATE_OFFSET + 2 * D_MODEL
MOE_GROUPS = 8
EXPERTS_PER_GROUP = 8
N_EXPERTS = MOE_GROUPS * EXPERTS_PER_GROUP
TOP_K_WITHIN = 2
EXPERT_FF = D_MODEL // 4
MOE_BLOCK = 128
RMS_EPS = 1e-6

kernel_name = 'hybrid_pool_s5_hmoe_block'


def rms_norm(x, g):
    x32 = x.astype(jnp.float32)
    y = x32 * lax.rsqrt(jnp.mean(x32 * x32, axis=-1, keepdims=True) + RMS_EPS)
    return (y * g.astype(jnp.float32)).astype(x.dtype)


def pool_mixer(u, pool_w, pool_scale):
    bn, L, C = u.shape
    u32 = u.astype(jnp.float32)
    cs = jnp.cumsum(u32, axis=1)
    pos = jnp.arange(L)
    parts = []
    for g, w in enumerate(POOL_WINDOWS):
        sl = slice(g * POOL_GROUP_WIDTH, (g + 1) * POOL_GROUP_WIDTH)
        csg = cs[..., sl]
        shifted = jnp.pad(csg, ((0, 0), (w, 0), (0, 0)))[:, :L]
        cnt = jnp.minimum(pos + 1, w).astype(jnp.float32)[None, :, None]
        parts.append((csg - shifted) / cnt - u32[..., sl])
    d = jnp.stack(parts, axis=2).astype(u.dtype)
    y = jnp.einsum('blgc,gcd->blgd', d, pool_w).reshape(bn, L, C)
    return y * pool_scale


def s5_ssm(u, a_re, a_im, log_dt, b_re, b_im, c_re, c_im, d):
    bn, L, Q = u.shape
    u32 = u.astype(jnp.float32)
    ug = u32.reshape(bn, L, SSM_GROUPS, SSM_GROUP_CH)
    ar = a_re.astype(jnp.float32)
    ai = a_im.astype(jnp.float32)
    dt = jnp.exp(log_dt.astype(jnp.float32))[:, None]
    mag = jnp.exp(ar * dt)
    lam_re = mag * jnp.cos(ai * dt)
    lam_im = mag * jnp.sin(ai * dt)
    den = ar * ar + ai * ai
    nr = lam_re - 1.0
    coef_re = (nr * ar + lam_im * ai) / den
    coef_im = (lam_im * ar - nr * ai) / den
    br = b_re.astype(jnp.float32)
    bi = b_im.astype(jnp.float32)
    bb_re = coef_re[..., None] * br - coef_im[..., None] * bi
    bb_im = coef_re[..., None] * bi + coef_im[..., None] * br
    bu_re = jnp.einsum('blgh,gnh->blgn', ug, bb_re)
    bu_im = jnp.einsum('blgh,gnh->blgn', ug, bb_im)
    la_re = jnp.broadcast_to(lam_re, bu_re.shape)
    la_im = jnp.broadcast_to(lam_im, bu_im.shape)

    def combine(e1, e2):
        a1r, a1i, b1r, b1i = e1
        a2r, a2i, b2r, b2i = e2
        return (a2r * a1r - a2i * a1i,
                a2r * a1i + a2i * a1r,
                a2r * b1r - a2i * b1i + b2r,
                a2r * b1i + a2i * b1r + b2i)

    _, _, xr, xi = lax.associative_scan(combine, (la_re, la_im, bu_re, bu_im), axis=1)
    y = (jnp.einsum('blgn,ghn->blgh', xr, c_re.astype(jnp.float32))
         - jnp.einsum('blgn,ghn->blgh', xi, c_im.astype(jnp.float32)))
    y = y.reshape(bn, L, Q) + d.astype(jnp.float32) * u32
    return y.astype(u.dtype)


def hier_moe(v, w_rg, b_rg, w_re, b_re, w_gate, w_up, w_down):
    bn, L, D = v.shape
    N = bn * L
    vf = v.reshape(N, D)
    gl = (vf @ w_rg).astype(jnp.float32) + b_rg.astype(jnp.float32)
    p_group = jax.nn.softmax(gl, axis=-1)
    _, g_star = lax.top_k(gl, 1)
    pg = jnp.take_along_axis(p_group, g_star, axis=1)
    el = ((vf @ w_re).astype(jnp.float32) + b_re.astype(jnp.float32)).reshape(N, MOE_GROUPS, EXPERTS_PER_GROUP)
    g_idx = jnp.broadcast_to(g_star[:, :, None], (N, 1, EXPERTS_PER_GROUP))
    el_sel = jnp.take_along_axis(el, g_idx, axis=1)[:, 0]
    ev, ei = lax.top_k(el_sel, TOP_K_WITHIN)
    gate = pg * jax.nn.softmax(ev, axis=-1)
    eid = g_star * EXPERTS_PER_GROUP + ei

    M = N * TOP_K_WITHIN
    e_flat = eid.reshape(M)
    w_flat = gate.reshape(M)
    tok_flat = jnp.arange(M, dtype=jnp.int32) // TOP_K_WITHIN
    order = jnp.argsort(e_flat)
    se = e_flat[order]
    stok = tok_flat[order]
    sw = w_flat[order]
    counts = jnp.zeros((N_EXPERTS,), jnp.int32).at[e_flat].add(1)
    padded = ((counts + MOE_BLOCK - 1) // MOE_BLOCK) * MOE_BLOCK
    pend = jnp.cumsum(padded)
    pstart = pend - padded
    start = jnp.cumsum(counts) - counts
    dest = pstart[se] + (jnp.arange(M, dtype=jnp.int32) - start[se])
    T = ((M + N_EXPERTS * (MOE_BLOCK - 1) + MOE_BLOCK - 1) // MOE_BLOCK) * MOE_BLOCK
    NB = T // MOE_BLOCK
    buf_tok = jnp.full((T,), N, jnp.int32).at[dest].set(stok)
    buf_w = jnp.zeros((T,), jnp.float32).at[dest].set(sw)
    block_e = jnp.clip(jnp.searchsorted(pend, jnp.arange(NB, dtype=jnp.int32) * MOE_BLOCK, side='right'),
                       0, N_EXPERTS - 1)
    vpad = jnp.concatenate([vf, jnp.zeros((1, D), vf.dtype)], axis=0)
    xb = vpad[buf_tok].reshape(NB, MOE_BLOCK, D)

    def expert_block(args):
        xblk, e = args
        hg = xblk @ w_gate[e]
        hu = xblk @ w_up[e]
        return (jax.nn.silu(hg) * hu) @ w_down[e]

    yb = lax.map(expert_block, (xb, block_e)).reshape(T, D)
    yb = yb * buf_w[:, None].astype(yb.dtype)
    out = jnp.zeros((N + 1, D), yb.dtype).at[buf_tok].add(yb)[:N]
    return out.reshape(bn, L, D)


def setup_inputs(seed: int = 0) -> dict:
    key = jax.random.key(seed)
    ks = jax.random.split(key, 32)
    f32 = jnp.float32

    def nrm(k, shape, scale):
        return jax.random.normal(k, shape, f32) * scale

    D = D_MODEL
    G, N, H = SSM_GROUPS, SSM_STATE, SSM_GROUP_CH
    n_idx = jnp.arange(SSM_STATE, dtype=f32)
    return {
        'x': nrm(ks[0], (BATCH, SEQ, D), 1.0),
        'meta': nrm(ks[1], (N_META, D), 1.0),
        'norm_mix': 1.0 + nrm(ks[2], (DEPTH, D), 0.02),
        'w_in': nrm(ks[3], (DEPTH, D, IN_WIDTH), D ** -0.5),
        'pool_w': nrm(ks[4], (DEPTH, POOL_GROUPS, POOL_GROUP_WIDTH, POOL_GROUP_WIDTH), POOL_GROUP_WIDTH ** -0.5),
        'pool_scale': 1.0 + nrm(ks[5], (DEPTH, POOL_WIDTH), 0.1),
        'ssm_a_re': -0.5 + nrm(ks[6], (DEPTH, G, N), 0.01),
        'ssm_a_im': math.pi * n_idx + nrm(ks[7], (DEPTH, G, N), 0.01),
        'ssm_log_dt': jax.random.uniform(ks[8], (DEPTH, G), f32, math.log(DT_MIN), math.log(DT_MAX)),
        'ssm_b_re': nrm(ks[9], (DEPTH, G, N, H), (2 * H) ** -0.5),
        'ssm_b_im': nrm(ks[10], (DEPTH, G, N, H), (2 * H) ** -0.5),
        'ssm_c_re': nrm(ks[11], (DEPTH, G, H, N), N ** -0.5),
        'ssm_c_im': nrm(ks[12], (DEPTH, G, H, N), N ** -0.5),
        'ssm_d': nrm(ks[13], (DEPTH, SSM_WIDTH), 1.0),
        'w_glu': nrm(ks[14], (DEPTH, SSM_WIDTH, SSM_WIDTH), SSM_WIDTH ** -0.5),
        'b_glu': nrm(ks[15], (DEPTH, SSM_WIDTH), 0.01),
        'w_branch_pool': nrm(ks[16], (DEPTH, POOL_WIDTH, D), POOL_WIDTH ** -0.5),
        'w_branch_ssm': nrm(ks[17], (DEPTH, SSM_WIDTH, D), SSM_WIDTH ** -0.5),
        'w_out': nrm(ks[18], (DEPTH, D, D), D ** -0.5),
        'norm_ffn': 1.0 + nrm(ks[19], (DEPTH, D), 0.02),
        'w_router_group': nrm(ks[20], (DEPTH, D, MOE_GROUPS), D ** -0.5),
        'b_router_group': nrm(ks[21], (DEPTH, MOE_GROUPS), 0.01),
        'w_router_expert': nrm(ks[22], (DEPTH, D, N_EXPERTS), D ** -0.5),
        'b_router_expert': nrm(ks[23], (DEPTH, N_EXPERTS), 0.01),
        'w_gate': nrm(ks[24], (DEPTH, N_EXPERTS, D, EXPERT_FF), D ** -0.5),
        'w_up': nrm(ks[25], (DEPTH, N_EXPERTS, D, EXPERT_FF), D ** -0.5),
        'w_down': nrm(ks[26], (DEPTH, N_EXPERTS, EXPERT_FF, D), EXPERT_FF ** -0.5),
        'norm_final': 1.0 + nrm(ks[27], (D,), 0.02),
    }


def reference(x, meta, norm_mix, w_in, pool_w, pool_scale, ssm_a_re, ssm_a_im, ssm_log_dt,
              ssm_b_re, ssm_b_im, ssm_c_re, ssm_c_im, ssm_d, w_glu, b_glu, w_branch_pool,
              w_branch_ssm, w_out, norm_ffn, w_router_group, b_router_group, w_router_expert,
              b_router_expert, w_gate, w_up, w_down, norm_final):
    bn = x.shape[0]
    meta_b = jnp.broadcast_to(meta[None].astype(x.dtype), (bn, N_META, D_MODEL))
    h = jnp.concatenate([meta_b, x], axis=1)
    for l in range(DEPTH):
        u = rms_norm(h, norm_mix[l])
        z = u @ w_in[l]
        z_pool = z[..., :POOL_WIDTH]
        z_ssm = z[..., POOL_WIDTH:GATE_OFFSET]
        gate_pool = jax.nn.sigmoid(z[..., GATE_OFFSET:GATE_OFFSET + D_MODEL])
        gate_ssm = jax.nn.sigmoid(z[..., GATE_OFFSET + D_MODEL:])
        y_pool = pool_mixer(z_pool, pool_w[l], pool_scale[l]) @ w_branch_pool[l]
        s = s5_ssm(z_ssm, ssm_a_re[l], ssm_a_im[l], ssm_log_dt[l], ssm_b_re[l], ssm_b_im[l],
                   ssm_c_re[l], ssm_c_im[l], ssm_d[l])
        s = jax.nn.gelu(s)
        s = s * jax.nn.sigmoid(s @ w_glu[l] + b_glu[l])
        y_ssm = s @ w_branch_ssm[l]
        merged = gate_pool * y_pool + gate_ssm * y_ssm
        h = h + merged @ w_out[l]
        v = rms_norm(h, norm_ffn[l])
        h = h + hier_moe(v, w_router_group[l], b_router_group[l], w_router_expert[l],
                         b_router_expert[l], w_gate[l], w_up[l], w_down[l])
    return rms_norm(h, norm_final)[:, N_META:]
```

```python
import math
from contextlib import ExitStack

import numpy as np
import concourse.bass as bass
import concourse.mybir as mybir
from concourse.bass_utils import run_bass_kernel_spmd

F32 = mybir.dt.float32
BF16 = mybir.dt.bfloat16
I32 = mybir.dt.int32
ALU = mybir.AluOpType
AF = mybir.ActivationFunctionType
AX = mybir.AxisListType

D = 2048
KC = 16
NT = 2048
NH = 16
NTOK = NT + NH
TCH = 8
NCH = NTOK // TCH
NE = 64
CAP = 128
EPS = 1e-6
MAGIC = 12582912.0
TWO_PI = 2.0 * math.pi
DEBUG = {}


class Sched:
    ENGS = ("pe", "act", "dve", "pool", "sp")
    NDMA = 14

    def __init__(self, nc, stack):
        self.nc = nc
        self.ops = {e: [] for e in self.ENGS}
        self.cnt = {e: 0 for e in self.ENGS}
        self.sem = {e: stack.enter_context(nc.semaphore("s_" + e)) for e in self.ENGS}
        self.dsem = {
            q: [stack.enter_context(nc.semaphore(f"d_{q}{i}")) for i in range(self.NDMA)]
            for q in ("sp", "pool")
        }
        self.ccsem = stack.enter_context(nc.semaphore("cc"))
        self.dcnt = {"sp": 0, "pool": 0}
        self.known = {e: {} for e in self.ENGS}
        self.last_w = {}
        self.readers = {}
        self.out_tokens = []
        self.all_tokens = {}
        self.block = stack.enter_context(nc.Block())
        self.eobj = {"pe": nc.tensor, "act": nc.scalar, "dve": nc.vector, "pool": nc.gpsimd, "sp": nc.sync}

    def _emit(self, eng, waits, fn, sem, inc):
        e = self.eobj[eng]
        for (s_, v) in waits:
            e.wait_ge(s_, v)
        if fn is not None:
            fn(e).then_inc(sem, inc)

    def _deps(self, eng, reads, writes):
        toks = []
        for k in reads:
            w = self.last_w.get(k)
            if w is not None:
                toks.append(w)
        for k in writes:
            w = self.last_w.get(k)
            if w is not None:
                toks.append(w)
            toks.extend(self.readers.get(k, ()))
        waits = {}
        for (sem, val, src) in toks:
            if src == "pe" and eng == "pe":
                continue
            key = id(sem)
            if self.known[eng].get(key, 0) >= val:
                continue
            if key not in waits or waits[key][1] < val:
                waits[key] = (sem, val)
        for key, (sem, val) in waits.items():
            self.known[eng][key] = val
        return list(waits.values())

    def _record(self, tok, reads, writes):
        self.all_tokens[id(tok[0])] = (tok[0], max(tok[1], self.all_tokens.get(id(tok[0]), (None, 0))[1]))
        for k in writes:
            self.last_w[k] = tok
            self.readers[k] = []
        for k in reads:
            self.readers.setdefault(k, []).append(tok)

    def op(self, eng, fn, reads=(), writes=()):
        waits = self._deps(eng, reads, writes)
        self.cnt[eng] += 1
        tok = (self.sem[eng], self.cnt[eng], eng)
        self._emit(eng, waits, fn, self.sem[eng], 1)
        self._record(tok, reads, writes)
        return tok

    def dma(self, fn, reads=(), writes=(), q="sp", is_out=False):
        waits = self._deps(q, reads, writes)
        i = self.dcnt[q]
        self.dcnt[q] += 1
        slot = i % self.NDMA
        rnd = i // self.NDMA
        sem = self.dsem[q][slot]
        if rnd > 0:
            key = id(sem)
            if self.known[q].get(key, 0) < 16 * rnd:
                waits.append((sem, 16 * rnd))
                self.known[q][key] = 16 * rnd
        tok = (sem, 16 * (rnd + 1), "dma")
        self._emit(q, waits, fn, sem, 16)
        self._record(tok, reads, writes)
        if is_out:
            self.out_tokens.append(tok)
        return tok

    def collective(self, fn, reads=(), writes=()):
        waits = self._deps("pool", reads, writes)
        tok = (self.ccsem, 1, "dma")
        self._emit("pool", waits, fn, self.ccsem, 1)
        self._record(tok, reads, writes)
        return tok

    def barrier(self):
        for e in self.ENGS:
            waits = []
            for key, (sem, val) in self.all_tokens.items():
                if self.known[e].get(key, 0) < val:
                    if e == "pe" and sem is self.sem["pe"]:
                        continue
                    waits.append((sem, val))
                    self.known[e][key] = val
            if waits:
                self._emit(e, waits, None, None, 0)
        self.last_w = {}
        self.readers = {}

    def emit(self):
        final = {}
        for (sem, val, _) in self.out_tokens:
            if id(sem) not in final or final[id(sem)][1] < val:
                final[id(sem)] = (sem, val)
        for (s_, v) in final.values():
            self.eobj["sp"].wait_ge(s_, v)


def hi(ap):
    return ap.bitcast(BF16)[:, 1::2]


def build(debug=False, stage=3, dev=None):
    dev = dev or {}
    NBLK = dev.get("nblk", 4)
    nc = bass.Bass("TRN2", target_bir_lowering=False)

    def din(name, shape, dt=F32):
        return nc.dram_tensor(name, list(shape), dt, kind="ExternalInput").ap()

    def dscr(name, shape, dt=F32):
        return nc.dram_tensor(name, list(shape), dt, kind="Internal").ap()

    xs = din("xs", [NTOK, D])
    hm_d = din("hm", [128, 1])
    cm_d = din("cm", [128, 12])
    w_in = din("w_in", [D, 6144])
    pool_w = din("pool_w", [4, 256, 256])
    w_glu = din("w_glu", [1024, 1024])
    w_bp = din("w_bp", [1024, D])
    w_bs = din("w_bs", [1024, D])
    w_out = din("w_out", [D, D])
    if stage >= 3:
        w_gate = din("w_gate", [NE, D, 512])
        w_up = din("w_up", [NE, D, 512])
        w_down = din("w_down", [NE, 512, D])
    gains_d = din("gains", [128, 3, D])
    cols_d = din("cols", [128, 3, 8])
    wr_d = din("wr", [128, KC, 72])
    br_d = din("br", [128, 72])
    aL1_d = din("aL1", [128, 3, 8, 64])
    bL1_d = din("bL1", [128, 2, 8, 64])
    aL2_d = din("aL2", [128, 3, 32])
    bL2_d = din("bL2", [128, 2, 32, 16])
    cL2_d = din("cL2", [128, 2, 32, 16])
    cst_d = din("cst", [128, 1024])
    out_d = nc.dram_tensor("out", [NT, D], F32, kind="ExternalOutput").ap()

    cc_in = dscr("cc_in", [128, 64])
    cc_out = dscr("cc_out", [4 * 128, 64])
    hs = dscr("hs", [NT, D])
    xg = dscr("xg", [NE * CAP + 128, D], BF16)
    ysc = dscr("ysc", [NE * CAP + 128, D])
    with ExitStack() as st:
        S = Sched(nc, st)

        def dump(name, tile_, shape, dt, key):
            if not debug:
                return
            d_ap = nc.dram_tensor("dbg_" + name, list(shape), dt, kind="ExternalOutput").ap()
            S.dma(lambda e: e.dma_start(out=d_ap, in_=tile_[:]), [key], [], is_out=True)

        def V(fn, r=(), w=()): return S.op("dve", fn, r, w)
        def A(fn, r=(), w=()): return S.op("act", fn, r, w)
        def G(fn, r=(), w=()): return S.op("pool", fn, r, w)
        def PE(fn, r=(), w=()): return S.op("pe", fn, r, w)
        def DM(fn, r=(), w=(), **kw): return S.dma(fn, r, w, **kw)

        def T(stk, name, shape, dt=F32):
            return stk.enter_context(nc.sbuf_tensor("sb_" + name, list(shape), dt))

        ps = [st.enter_context(nc.psum_tensor(f"ps{i}", [128, 512], F32)) for i in range(8)]
        ps_rr = [0]

        def nbank():
            i = ps_rr[0]
            ps_rr[0] = (i + 1) % 8
            return i

        cst = T(st, "cst", [128, 1024])
        cstb = T(st, "cstb", [128, 512], BF16)
        cols = T(st, "cols", [128, 3, 8])
        hm = T(st, "hm", [128, 1])
        cm = T(st, "cm", [128, 12])
        DM(lambda e: e.dma_start(out=cst[:], in_=cst_d), w=["cst"])
        DM(lambda e: e.dma_start(out=cols[:], in_=cols_d), w=["cols"])
        DM(lambda e: e.dma_start(out=hm[:], in_=hm_d), w=["hm"])
        DM(lambda e: e.dma_start(out=cm[:], in_=cm_d), w=["cm"])
        V(lambda e: e.tensor_copy(out=cstb[:, 0:384], in_=cst[:, 0:384]), ["cst"], ["cstb"])
        ident32 = cst[:, 0:128]
        identb = cstb[:, 0:128]
        trib = cstb[:, 128:256]
        onesb = cstb[:, 256:384]
        iota_c = cst[:, 384:384 + 259]
        iota_e = cst[:, 648:712]
        mg2 = [cst[:, 712:713], cst[:, 713:714]]
        mqd = [cst[:, 714 + j:715 + j] for j in range(4)]
        mh2 = [cst[:, 718:719], cst[:, 719:720]]
        iota_p = cst[:, 720:721]

        sT = T(st, "sT", [128, 8, NT], BF16)
        gw = T(st, "gw", [128, 16, 2])
        gidx = T(st, "gidx", [128, 16, 2], I32)
        zs = None

        def load_norm_T(stk_tiles, row0, nrows, gain, uT_ap, col0, tag):
            xt, ub, ssq, kx, ku = stk_tiles
            DM(lambda e: e.dma_start(out=xt[:nrows, :], in_=xs[row0:row0 + nrows, :]), [], [kx])
            A(lambda e: e.activation(out=ub[:nrows, :], in_=xt[:nrows, :], func=AF.Square, accum_out=ssq[:nrows, 0:1]), [kx], [ku, ku + "s"])
            V(lambda e: e.tensor_scalar(out=ssq[:nrows, 1:2], in0=ssq[:nrows, 0:1], scalar1=1.0 / D, scalar2=EPS,
                                        op0=ALU.mult, op1=ALU.add), [ku + "s"], [ku + "s"])
            A(lambda e: e.activation(out=ssq[:nrows, 1:2], in_=ssq[:nrows, 1:2], func=AF.Sqrt), [ku + "s"], [ku + "s"])
            V(lambda e: e.reciprocal(out=ssq[:nrows, 2:3], in_=ssq[:nrows, 1:2]), [ku + "s"], [ku + "s"])
            V(lambda e: e.scalar_tensor_tensor(out=ub[:nrows, :], in0=xt[:nrows, :], scalar=ssq[:nrows, 2:3],
                                               in1=gain[:nrows, :], op0=ALU.mult, op1=ALU.mult),
              [kx, ku + "s", "gains", ku], [ku])
            for half in range(2):
                b = nbank()
                pk = "ps%d" % b
                psb = ps[b][:].bitcast(BF16)

                def tr(e, half=half, psb=psb):
                    ins = None
                    for j in range(8):
                        kc = half * 8 + j
                        ins = e.transpose(out=psb[:, j * 128:j * 128 + nrows], in_=ub[:nrows, kc * 128:(kc + 1) * 128],
                                          identity=identb[:nrows, :nrows])
                    return ins
                PE(tr, [ku, "cstb"], [pk])
                eng = A if half == 0 else V
                if half == 0:
                    A(lambda e, half=half, psb=psb: e.activation(
                        out=uT_ap[:, half * 8:half * 8 + 8, col0:col0 + nrows],
                        in_=psb[:, 0:1024].rearrange("p (k t) -> p k t", t=128)[:, :, 0:nrows], func=AF.Copy), [pk], tag if isinstance(tag, list) else [tag])
                else:
                    V(lambda e, half=half, psb=psb: e.tensor_copy(
                        out=uT_ap[:, half * 8:half * 8 + 8, col0:col0 + nrows],
                        in_=psb[:, 0:1024].rearrange("p (k t) -> p k t", t=128)[:, :, 0:nrows]), [pk], tag if isinstance(tag, list) else [tag])


        if dev.get("skip_ssm"):
            sT_in = nc.dram_tensor("sT_in", [128, 8, NT], BF16, kind="ExternalInput").ap()
            DM(lambda e: e.dma_start(out=sT[:], in_=sT_in), [], ["sT"])
        else:
            ssm_stack = ExitStack()
            zs = T(ssm_stack, "zs", [128, 8, 8, NCH], BF16)

            with ExitStack() as a1:
                gmix = T(a1, "gmix", [128, D])
                DM(lambda e: e.dma_start(out=gmix[:], in_=gains_d[:, 0, :]), w=["gains"])
                wss = T(a1, "wss", [128, KC, 1024])
                w_in_v = w_in.rearrange("(k p) n -> p k n", p=128)
                for j in range(8):
                    DM(lambda e, j=j: e.dma_start(out=wss[:, 2 * j:2 * j + 2, :], in_=w_in_v[:, 2 * j:2 * j + 2, 1024:2048]), [], ["wss"])
                xts = [T(a1, "xt%d" % i, [128, D]) for i in range(2)]
                ubs = [T(a1, "ub%d" % i, [128, D], BF16) for i in range(2)]
                sqs = [T(a1, "sq%d" % i, [128, 4]) for i in range(2)]
                uTs = [T(a1, "uT%d" % i, [128, KC, 512], BF16) for i in range(2)]
                tile_i = [0]

                def norm_tiles():
                    i = tile_i[0] % 2
                    tile_i[0] += 1
                    return (xts[i], ubs[i], sqs[i], "xt%d" % i, "ub%d" % i)

                blocks = [(0, NH)] + [(NH + 512 * b, 512) for b in range(4)]
                for bi, (c0, n) in enumerate(blocks):
                    uT = uTs[bi % 2]
                    tag = "uT%d" % (bi % 2)
                    for t0 in range(0, n, 128):
                        nr = min(128, n - t0)
                        load_norm_T(norm_tiles(), c0 + t0, nr, gmix, uT, t0, tag)
                    for m in range(8):
                        b = nbank()
                        pk = "ps%d" % b

                        def zmm(e, m=m, b=b, uT=uT, n=n):
                            ins = None
                            for kc in range(KC):
                                ins = e.matmul(ps[b][:, 0:n], lhsT=hi(wss[:, kc, m * 128:(m + 1) * 128]), rhs=uT[:, kc, 0:n],
                                               start=(kc == 0), stop=(kc == KC - 1))
                            return ins
                        PE(zmm, ["wss", tag], [pk])
                        ch0 = c0 // TCH
                        nchk = n // TCH
                        src = ps[b][:, 0:n].rearrange("p (c s) -> p s c", s=TCH)
                        dst = zs[:, m, :, ch0:ch0 + nchk]
                        if bi == 0:
                            V(lambda e, src=src, dst=dst: e.tensor_scalar(out=dst, in0=src, scalar1=hm[:, 0:1], scalar2=None,
                                                                          op0=ALU.mult), [pk, "hm"], ["zs"])
                        elif m % 2 == 0:
                            A(lambda e, src=src, dst=dst: e.activation(out=dst, in_=src, func=AF.Copy), [pk], ["zs"])
                        else:
                            V(lambda e, src=src, dst=dst: e.tensor_copy(out=dst, in_=src), [pk], ["zs"])
            S.barrier()
            Ptab = T(ssm_stack, "Ptab", [128, 8, 8, 2, 128], BF16)
            Qtab = T(ssm_stack, "Qtab", [128, 32, 9, 2, 32], BF16)
            Ktab = T(ssm_stack, "Ktab", [128, 8, 8, 128], BF16)
            sml = T(ssm_stack, "sml", [128, 8, 32])
            Fst = T(ssm_stack, "Fst", [128, 32, 2])
            Xc = T(ssm_stack, "Xc", [128, 32, 2])

            t1 = ExitStack()
            if True:
                aL1 = T(t1, "aL1", [128, 3, 256])
                bL1 = T(t1, "bL1", [128, 2, 256])
                w1 = T(t1, "w1", [128, 12, 256])
                pw = T(t1, "pw", [128, 2, 2, 256])
                PP = T(t1, "PP", [128, 2, 2, 256])

                def lam_block(pref, akey, a_re, a_im, ldt, W, n, mults):
                    r = [pref + "w"]
                    k = pref + "w"
                    A(lambda e: e.activation(out=W[:, 4, :n], in_=ldt, func=AF.Exp), r + [akey], [k])
                    V(lambda e: e.tensor_tensor(out=W[:, 5, :n], in0=a_re, in1=W[:, 4, :n], op=ALU.mult), r + [akey], [k])
                    V(lambda e: e.tensor_tensor(out=W[:, 6, :n], in0=a_im, in1=W[:, 4, :n], op=ALU.mult), r + [akey], [k])

                    def expi(kk, o_mag, o_re, o_im):
                        A(lambda e: e.activation(out=W[:, 7, :n], in_=W[:, 5, :n], func=AF.Exp, scale=float(kk)), r, [k])
                        for (off, dst) in ((0.0, 8), (0.25, 9)):
                            V(lambda e, off=off, dst=dst: e.tensor_scalar(out=W[:, dst, :n], in0=W[:, 6, :n], scalar1=float(kk) / TWO_PI,
                                                                          scalar2=off, op0=ALU.mult, op1=ALU.add), r, [k])
                            V(lambda e, dst=dst: e.tensor_scalar(out=W[:, 10, :n], in0=W[:, dst, :n], scalar1=MAGIC, scalar2=None,
                                                                 op0=ALU.add), r, [k])
                            V(lambda e, dst=dst: e.tensor_scalar(out=W[:, 10, :n], in0=W[:, 10, :n], scalar1=-MAGIC, scalar2=None,
                                                                 op0=ALU.add), r, [k])
                            V(lambda e, dst=dst: e.tensor_tensor(out=W[:, dst, :n], in0=W[:, dst, :n], in1=W[:, 10, :n],
                                                                 op=ALU.subtract), r, [k])
                            A(lambda e, dst=dst: e.activation(out=W[:, dst, :n], in_=W[:, dst, :n], func=AF.Sin, scale=TWO_PI), r, [k])
                        if o_mag is not None:
                            V(lambda e: e.tensor_copy(out=o_mag, in_=W[:, 7, :n]), r, [k, pref + "o"])
                        V(lambda e: e.tensor_tensor(out=o_re, in0=W[:, 7, :n], in1=W[:, 9, :n], op=ALU.mult), r, [k, pref + "o"])
                        V(lambda e: e.tensor_tensor(out=o_im, in0=W[:, 7, :n], in1=W[:, 8, :n], op=ALU.mult), r, [k, pref + "o"])

                    expi(1, None, W[:, 0, :n], W[:, 1, :n])
                    for (kk, om, ore, oim) in mults:
                        expi(kk, om, ore, oim)
                    lam_block.expi = expi
                    V(lambda e: e.tensor_tensor(out=W[:, 7, :n], in0=a_re, in1=a_re, op=ALU.mult), r + [akey], [k])
                    V(lambda e: e.tensor_tensor(out=W[:, 8, :n], in0=a_im, in1=a_im, op=ALU.mult), r + [akey], [k])
                    V(lambda e: e.tensor_tensor(out=W[:, 7, :n], in0=W[:, 7, :n], in1=W[:, 8, :n], op=ALU.add), r, [k])
                    V(lambda e: e.reciprocal(out=W[:, 7, :n], in_=W[:, 7, :n]), r, [k])
                    V(lambda e: e.tensor_scalar(out=W[:, 8, :n], in0=W[:, 0, :n], scalar1=-1.0, scalar2=None, op0=ALU.add), r, [k])
                    V(lambda e: e.tensor_tensor(out=W[:, 9, :n], in0=W[:, 8, :n], in1=a_re, op=ALU.mult), r + [akey], [k])
                    V(lambda e: e.tensor_tensor(out=W[:, 10, :n], in0=W[:, 1, :n], in1=a_im, op=ALU.mult), r + [akey], [k])
                    V(lambda e: e.tensor_tensor(out=W[:, 9, :n], in0=W[:, 9, :n], in1=W[:, 10, :n], op=ALU.add), r, [k])
                    V(lambda e: e.tensor_tensor(out=W[:, 2, :n], in0=W[:, 9, :n], in1=W[:, 7, :n], op=ALU.mult), r, [k])
                    V(lambda e: e.tensor_tensor(out=W[:, 9, :n], in0=W[:, 1, :n], in1=a_re, op=ALU.mult), r + [akey], [k])
                    V(lambda e: e.tensor_tensor(out=W[:, 10, :n], in0=W[:, 8, :n], in1=a_im, op=ALU.mult), r + [akey], [k])
                    V(lambda e: e.tensor_tensor(out=W[:, 9, :n], in0=W[:, 9, :n], in1=W[:, 10, :n], op=ALU.subtract), r, [k])
                    V(lambda e: e.tensor_tensor(out=W[:, 3, :n], in0=W[:, 9, :n], in1=W[:, 7, :n], op=ALU.mult), r, [k])

                def cmul(o_re, o_im, a_re, a_im, b_re, b_im, t1_, t2_, rk, wk, neg_im=False):
                    V(lambda e: e.tensor_tensor(out=t1_, in0=a_re, in1=b_re, op=ALU.mult), rk, wk)
                    V(lambda e: e.tensor_tensor(out=t2_, in0=a_im, in1=b_im, op=ALU.mult), rk, wk)
                    V(lambda e: e.tensor_tensor(out=o_re, in0=t1_, in1=t2_, op=ALU.subtract), rk, wk)
                    V(lambda e: e.tensor_tensor(out=t1_, in0=a_re, in1=b_im, op=ALU.mult), rk, wk)
                    V(lambda e: e.tensor_tensor(out=t2_, in0=a_im, in1=b_re, op=ALU.mult), rk, wk)
                    if neg_im:
                        V(lambda e: e.scalar_tensor_tensor(out=o_im, in0=t1_, scalar=-1.0, in1=t2_, op0=ALU.mult, op1=ALU.subtract), rk, wk)
                    else:
                        V(lambda e: e.tensor_tensor(out=o_im, in0=t1_, in1=t2_, op=ALU.add), rk, wk)

                Bb = T(t1, "Bb", [128, 2, 256])
                tA = T(t1, "tA", [128, 2, 256])
                tB = T(t1, "tB", [128, 2, 256])
                for hb in range(2):
                    DM(lambda e, hb=hb: e.dma_start(out=aL1[:].rearrange("p a (g n) -> p a g n", g=4), in_=aL1_d[:, :, 4 * hb:4 * hb + 4, :]), [], ["aL1"])
                    DM(lambda e, hb=hb: e.dma_start(out=bL1[:].rearrange("p a (g n) -> p a g n", g=4), in_=bL1_d[:, :, 4 * hb:4 * hb + 4, :]), [], ["bL1"])
                    lam_block("L1", "aL1", aL1[:, 0, :], aL1[:, 1, :], aL1[:, 2, :], w1, 256, [])
                    expi1 = lam_block.expi
                    KL1 = ["L1w", "L1o", "bL1"]
                    cmul(Bb[:, 0, :], Bb[:, 1, :], w1[:, 2, :], w1[:, 3, :], bL1[:, 0, :], bL1[:, 1, :],
                         w1[:, 10, :], w1[:, 11, :], KL1, ["L1w", "Bb"])
                    bb_re = Bb[:, 0:1, :].to_broadcast([128, 2, 256])
                    bb_im = Bb[:, 1:2, :].to_broadcast([128, 2, 256])
                    for kq in range(4):
                        for j in range(2):
                            kk_ = 2 * kq + j
                            if kk_ == 0:
                                V(lambda e: e.memset(pw[:, 0, 0, :], 1.0), ["PP"], ["L1o"])
                                V(lambda e: e.memset(pw[:, 1, 0, :], 0.0), ["PP"], ["L1o"])
                            else:
                                expi1(kk_, None, pw[:, 0, j, :], pw[:, 1, j, :])
                        cmul(PP[:, 0], PP[:, 1], pw[:, 0], pw[:, 1], bb_re, bb_im, tA[:], tB[:], KL1 + ["tAB", "Bb"], ["PP", "tAB"])
                        for ri in range(2):
                            for g2 in range(2):
                                V(lambda e, ri=ri, g2=g2, kq=kq, hb=hb: e.tensor_scalar(
                                    out=Ptab[:, 4 * hb:4 * hb + 4, 2 * kq:2 * kq + 2, ri, g2 * 64:(g2 + 1) * 64],
                                    in0=PP[:, ri].rearrange("p k (g n) -> p g k n", g=4),
                                    scalar1=mg2[g2], scalar2=None, op0=ALU.mult), ["PP", "cst"], ["Ptab"])
            S.barrier()
            t1.close()

            with ExitStack() as t2:
                aL2 = T(t2, "aL2", [128, 3, 32])
                bL2 = T(t2, "bL2", [128, 2, 512])
                cL2 = T(t2, "cL2", [128, 2, 512])
                DM(lambda e: e.dma_start(out=aL2[:], in_=aL2_d), w=["aL2"])
                DM(lambda e: e.dma_start(out=bL2[:], in_=bL2_d.rearrange("p a q h -> p a (q h)")), w=["bL2"])
                DM(lambda e: e.dma_start(out=cL2[:], in_=cL2_d.rearrange("p a q h -> p a (q h)")), w=["cL2"])
                w2 = T(t2, "w2", [128, 12, 32])
                pw2 = T(t2, "pw2", [128, 2, 9, 32])
                V(lambda e: e.memset(pw2[:, 0, 0, :], 1.0), [], ["L2o"])
                V(lambda e: e.memset(pw2[:, 1, 0, :], 0.0), [], ["L2o"])
                mults = [(kk, None, pw2[:, 0, kk, :], pw2[:, 1, kk, :]) for kk in range(1, 8)]
                mults.append((8, sml[:, 0, :], pw2[:, 0, 8, :], pw2[:, 1, 8, :]))
                mults.append((2048, None, sml[:, 2, :], sml[:, 3, :]))
                lam_block("L2", "aL2", aL2[:, 0, :], aL2[:, 1, :], aL2[:, 2, :], w2, 32, mults)
                KL2 = ["L2w", "L2o", "bL2", "cL2"]
                V(lambda e: e.tensor_scalar(out=sml[:, 1, :], in0=w2[:, 6, :], scalar1=8.0 / TWO_PI, scalar2=None, op0=ALU.mult), KL2, ["L2o"])
                V(lambda e: e.tensor_scalar(out=w2[:, 10, :], in0=sml[:, 1, :], scalar1=MAGIC, scalar2=None, op0=ALU.add), KL2, ["L2w"])
                V(lambda e: e.tensor_scalar(out=w2[:, 10, :], in0=w2[:, 10, :], scalar1=-MAGIC, scalar2=None, op0=ALU.add), KL2, ["L2w"])
                V(lambda e: e.tensor_tensor(out=sml[:, 1, :], in0=sml[:, 1, :], in1=w2[:, 10, :], op=ALU.subtract), KL2, ["L2o"])
                BB2 = T(t2, "BB2", [128, 2, 512])
                tC = T(t2, "tC", [128, 512])
                tD = T(t2, "tD", [128, 512])
                cf_re = w2[:, 2, :].unsqueeze(2).to_broadcast([128, 32, 16])
                cf_im = w2[:, 3, :].unsqueeze(2).to_broadcast([128, 32, 16])
                v3 = lambda ap: ap.rearrange("p (q h) -> p q h", h=16)
                cmul(v3(BB2[:, 0, :]), v3(BB2[:, 1, :]), cf_re, cf_im, v3(bL2[:, 0, :]), v3(bL2[:, 1, :]),
                     v3(tC[:]), v3(tD[:]), KL2 + ["tCD"], ["BB2", "tCD"])
                BBp = T(t2, "BBp", [128, 32, 2, 32], BF16)
                for ri in range(2):
                    for g2 in range(2):
                        V(lambda e, ri=ri, g2=g2: e.tensor_scalar(
                            out=BBp[:, :, ri, g2 * 16:(g2 + 1) * 16], in0=v3(BB2[:, ri, :]),
                            scalar1=mh2[g2], scalar2=None, op0=ALU.mult), ["BB2", "cst"], ["BBp"])
                CL = T(t2, "CL", [128, 2, 3, 512])
                tE = T(t2, "tE", [128, 3, 512])
                tF = T(t2, "tF", [128, 3, 512])
                v4 = lambda ap: ap.rearrange("p k (q h) -> p k q h", h=16)
                c_re = v3(cL2[:, 0, :]).unsqueeze(1).to_broadcast([128, 3, 32, 16])
                c_im = v3(cL2[:, 1, :]).unsqueeze(1).to_broadcast([128, 3, 32, 16])
                for k3 in range(3):
                    p_re = pw2[:, 0, 3 * k3:3 * k3 + 3, :].unsqueeze(3).to_broadcast([128, 3, 32, 16])
                    p_im = pw2[:, 1, 3 * k3:3 * k3 + 3, :].unsqueeze(3).to_broadcast([128, 3, 32, 16])
                    cmul(v4(CL[:, 0]), v4(CL[:, 1]), c_re, c_im, p_re, p_im, v4(tE[:]), v4(tF[:]), KL2 + ["tEF"], ["CL", "tEF"], neg_im=True)
                    for ri in range(2):
                        for g2 in range(2):
                            V(lambda e, ri=ri, g2=g2, k3=k3: e.tensor_scalar(
                                out=Qtab[:, :, 3 * k3:3 * k3 + 3, ri, g2 * 16:(g2 + 1) * 16],
                                in0=v4(CL[:, ri]).rearrange("p k q h -> p q k h"),
                                scalar1=mh2[g2], scalar2=None, op0=ALU.mult), ["CL", "cst"], ["Qtab"])
                for gb in range(8):
                    b = nbank()
                    pk = "ps%d" % b

                    def kmm(e, gb=gb, b=b):
                        ins = None
                        for qd in range(4):
                            q = 4 * gb + qd
                            for ri in range(2):
                                ins = e.matmul(ps[b][32 * qd:32 * qd + 32, 0:256], lhsT=BBp[:, q, ri, :],
                                               rhs=Qtab[:, q, 0:8, ri, :], start=(ri == 0), stop=(ri == 1),
                                               tile_position=(0, 32 * qd))
                        return ins
                    PE(kmm, ["BBp", "Qtab"], [pk])
                    for qd in range(4):
                        V(lambda e, gb=gb, b=b, qd=qd: e.tensor_scalar(
                            out=Ktab[:, gb, :, 32 * qd:32 * qd + 32],
                            in0=ps[b][:, 0:256].rearrange("p (t c) -> p t c", c=32),
                            scalar1=mqd[qd], scalar2=None, op0=ALU.mult), [pk, "cst"], ["Ktab"])

            dump("zs", zs, [128, 8, 8, NCH], BF16, "zs")
            dump("Ptab", Ptab, [128, 8, 8, 2, 128], BF16, "Ptab")
            dump("Qtab", Qtab, [128, 32, 9, 2, 32], BF16, "Qtab")
            dump("Ktab", Ktab, [128, 8, 8, 128], BF16, "Ktab")
            dump("sml", sml, [128, 8, 32], F32, "L2o")
            S.barrier()

            with ExitStack() as a3:
                Sq = T(a3, "Sq", [128, 4, 2, NCH])
                St = T(a3, "St", [128, 4, 2, NCH])
                Xt = Sq
                Wcs = T(a3, "Wcs", [128, 2, 4, NCH + 1])
                Rt = T(a3, "Rt", [128, NCH])
                tm = T(a3, "tm", [128, 2, 4, NCH + 1])
                Xb = T(a3, "Xb", [128, 4, 2, NCH], BF16)
                yt = T(a3, "yt", [128, 1024])
                y2 = T(a3, "y2", [128, 1024])
                Fall = T(a3, "Fall", [128, 4, 64])
                Gh = T(a3, "Gh", [128, 4, 64])

                def chunk_states(blk, with_carry):
                    q0 = 4 * blk
                    for ql in range(4):
                        for ri in range(2):
                            b = nbank()
                            pk = "ps%d" % b

                            def smm(e, ql=ql, ri=ri, b=b):
                                ins = None
                                for s in range(TCH):
                                    ins = e.matmul(ps[b][:, 0:NCH], lhsT=Ptab[32 * ql:32 * ql + 32, blk, 7 - s, ri, :],
                                                   rhs=zs[32 * ql:32 * ql + 32, blk, s, :], start=(s == 0), stop=(s == TCH - 1),
                                                   tile_position=(32 * ql, 0))
                                return ins
                            PE(smm, ["Ptab", "zs"], [pk])
                            if ri == 0:
                                A(lambda e, ql=ql, ri=ri, b=b: e.activation(out=Sq[:, ql, ri, :], in_=ps[b][:, 0:NCH], func=AF.Copy), [pk], ["Sq"])
                            else:
                                V(lambda e, ql=ql, ri=ri, b=b: e.tensor_copy(out=Sq[:, ql, ri, :], in_=ps[b][:, 0:NCH]), [pk], ["Sq"])
                    if with_carry:
                        V(lambda e: e.tensor_tensor(out=Sq[:, :, :, 1], in0=Sq[:, :, :, 1], in1=Xc[:, q0:q0 + 4, :], op=ALU.add), ["Sq", "Xc"], ["Sq"])
                    fr = sml[:, 1, q0:q0 + 4].unsqueeze(2).to_broadcast([128, 4, NCH + 1])
                    io = iota_c.unsqueeze(1).to_broadcast([128, 4, NCH + 1])
                    V(lambda e: e.tensor_tensor(out=tm[:, 0], in0=fr, in1=io, op=ALU.mult), ["sml", "cst", "tm"], ["tm"])
                    for (j, off) in ((1, 0.0), (0, 0.25)):
                        V(lambda e, off=off: e.tensor_scalar(out=tm[:, 1], in0=tm[:, 0], scalar1=off, scalar2=MAGIC, op0=ALU.add, op1=ALU.add), ["tm"], ["tm"])
                        V(lambda e: e.tensor_scalar(out=tm[:, 1], in0=tm[:, 1], scalar1=-MAGIC, scalar2=None, op0=ALU.add), ["tm"], ["tm"])
                        V(lambda e, off=off: e.scalar_tensor_tensor(out=tm[:, 1], in0=tm[:, 0], scalar=off, in1=tm[:, 1], op0=ALU.add, op1=ALU.subtract), ["tm"], ["tm"])
                        A(lambda e, j=j: e.activation(out=Wcs[:, j], in_=tm[:, 1], func=AF.Sin, scale=TWO_PI), ["tm"], ["Wcs"])
                    cw = Wcs[:, 0, :, 1:NCH + 1]
                    sw = Wcs[:, 1, :, 1:NCH + 1]
                    t_a = tm[:, 0, :, 0:NCH]
                    t_b = tm[:, 1, :, 0:NCH]
                    kk = ["Sq", "Wcs", "tm"]
                    V(lambda e: e.tensor_tensor(out=t_a, in0=Sq[:, :, 0, :], in1=cw, op=ALU.mult), kk, ["tm"])
                    V(lambda e: e.tensor_tensor(out=t_b, in0=Sq[:, :, 1, :], in1=sw, op=ALU.mult), kk, ["tm"])
                    V(lambda e: e.tensor_tensor(out=St[:, :, 0, :], in0=t_a, in1=t_b, op=ALU.add), ["tm"], ["St"])
                    V(lambda e: e.tensor_tensor(out=t_a, in0=Sq[:, :, 1, :], in1=cw, op=ALU.mult), kk, ["tm"])
                    V(lambda e: e.tensor_tensor(out=t_b, in0=Sq[:, :, 0, :], in1=sw, op=ALU.mult), kk, ["tm"])
                    V(lambda e: e.tensor_tensor(out=St[:, :, 1, :], in0=t_a, in1=t_b, op=ALU.subtract), ["tm"], ["St"])
                    for ql in range(4):
                        V(lambda e, ql=ql: e.tensor_copy(out=Rt[:], in_=sml[:, 0, q0 + ql:q0 + ql + 1].to_broadcast([128, NCH])), ["sml", "Rt"], ["Rt"])
                        for ri in range(2):
                            V(lambda e, ri=ri, ql=ql: e.tensor_tensor_scan(out=Xt[:, ql, ri, :], data0=Rt[:], data1=St[:, ql, ri, :],
                                                                           initial=0.0, op0=ALU.mult, op1=ALU.add), ["Rt", "St", "Sq"], ["Sq"])

                for blk in range(8):
                    chunk_states(blk, False)
                    q0 = 4 * blk
                    c258 = Wcs[:, 0, :, NCH]
                    s258 = Wcs[:, 1, :, NCH]
                    xr = Xt[:, :, 0, NCH - 1]
                    xi = Xt[:, :, 1, NCH - 1]
                    ta = tm[:, 0, :, 0]
                    tb = tm[:, 1, :, 0]
                    kk = ["Sq", "Wcs", "tm"]
                    V(lambda e, xr=xr, c258=c258, ta=ta: e.tensor_tensor(out=ta, in0=xr, in1=c258, op=ALU.mult), kk, ["tm"])
                    V(lambda e, xi=xi, s258=s258, tb=tb: e.tensor_tensor(out=tb, in0=xi, in1=s258, op=ALU.mult), kk, ["tm"])
                    V(lambda e, q0=q0, ta=ta, tb=tb: e.tensor_tensor(out=Fst[:, q0:q0 + 4, 0], in0=ta, in1=tb, op=ALU.subtract), ["tm"], ["Fst"])
                    V(lambda e, xr=xr, s258=s258, ta=ta: e.tensor_tensor(out=ta, in0=xr, in1=s258, op=ALU.mult), kk, ["tm"])
                    V(lambda e, xi=xi, c258=c258, tb=tb: e.tensor_tensor(out=tb, in0=xi, in1=c258, op=ALU.mult), kk, ["tm"])
                    V(lambda e, q0=q0, ta=ta, tb=tb: e.tensor_tensor(out=Fst[:, q0:q0 + 4, 1], in0=ta, in1=tb, op=ALU.add), ["tm"], ["Fst"])
                DM(lambda e: e.dma_start(out=cc_in, in_=Fst[:].rearrange("p q r -> p (q r)")), ["Fst"], ["cc_in"])
                S.collective(lambda e: e.collective_compute("AllGather", ALU.bypass, replica_groups=[[0, 1, 2, 3], [4, 5, 6, 7]],
                                                            ins=[cc_in], outs=[cc_out]), ["cc_in"], ["cc_out"])
                DM(lambda e: e.dma_start(out=Fall[:], in_=cc_out.rearrange("(j p) f -> p j f", p=128)), ["cc_out"], ["Fall"])
                for p_ in range(3):
                    V(lambda e, p_=p_: e.tensor_scalar(out=Gh[:, p_, :], in0=Fall[:, 0, :], scalar1=cm[:, p_:p_ + 1], scalar2=None, op0=ALU.mult), ["Fall", "cm"], ["Gh"])
                    for j in range(1, 4):
                        V(lambda e, p_=p_, j=j: e.scalar_tensor_tensor(out=Gh[:, p_, :], in0=Fall[:, j, :], scalar=cm[:, 3 * j + p_:3 * j + p_ + 1],
                                                                       in1=Gh[:, p_, :], op0=ALU.mult, op1=ALU.add), ["Fall", "cm", "Gh"], ["Gh"])
                gv = lambda p_, ri: Gh[:, p_, :].rearrange("p (q r) -> p q r", r=2)[:, :, ri]
                lre = sml[:, 2, :]
                lim = sml[:, 3, :]
                ta = tm[:, 0, 0, 0:32]
                tb = tm[:, 1, 0, 0:32]
                acc_re = Gh[:, 3, 0:32]
                acc_im = Gh[:, 3, 32:64]
                kk = ["Gh", "sml", "tm"]

                def horner(src_re, src_im, add_p, o_re, o_im):
                    V(lambda e: e.tensor_tensor(out=ta, in0=src_re, in1=lre, op=ALU.mult), kk, ["tm"])
                    V(lambda e: e.tensor_tensor(out=tb, in0=src_im, in1=lim, op=ALU.mult), kk, ["tm"])
                    V(lambda e: e.tensor_tensor(out=ta, in0=ta, in1=tb, op=ALU.subtract), ["tm"], ["tm"])
                    V(lambda e: e.tensor_tensor(out=tb, in0=src_re, in1=lim, op=ALU.mult), kk, ["tm"])
                    V(lambda e: e.tensor_tensor(out=o_re, in0=ta, in1=gv(add_p, 0), op=ALU.add), kk, ["Gh", "Xc"])
                    V(lambda e: e.tensor_tensor(out=ta, in0=src_im, in1=lre, op=ALU.mult), kk, ["tm"])
                    V(lambda e: e.tensor_tensor(out=tb, in0=tb, in1=ta, op=ALU.add), ["tm"], ["tm"])
                    V(lambda e: e.tensor_tensor(out=o_im, in0=tb, in1=gv(add_p, 1), op=ALU.add), kk, ["Gh", "Xc"])
                horner(gv(2, 0), gv(2, 1), 1, acc_re, acc_im)
                horner(acc_re, acc_im, 0, Xc[:, :, 0], Xc[:, :, 1])
                dump("Fst", Fst, [128, 32, 2], F32, "Fst")
                dump("Xc", Xc, [128, 32, 2], F32, "Xc")

                for blk in range(8):
                    chunk_states(blk, True)
                    cw = Wcs[:, 0, :, 1:NCH]
                    sw = Wcs[:, 1, :, 1:NCH]
                    xr = Xt[:, :, 0, 0:NCH - 1]
                    xi = Xt[:, :, 1, 0:NCH - 1]
                    t_a = tm[:, 0, :, 0:NCH - 1]
                    t_b = tm[:, 1, :, 0:NCH - 1]
                    kk = ["Sq", "Wcs", "tm"]
                    V(lambda e, xr=xr, cw=cw, t_a=t_a: e.tensor_tensor(out=t_a, in0=xr, in1=cw, op=ALU.mult), kk, ["tm"])
                    V(lambda e, xi=xi, sw=sw, t_b=t_b: e.tensor_tensor(out=t_b, in0=xi, in1=sw, op=ALU.mult), kk, ["tm"])
                    V(lambda e, t_a=t_a, t_b=t_b: e.tensor_tensor(out=Xb[:, :, 0, 1:NCH], in0=t_a, in1=t_b, op=ALU.subtract), ["tm"], ["Xb"])
                    V(lambda e, xr=xr, sw=sw, t_a=t_a: e.tensor_tensor(out=t_a, in0=xr, in1=sw, op=ALU.mult), kk, ["tm"])
                    V(lambda e, xi=xi, cw=cw, t_b=t_b: e.tensor_tensor(out=t_b, in0=xi, in1=cw, op=ALU.mult), kk, ["tm"])
                    V(lambda e, t_a=t_a, t_b=t_b: e.tensor_tensor(out=Xb[:, :, 1, 1:NCH], in0=t_a, in1=t_b, op=ALU.add), ["tm"], ["Xb"])
                    banks = [nbank() for _ in range(4)]
                    pks = ["ps%d" % b for b in banks]

                    def omm(e, blk=blk, banks=banks):
                        ins = None
                        for j in range(4):
                            pb = ps[banks[j]]
                            first = True
                            for tau in range(0, 2 * j + 2):
                                s_lo = max(2 * j, tau)
                                ns = 2 * j + 2 - s_lo
                                o0 = (s_lo - 2 * j) * 256
                                ins = e.matmul(pb[:, o0:o0 + ns * 256], lhsT=Ktab[:, blk, tau, :],
                                               rhs=zs[:, blk, s_lo - tau:s_lo - tau + ns, 2:NCH],
                                               start=first, stop=False)
                                first = False
                            for sl in range(2):
                                s = 2 * j + sl
                                for ql in range(4):
                                    for ri in range(2):
                                        ins = e.matmul(pb[32 * ql:32 * ql + 32, sl * 256:(sl + 1) * 256],
                                                       lhsT=Qtab[:, 4 * blk + ql, s + 1, ri, :], rhs=Xb[:, ql, ri, 2:NCH],
                                                       start=False, stop=(sl == 1 and ql == 3 and ri == 1),
                                                       tile_position=(0, 32 * ql))
                        return ins
                    PE(omm, ["Ktab", "zs", "Qtab", "Xb"], pks)
                    for hf in range(2):
                        for jj in range(2):
                            j = 2 * hf + jj
                            V(lambda e, j=j, jj=jj, blk=blk, banks=banks: e.scalar_tensor_tensor(
                                out=yt[:, jj * 512:(jj + 1) * 512].rearrange("p (s c) -> p s c", s=2),
                                in0=zs[:, blk, 2 * j:2 * j + 2, 2:NCH], scalar=cols[:, 0, blk:blk + 1],
                                in1=ps[banks[j]][:, 0:512].rearrange("p (s c) -> p s c", s=2), op0=ALU.mult, op1=ALU.add),
                              ["zs", "cols", pks[j]], ["yt"])
                        A(lambda e: e.activation(out=y2[:], in_=yt[:], func=AF.Square), ["yt"], ["y2"])
                        V(lambda e: e.tensor_scalar(out=y2[:], in0=y2[:], scalar1=0.044715, scalar2=1.0, op0=ALU.mult, op1=ALU.add), ["y2"], ["y2"])
                        V(lambda e: e.tensor_tensor(out=y2[:], in0=y2[:], in1=yt[:], op=ALU.mult), ["y2", "yt"], ["y2"])
                        A(lambda e: e.activation(out=y2[:], in_=y2[:], func=AF.Sigmoid, scale=1.5957691216057308), ["y2"], ["y2"])
                        V(lambda e, blk=blk, hf=hf: e.tensor_tensor(
                            out=sT[:, blk, :].rearrange("p (c s) -> p s c", s=TCH)[:, 4 * hf:4 * hf + 4, :],
                            in0=yt[:].rearrange("p (s c) -> p s c", s=4),
                            in1=y2[:].rearrange("p (s c) -> p s c", s=4), op=ALU.mult), ["yt", "y2"], ["sT"])
            dump("sT", sT, [128, 8, NT], BF16, "sT")
            S.barrier()
            ssm_stack.close()


        w_in_v = w_in.rearrange("(k p) n -> p k n", p=128)
        pbk = ExitStack()
        gain = T(pbk, "gain", [128, D])
        XH = T(pbk, "XH", [128, 4, D])
        ubs = [T(pbk, "vb%d" % i, [128, D], BF16) for i in range(2)]
        sqs = [T(pbk, "sqb%d" % i, [128, 4]) for i in range(2)]
        uT = XH[:, 2:4, :].rearrange("p a b -> p (a b)").bitcast(BF16).rearrange("p (k t) -> p k t", t=512)
        UTK = ["xh2", "xh3"]
        zp = T(pbk, "zp", [128, 8, NH + 512], BF16)
        pa = T(pbk, "pa", [128, NH + 512])
        pb_ = T(pbk, "pb", [128, NH + 512])
        dT = T(pbk, "dT", [128, 8, 512], BF16)
        s2T = T(pbk, "s2T", [128, 8, 512], BF16)
        mgT = T(pbk, "mgT", [128, KC, 512], BF16)
        NRING = 5
        ring = [T(pbk, "ring%d" % i, [128, 2048]) for i in range(NRING)]
        ring_i = [0]
        sg = [T(pbk, "sg%d" % i, [128, 512]) for i in range(3)]
        vT32 = T(pbk, "vT32", [128, KC, 128])
        wr = T(pbk, "wr", [128, KC, 72])
        br = T(pbk, "br", [128, 72])
        lg = T(pbk, "lg", [128, 72])
        rt = T(pbk, "rt", [128, 12, 64])
        rs = T(pbk, "rs", [128, 16])
        base = T(pbk, "base", [128, 64])
        mb = T(pbk, "mb", [128, 64], BF16)
        DM(lambda e: e.dma_start(out=wr[:], in_=wr_d), [], ["wr"])
        DM(lambda e: e.dma_start(out=br[:], in_=br_d), [], ["br"])
        V(lambda e: e.memset(base[:], 0.0), [], ["base"])
        V(lambda e: e.memset(ubs[0][:], 0.0), [], ["vb0"])
        for r in range((NE * CAP + 128) // 128):
            DM(lambda e, r=r: e.dma_start(out=xg[r * 128:(r + 1) * 128, :], in_=ubs[0][:]), ["vb0"], ["xg"])

        def ring_load(src_ap, shape_view):
            i = ring_i[0] % NRING
            ring_i[0] += 1
            key = "ring%d" % i
            dst = ring[i]
            DM(lambda e: e.dma_start(out=shape_view(dst), in_=src_ap), [], [key])
            return dst, key

        def w_piece(w_v, kc0, nkc, c0, ncols):
            dst, key = ring_load(w_v[:, kc0:kc0 + nkc, c0:c0 + ncols],
                                 lambda d: d[:, 0:nkc * ncols].rearrange("p (k n) -> p k n", n=ncols))
            return (lambda kl, c, n: hi(dst[:, kl * ncols + c:kl * ncols + c + n])), key

        nt_i = [0]

        def ntile():
            i = nt_i[0] % 2
            nt_i[0] += 1
            return (XH[:, i, :], ubs[i], sqs[i], "xh%d" % i, "vb%d" % i)

        pool_w_v = pool_w.rearrange("g (k p) n -> p g k n", p=128)
        w_glu_v = w_glu.rearrange("(k p) n -> p k n", p=128)
        w_bp_v = w_bp.rearrange("(k p) n -> p k n", p=128)
        w_bs_v = w_bs.rearrange("(k p) n -> p k n", p=128)
        w_out_v = w_out.rearrange("(k p) n -> p k n", p=128)
        ev_i = [0]

        def evac_copy(dst, src, rk, wk):
            ev_i[0] += 1
            if ev_i[0] % 2 == 0:
                A(lambda e: e.activation(out=dst, in_=src, func=AF.Copy), rk, wk)
            else:
                V(lambda e: e.tensor_copy(out=dst, in_=src), rk, wk)

        def zpool_cols(ncol, col_off, uT_ap, utk):
            for mp in range(4):
                getters = [w_piece(w_in_v, 8 * hk, 8, 256 * mp, 256) for hk in range(2)]
                for mm in range(2):
                    m = 2 * mp + mm
                    b = nbank()
                    pk = "ps%d" % b

                    def zmm(e, mm=mm, b=b):
                        ins = None
                        for kc in range(KC):
                            g_, _ = getters[kc // 8]
                            ins = e.matmul(ps[b][:, 0:ncol], lhsT=g_(kc % 8, mm * 128, 128), rhs=uT_ap[:, kc, 0:ncol],
                                           start=(kc == 0), stop=(kc == KC - 1))
                        return ins
                    PE(zmm, [getters[0][1], getters[1][1]] + utk, [pk])
                    evac_copy(zp[:, m, col_off:col_off + ncol], ps[b][:, 0:ncol], [pk], ["zp"])

        for tb in range(NBLK):
            tok0 = 512 * tb
            DM(lambda e: e.dma_start(out=gain[:], in_=gains_d[:, 0, :]), [], ["gains"])
            if tb == 0:
                load_norm_T(ntile(), 0, NH, gain, uT, 0, UTK)
                zpool_cols(NH, 0, uT, UTK)
            else:
                G(lambda e: e.tensor_copy(out=zp[:, :, 0:NH], in_=zp[:, :, 512:512 + NH]), ["zp"], ["zp"])
            for tt in range(4):
                load_norm_T(ntile(), NH + tok0 + 128 * tt, 128, gain, uT, 128 * tt, UTK)
            zpool_cols(512, NH, uT, UTK)
            for m in range(8):
                steps = m // 2 + 1
                srcs = [zp[:, m, :], pa[:], pb_[:], pa[:], pb_[:]]
                keys = ["zp", "pa", "pb", "pa", "pb"]
                for sidx in range(steps):
                    sh = 1 << sidx
                    lo = 2 * sh - 1
                    G(lambda e, sidx=sidx, sh=sh, lo=lo, srcs=srcs: e.tensor_tensor(
                        out=srcs[sidx + 1][:, lo:NH + 512], in0=srcs[sidx][:, lo:NH + 512], in1=srcs[sidx][:, lo - sh:NH + 512 - sh],
                        op=ALU.add), [keys[sidx]], [keys[sidx + 1]])
                V(lambda e, m=m, steps=steps, srcs=srcs: e.scalar_tensor_tensor(
                    out=dT[:, m, :], in0=srcs[steps][:, NH:NH + 512], scalar=1.0 / (1 << steps), in1=zp[:, m, NH:NH + 512],
                    op0=ALU.mult, op1=ALU.subtract), [keys[steps], "zp"], ["dT"])
            for gi in range(4):
                dst, key = ring_load(pool_w_v[:, gi, :, :], lambda d: d[:, 0:512].rearrange("p (k n) -> p k n", n=256))
                bks = [nbank(), nbank()]

                def pmm(e, gi=gi, dst=dst, bks=bks):
                    ins = None
                    for mo in range(2):
                        for kc in range(2):
                            ins = e.matmul(ps[bks[mo]][:, 0:512], lhsT=hi(dst[:, kc * 256 + mo * 128:kc * 256 + mo * 128 + 128]),
                                           rhs=dT[:, 2 * gi + kc, :], start=(kc == 0), stop=(kc == 1))
                    return ins
                PE(pmm, [key, "dT"], ["ps%d" % bks[0], "ps%d" % bks[1]])
                for mo in range(2):
                    m = 2 * gi + mo
                    A(lambda e, m=m, mo=mo, bks=bks: e.activation(out=dT[:, m, :], in_=ps[bks[mo]][:, 0:512], func=AF.Copy,
                                                                   scale=cols[:, 1, m:m + 1]), ["ps%d" % bks[mo], "cols"], ["dT"])
            for mp in range(4):
                g_, key = w_piece(w_glu_v, 0, 8, 256 * mp, 256)
                for mm in range(2):
                    mo = 2 * mp + mm
                    b = nbank()
                    pk = "ps%d" % b

                    def gmm(e, mm=mm, b=b, g_=g_):
                        ins = None
                        for kc in range(8):
                            ins = e.matmul(ps[b][:, 0:512], lhsT=g_(kc, mm * 128, 128), rhs=sT[:, kc, tok0:tok0 + 512],
                                           start=(kc == 0), stop=(kc == 7))
                        return ins
                    PE(gmm, [key, "sT"], [pk])
                    A(lambda e, mo=mo, b=b: e.activation(out=sg[0][:], in_=ps[b][:, 0:512], func=AF.Sigmoid,
                                                         bias=cols[:, 2, mo:mo + 1], scale=1.0), [pk, "cols"], ["sg0"])
                    V(lambda e, mo=mo: e.tensor_tensor(out=s2T[:, mo, :], in0=sT[:, mo, tok0:tok0 + 512], in1=sg[0][:], op=ALU.mult),
                      ["sg0", "sT"], ["s2T"])
            for jp in range(8):
                bk = [nbank() for _ in range(8)]
                pks = ["ps%d" % b for b in bk]
                gbp, kbp = w_piece(w_bp_v, 0, 8, 256 * jp, 256)

                def m_yp(e, bk=bk, gbp=gbp):
                    ins = None
                    for jj in range(2):
                        for kc in range(8):
                            ins = e.matmul(ps[bk[jj]][:, 0:512], lhsT=gbp(kc, jj * 128, 128), rhs=dT[:, kc, :], start=(kc == 0), stop=(kc == 7))
                    return ins
                PE(m_yp, [kbp, "dT"], pks[0:2])
                gbs, kbs = w_piece(w_bs_v, 0, 8, 256 * jp, 256)

                def m_ys(e, bk=bk, gbs=gbs):
                    ins = None
                    for jj in range(2):
                        for kc in range(8):
                            ins = e.matmul(ps[bk[2 + jj]][:, 0:512], lhsT=gbs(kc, jj * 128, 128), rhs=s2T[:, kc, :], start=(kc == 0), stop=(kc == 7))
                    return ins
                PE(m_ys, [kbs, "s2T"], pks[2:4])
                for gsel in range(2):
                    gg = [w_piece(w_in_v, 8 * hk, 8, 2048 * (gsel + 1) + 256 * jp, 256) for hk in range(2)]

                    def m_g(e, bk=bk, gg=gg, gsel=gsel):
                        ins = None
                        for jj in range(2):
                            for kc in range(KC):
                                ins = e.matmul(ps[bk[4 + 2 * gsel + jj]][:, 0:512], lhsT=gg[kc // 8][0](kc % 8, jj * 128, 128), rhs=uT[:, kc, :],
                                               start=(kc == 0), stop=(kc == KC - 1))
                        return ins
                    PE(m_g, [gg[0][1], gg[1][1]] + UTK, pks[4 + 2 * gsel:6 + 2 * gsel])
                for jj in range(2):
                    j = 2 * jp + jj
                    A(lambda e, bk=bk, jj=jj: e.activation(out=sg[0][:], in_=ps[bk[4 + jj]][:, 0:512], func=AF.Sigmoid), [pks[4 + jj]], ["sg0"])
                    A(lambda e, bk=bk, jj=jj: e.activation(out=sg[1][:], in_=ps[bk[6 + jj]][:, 0:512], func=AF.Sigmoid), [pks[6 + jj]], ["sg1"])
                    V(lambda e, bk=bk, jj=jj: e.tensor_tensor(out=sg[0][:], in0=sg[0][:], in1=ps[bk[jj]][:, 0:512], op=ALU.mult), ["sg0", pks[jj]], ["sg0"])
                    V(lambda e, bk=bk, jj=jj: e.tensor_tensor(out=sg[1][:], in0=sg[1][:], in1=ps[bk[2 + jj]][:, 0:512], op=ALU.mult), ["sg1", pks[2 + jj]], ["sg1"])
                    G(lambda e, j=j: e.tensor_tensor(out=mgT[:, j, :], in0=sg[0][:], in1=sg[1][:], op=ALU.add), ["sg0", "sg1"], ["mgT"])
            if tb == 0:
                dump("zp", zp, [128, 8, NH + 512], BF16, "zp")
                dump("dT", dT, [128, 8, 512], BF16, "dT")
                dump("s2T", s2T, [128, 8, 512], BF16, "s2T")
                dump("mgT", mgT, [128, KC, 512], BF16, "mgT")
            for tt in range(4):
                DM(lambda e, tt=tt: e.dma_start(out=XH[:, tt, :], in_=xs[NH + tok0 + 128 * tt:NH + tok0 + 128 * (tt + 1), :]),
                   [], ["xh%d" % tt])
            DM(lambda e: e.dma_start(out=gain[:], in_=gains_d[:, 1, :]), [], ["gains"])
            for nb in range(4):
                bk = [nbank() for _ in range(4)]
                pks = ["ps%d" % b for b in bk]
                pieces = [w_piece(w_out_v, 4 * kq, 4, 512 * nb, 512) for kq in range(4)]

                def omm2(e, bk=bk, pieces=pieces):
                    ins = None
                    for kq in range(4):
                        for tt in range(4):
                            for kl in range(4):
                                kc = 4 * kq + kl
                                ins = e.matmul(ps[bk[tt]][:, 0:512], lhsT=mgT[:, kc, 128 * tt:128 * (tt + 1)],
                                               rhs=pieces[kq][0](kl, 0, 512), start=(kc == 0), stop=(kc == KC - 1))
                    return ins
                PE(omm2, [p_[1] for p_ in pieces] + ["mgT"], pks)
                for tt in range(4):
                    V(lambda e, tt=tt, nb=nb, bk=bk: e.tensor_tensor(out=XH[:, tt, 512 * nb:512 * (nb + 1)], in0=XH[:, tt, 512 * nb:512 * (nb + 1)],
                                                                     in1=ps[bk[tt]][:, 0:512], op=ALU.add), [pks[tt], "xh%d" % tt], ["xh%d" % tt])
            for tt in range(4):
                ti = 4 * tb + tt
                hk = "xh%d" % tt
                ht = XH[:, tt, :]
                vb = ubs[tt % 2]
                vk = "vb%d" % (tt % 2)
                sq = sqs[tt % 2]
                sk = "sqb%d" % (tt % 2)
                DM(lambda e, ti=ti, ht=ht: e.dma_start(out=hs[128 * ti:128 * (ti + 1), :], in_=ht), [hk], ["hs"])
                A(lambda e, ht=ht, vb=vb, sq=sq: e.activation(out=vb[:], in_=ht, func=AF.Square, accum_out=sq[:, 0:1]), [hk], [vk, sk])
                V(lambda e, sq=sq: e.tensor_scalar(out=sq[:, 1:2], in0=sq[:, 0:1], scalar1=1.0 / D, scalar2=EPS, op0=ALU.mult, op1=ALU.add), [sk], [sk])
                A(lambda e, sq=sq: e.activation(out=sq[:, 1:2], in_=sq[:, 1:2], func=AF.Sqrt), [sk], [sk])
                V(lambda e, sq=sq: e.reciprocal(out=sq[:, 2:3], in_=sq[:, 1:2]), [sk], [sk])
                V(lambda e, ht=ht, sq=sq: e.scalar_tensor_tensor(out=ht, in0=ht, scalar=sq[:, 2:3], in1=gain[:], op0=ALU.mult, op1=ALU.mult),
                  [hk, sk, "gains"], [hk])
                G(lambda e, ht=ht, vb=vb: e.tensor_copy(out=vb[:], in_=ht), [hk], [vk])
                for qd in range(4):
                    b = nbank()
                    pk = "ps%d" % b

                    def vtr(e, qd=qd, b=b, ht=ht):
                        ins = None
                        for j in range(4):
                            kc = 4 * qd + j
                            ins = e.transpose(out=ps[b][:, j * 128:(j + 1) * 128], in_=ht[:, kc * 128:(kc + 1) * 128], identity=ident32)
                        return ins
                    PE(vtr, [hk, "cst"], [pk])
                    evac_copy(vT32[:, 4 * qd:4 * qd + 4, :], ps[b][:, 0:512].rearrange("p (k t) -> p k t", t=128), [pk], ["vT32"])
                b = nbank()
                pk = "ps%d" % b

                def rmm(e, b=b):
                    ins = None
                    for kc in range(KC):
                        ins = e.matmul(ps[b][:, 0:72], lhsT=vT32[:, kc, :], rhs=wr[:, kc, :], start=(kc == 0), stop=(kc == KC - 1))
                    return ins
                PE(rmm, ["vT32", "wr"], [pk])
                V(lambda e, b=b: e.tensor_tensor(out=lg[:], in0=ps[b][:, 0:72], in1=br[:], op=ALU.add), [pk, "br"], ["lg"])
                R = lambda i: rt[:, i, :]
                K_ = ["rt", "rs", "lg"]
                W_ = ["rt", "rs"]
                V(lambda e: e.reduce_max(out=rs[:, 0:1], in_=lg[:, 0:8], axis=AX.X), K_, W_)
                V(lambda e: e.tensor_scalar(out=R(0)[:, 0:8], in0=lg[:, 0:8], scalar1=rs[:, 0:1], scalar2=None, op0=ALU.is_equal), K_, W_)
                V(lambda e: e.tensor_scalar(out=rs[:, 1:2], in0=rs[:, 0:1], scalar1=-1.0, scalar2=None, op0=ALU.mult), K_, W_)
                A(lambda e: e.activation(out=R(1)[:, 0:8], in_=lg[:, 0:8], func=AF.Exp, bias=rs[:, 1:2], scale=1.0, accum_out=rs[:, 2:3]), K_, W_)
                V(lambda e: e.reciprocal(out=rs[:, 3:4], in_=rs[:, 2:3]), K_, W_)
                V(lambda e: e.tensor_scalar(out=R(1)[:, 0:8], in0=R(0)[:, 0:8], scalar1=1e30, scalar2=-1e30, op0=ALU.mult, op1=ALU.add), K_, W_)
                V(lambda e: e.tensor_tensor(out=R(2).rearrange("p (g x) -> p g x", x=8), in0=lg[:, 8:72].rearrange("p (g x) -> p g x", x=8),
                                            in1=R(1)[:, 0:8].unsqueeze(2).to_broadcast([128, 8, 8]), op=ALU.add), K_, W_)
                V(lambda e: e.reduce_max(out=rs[:, 4:5], in_=R(2), axis=AX.X), K_, W_)
                V(lambda e: e.tensor_scalar(out=R(3), in0=R(2), scalar1=rs[:, 4:5], scalar2=None, op0=ALU.is_equal), K_, W_)
                V(lambda e: e.scalar_tensor_tensor(out=R(4), in0=R(3), scalar=-1e30, in1=R(2), op0=ALU.mult, op1=ALU.add), K_, W_)
                V(lambda e: e.reduce_max(out=rs[:, 5:6], in_=R(4), axis=AX.X), K_, W_)
                V(lambda e: e.tensor_scalar(out=R(5), in0=R(4), scalar1=rs[:, 5:6], scalar2=None, op0=ALU.is_equal), K_, W_)
                V(lambda e: e.tensor_tensor(out=rs[:, 6:7], in0=rs[:, 5:6], in1=rs[:, 4:5], op=ALU.subtract), K_, W_)
                A(lambda e: e.activation(out=rs[:, 6:7], in_=rs[:, 6:7], func=AF.Exp), K_, W_)
                V(lambda e: e.tensor_scalar(out=rs[:, 6:7], in0=rs[:, 6:7], scalar1=1.0, scalar2=None, op0=ALU.add), K_, W_)
                V(lambda e: e.reciprocal(out=rs[:, 7:8], in_=rs[:, 6:7]), K_, W_)
                V(lambda e, ti=ti: e.tensor_tensor(out=gw[:, ti, 0:1], in0=rs[:, 7:8], in1=rs[:, 3:4], op=ALU.mult), K_, W_ + ["gw"])
                V(lambda e, ti=ti: e.tensor_tensor(out=gw[:, ti, 1:2], in0=rs[:, 3:4], in1=gw[:, ti, 0:1], op=ALU.subtract), K_ + ["gw"], W_ + ["gw"])
                V(lambda e: e.tensor_tensor(out=mb[:], in0=R(3), in1=R(5), op=ALU.add), K_, ["mb"])
                b = nbank()
                pk = "ps%d" % b

                def cmm(e, b=b):
                    e.matmul(ps[b][:, 0:64], lhsT=trib, rhs=mb[:], start=True, stop=True)
                    return e.matmul(ps[b][:, 64:128], lhsT=onesb, rhs=mb[:], start=True, stop=True)
                PE(cmm, ["mb", "cstb"], [pk])
                V(lambda e, b=b: e.tensor_tensor(out=R(6), in0=ps[b][:, 0:64], in1=base[:], op=ALU.add), [pk, "base"] + K_, W_)
                V(lambda e, b=b: e.tensor_tensor(out=base[:], in0=base[:], in1=ps[b][:, 64:128], op=ALU.add), [pk, "base"] + K_, ["base"])
                for k_ in range(2):
                    oh = R(3) if k_ == 0 else R(5)
                    V(lambda e, oh=oh: e.tensor_tensor(out=R(7), in0=oh, in1=R(6), op=ALU.mult), K_, W_)
                    V(lambda e: e.reduce_sum(out=rs[:, 8:9], in_=R(7), axis=AX.X), K_, W_)
                    V(lambda e: e.tensor_scalar(out=rs[:, 8:9], in0=rs[:, 8:9], scalar1=float(CAP - 1), scalar2=None, op0=ALU.min), K_, W_)
                    V(lambda e, oh=oh: e.tensor_tensor(out=R(7), in0=oh, in1=iota_e, op=ALU.mult), K_ + ["cst"], W_)
                    V(lambda e: e.reduce_sum(out=rs[:, 9:10], in_=R(7), axis=AX.X), K_, W_)
                    V(lambda e: e.scalar_tensor_tensor(out=rs[:, 10:11], in0=rs[:, 9:10], scalar=float(CAP), in1=rs[:, 8:9],
                                                       op0=ALU.mult, op1=ALU.add), K_, W_)
                    V(lambda e, ti=ti, k_=k_: e.tensor_copy(out=gidx[:, ti, k_:k_ + 1], in_=rs[:, 10:11]), K_, ["gi"])
                    S.dma(lambda e, ti=ti, k_=k_, vb=vb: e.indirect_dma_start(
                        out=xg, out_offset=bass.IndirectOffsetOnAxis(ap=gidx[:, ti, k_:k_ + 1], axis=0), in_=vb[:], in_offset=None),
                        ["gi", vk, "xg"], ["xg%d" % (ti * 2 + k_)], q="pool")
        dump("gw", gw, [128, 16, 2], F32, "gw")
        dump("lg", lg, [128, 72], F32, "lg")
        S.barrier()
        pbk.close()

        if stage >= 3:
            pc = ExitStack()
            NR2 = 8
            ring2 = [T(pc, "wrg%d" % i, [128, 4096]) for i in range(NR2)]
            r2_i = [0]
            Xe = [T(pc, "Xe%d" % i, [128, D], BF16) for i in range(2)]
            XeT = [T(pc, "XeT%d" % i, [128, KC, 128], BF16) for i in range(2)]
            hg = T(pc, "hg", [128, 512])
            hh = T(pc, "hh", [128, 512], BF16)
            hT = T(pc, "hT", [128, 4, 128], BF16)
            Ye = [T(pc, "Ye%d" % i, [128, D]) for i in range(2)]

            def piece2(src_ap, nk, ncols):
                i = r2_i[0] % NR2
                r2_i[0] += 1
                key = "wrg%d" % i
                dst = ring2[i]
                DM(lambda e: e.dma_start(out=dst[:, 0:nk * ncols].rearrange("p (k n) -> p k n", n=ncols), in_=src_ap), [], [key])
                return (lambda kl, c, n: hi(dst[:, kl * ncols + c:kl * ncols + c + n])), key

            for ex in range(NE):
                xe = Xe[ex % 2]
                xk = "Xe%d" % (ex % 2)
                xT = XeT[ex % 2]
                xtk = "XeT%d" % (ex % 2)
                DM(lambda e, ex=ex, xe=xe: e.dma_start(out=xe[:], in_=xg[ex * CAP:(ex + 1) * CAP, :]),
                   ["xg"] + ["xg%d" % i for i in range(32)], [xk])
                wg_v = w_gate[ex].rearrange("(k p) f -> p k f", p=128)
                wu_v = w_up[ex].rearrange("(k p) f -> p k f", p=128)
                wd_v = w_down[ex].rearrange("(k p) n -> p k n", p=128)
                pg_ = [piece2(wg_v[:, 8 * h_:8 * h_ + 8, :], 8, 512) for h_ in range(2)]
                pu_ = [piece2(wu_v[:, 8 * h_:8 * h_ + 8, :], 8, 512) for h_ in range(2)]
                pd_ = [piece2(wd_v[:, :, 1024 * h_:1024 * (h_ + 1)], 4, 1024) for h_ in range(2)]
                for half in range(2):
                    b = nbank()
                    pk = "ps%d" % b
                    psb = ps[b][:].bitcast(BF16)

                    def xtr(e, half=half, psb=psb, xe=xe):
                        ins = None
                        for j in range(8):
                            kc = half * 8 + j
                            ins = e.transpose(out=psb[:, j * 128:(j + 1) * 128], in_=xe[:, kc * 128:(kc + 1) * 128], identity=identb)
                        return ins
                    PE(xtr, [xk, "cstb"], [pk])
                    evac_copy(xT[:, half * 8:half * 8 + 8, :], psb[:, 0:1024].rearrange("p (k t) -> p k t", t=128), [pk], [xtk])
                bg, bu = nbank(), nbank()

                def gu(e, bg=bg, bu=bu, xT=xT, pg_=pg_, pu_=pu_):
                    ins = None
                    for kc in range(KC):
                        ins = e.matmul(ps[bg][:, 0:512], lhsT=xT[:, kc, :], rhs=pg_[kc // 8][0](kc % 8, 0, 512), start=(kc == 0), stop=(kc == KC - 1))
                    for kc in range(KC):
                        ins = e.matmul(ps[bu][:, 0:512], lhsT=xT[:, kc, :], rhs=pu_[kc // 8][0](kc % 8, 0, 512), start=(kc == 0), stop=(kc == KC - 1))
                    return ins
                PE(gu, [xtk] + [p_[1] for p_ in pg_ + pu_], ["ps%d" % bg, "ps%d" % bu])
                A(lambda e, bg=bg: e.activation(out=hg[:], in_=ps[bg][:, 0:512], func=AF.Silu), ["ps%d" % bg], ["hg"])
                V(lambda e, bu=bu: e.tensor_tensor(out=hh[:], in0=hg[:], in1=ps[bu][:, 0:512], op=ALU.mult), ["hg", "ps%d" % bu], ["hh"])
                b = nbank()
                pk = "ps%d" % b
                psb = ps[b][:].bitcast(BF16)

                def htr(e, psb=psb):
                    ins = None
                    for j in range(4):
                        ins = e.transpose(out=psb[:, j * 128:(j + 1) * 128], in_=hh[:, j * 128:(j + 1) * 128], identity=identb)
                    return ins
                PE(htr, ["hh", "cstb"], [pk])
                evac_copy(hT[:], psb[:, 0:512].rearrange("p (k t) -> p k t", t=128), [pk], ["hT"])
                ye = Ye[ex % 2]
                yk = "Ye%d" % (ex % 2)
                for nb in range(4):
                    b = nbank()
                    pk = "ps%d" % b

                    def dmm(e, nb=nb, b=b, pd_=pd_):
                        ins = None
                        for kc in range(4):
                            ins = e.matmul(ps[b][:, 0:512], lhsT=hT[:, kc, :], rhs=pd_[nb // 2][0](kc, (nb % 2) * 512, 512),
                                           start=(kc == 0), stop=(kc == 3))
                        return ins
                    PE(dmm, ["hT", pd_[nb // 2][1]], [pk])
                    evac_copy(ye[:, nb * 512:(nb + 1) * 512], ps[b][:, 0:512], [pk], [yk])
                DM(lambda e, ex=ex, ye=ye: e.dma_start(out=ysc[ex * CAP:(ex + 1) * CAP, :], in_=ye[:]), [yk], ["ysc"])
            S.barrier()
            pc.close()

        pd = ExitStack()
        gfin = T(pd, "gfin", [128, D])
        DM(lambda e: e.dma_start(out=gfin[:], in_=gains_d[:, 2, :]), [], ["gfin"])
        hts = [T(pd, "hD%d" % i, [128, D]) for i in range(2)]
        g1s = [T(pd, "g1_%d" % i, [128, D]) for i in range(2)]
        g2s = [T(pd, "g2_%d" % i, [128, D]) for i in range(2)]
        jnk = T(pd, "jnk", [128, D], BF16)
        sqd = [T(pd, "sqd%d" % i, [128, 4]) for i in range(2)]
        for ti in range(4 * NBLK):
            i = ti % 2
            ht, g1, g2, sq = hts[i], g1s[i], g2s[i], sqd[i]
            hk, k1, k2, sk = "hD%d" % i, "g1_%d" % i, "g2_%d" % i, "sqd%d" % i
            DM(lambda e, ti=ti, ht=ht: e.dma_start(out=ht[:], in_=hs[128 * ti:128 * (ti + 1), :]), ["hs"], [hk])
            if stage >= 3:
                S.dma(lambda e, ti=ti, g1=g1: e.indirect_dma_start(out=g1[:], out_offset=None, in_=ysc,
                      in_offset=bass.IndirectOffsetOnAxis(ap=gidx[:, ti, 0:1], axis=0)), ["ysc", "gi"], [k1], q="pool")
                S.dma(lambda e, ti=ti, g2=g2: e.indirect_dma_start(out=g2[:], out_offset=None, in_=ysc,
                      in_offset=bass.IndirectOffsetOnAxis(ap=gidx[:, ti, 1:2], axis=0)), ["ysc", "gi"], [k2], q="pool")
                V(lambda e, ti=ti, ht=ht, g1=g1: e.scalar_tensor_tensor(out=ht[:], in0=g1[:], scalar=gw[:, ti, 0:1], in1=ht[:],
                                                                        op0=ALU.mult, op1=ALU.add), [hk, k1, "gw"], [hk])
                V(lambda e, ti=ti, ht=ht, g2=g2: e.scalar_tensor_tensor(out=ht[:], in0=g2[:], scalar=gw[:, ti, 1:2], in1=ht[:],
                                                                        op0=ALU.mult, op1=ALU.add), [hk, k2, "gw"], [hk])
            A(lambda e, ht=ht, sq=sq: e.activation(out=jnk[:], in_=ht[:], func=AF.Square, accum_out=sq[:, 0:1]), [hk], ["jnk", sk])
            V(lambda e, sq=sq: e.tensor_scalar(out=sq[:, 1:2], in0=sq[:, 0:1], scalar1=1.0 / D, scalar2=EPS, op0=ALU.mult, op1=ALU.add), [sk], [sk])
            A(lambda e, sq=sq: e.activation(out=sq[:, 1:2], in_=sq[:, 1:2], func=AF.Sqrt), [sk], [sk])
            V(lambda e, sq=sq: e.reciprocal(out=sq[:, 2:3], in_=sq[:, 1:2]), [sk], [sk])
            V(lambda e, ht=ht, sq=sq: e.scalar_tensor_tensor(out=ht[:], in0=ht[:], scalar=sq[:, 2:3], in1=gfin[:], op0=ALU.mult, op1=ALU.mult),
              [hk, sk, "gfin"], [hk])
            DM(lambda e, ti=ti, ht=ht: e.dma_start(out=out_d[128 * ti:128 * (ti + 1), :], in_=ht[:]), [hk], ["out"], is_out=True)
        S.barrier()
        pd.close()
        S.emit()
    return nc


def build_rest(nc, st, S, L):
    pass


def _consts():
    c = np.zeros((128, 1024), np.float32)
    c[:, 0:128] = np.eye(128, dtype=np.float32)
    k = np.arange(128)
    c[:, 128:256] = (k[:, None] < k[None, :]).astype(np.float32)
    c[:, 256:384] = 1.0
    c[:, 384:384 + 259] = np.arange(259, dtype=np.float32)[None, :]
    c[:, 648:712] = np.arange(64, dtype=np.float32)[None, :]
    for j in range(2):
        c[:, 712 + j] = ((k // 16) % 2 == j)
        c[:, 718 + j] = (k // 64 == j)
    for j in range(4):
        c[:, 714 + j] = (k // 32 == j)
    c[:, 720] = k
    return c


def _prep(inp, stage=3):
    f = lambda a: np.ascontiguousarray(np.asarray(a, dtype=np.float32))
    x = f(inp["x"]); meta = f(inp["meta"])
    a_re = f(inp["ssm_a_re"])[0]; a_im = f(inp["ssm_a_im"])[0]; ldt = f(inp["ssm_log_dt"])[0]
    b_re = f(inp["ssm_b_re"])[0]; b_im = f(inp["ssm_b_im"])[0]
    c_re = f(inp["ssm_c_re"])[0]; c_im = f(inp["ssm_c_im"])[0]
    shared = {}
    shared["w_in"] = f(inp["w_in"])[0]
    shared["pool_w"] = f(inp["pool_w"])[0]
    shared["w_glu"] = f(inp["w_glu"])[0]
    shared["w_bp"] = f(inp["w_branch_pool"])[0]
    shared["w_bs"] = f(inp["w_branch_ssm"])[0]
    shared["w_out"] = f(inp["w_out"])[0]
    if stage >= 3:
        shared["w_gate"] = f(inp["w_gate"])[0]
        shared["w_up"] = f(inp["w_up"])[0]
        shared["w_down"] = f(inp["w_down"])[0]
    g = np.stack([f(inp["norm_mix"])[0], f(inp["norm_ffn"])[0], f(inp["norm_final"])], 0)
    shared["gains"] = np.ascontiguousarray(np.broadcast_to(g[None], (128, 3, D)))
    cols = np.stack([f(inp["ssm_d"])[0].reshape(8, 128).T, f(inp["pool_scale"])[0].reshape(8, 128).T,
                     f(inp["b_glu"])[0].reshape(8, 128).T], 1)
    shared["cols"] = np.ascontiguousarray(cols)
    wr = np.concatenate([f(inp["w_router_group"])[0], f(inp["w_router_expert"])[0]], 1)
    shared["wr"] = np.ascontiguousarray(wr.reshape(KC, 128, 72).transpose(1, 0, 2))
    br = np.concatenate([f(inp["b_router_group"])[0], f(inp["b_router_expert"])[0]], 0)
    shared["br"] = np.ascontiguousarray(np.broadcast_to(br[None], (128, 72)))
    def L1(a):
        t = a.reshape(8, 8, 64)
        t = np.broadcast_to(t[:, :, None, :], (8, 8, 16, 64))
        return t.transpose(1, 2, 0, 3).reshape(128, 8, 64)
    ldt_b = np.broadcast_to(ldt[:, None], (64, 64))
    shared["aL1"] = np.ascontiguousarray(np.stack([L1(a_re), L1(a_im), L1(ldt_b)], 1))
    def B1(b):
        t = b.reshape(8, 8, 64, 16)
        return t.transpose(1, 3, 0, 2).reshape(128, 8, 64)
    shared["bL1"] = np.ascontiguousarray(np.stack([B1(b_re), B1(b_im)], 1))
    def L2(a):
        return a.reshape(32, 2, 64).transpose(1, 2, 0).reshape(128, 32)
    shared["aL2"] = np.ascontiguousarray(np.stack([L2(a_re), L2(a_im), L2(ldt_b)], 1))
    def B2(b):
        return b.reshape(32, 2, 64, 16).transpose(1, 2, 0, 3).reshape(128, 32, 16)
    shared["bL2"] = np.ascontiguousarray(np.stack([B2(b_re), B2(b_im)], 1))
    def C2(c):
        return c.reshape(32, 2, 16, 64).transpose(1, 3, 0, 2).reshape(128, 32, 16)
    shared["cL2"] = np.ascontiguousarray(np.stack([C2(c_re), C2(c_im)], 1))
    shared["cst"] = _consts()
    maps = []
    for c in range(8):
        b, k = c // 4, c % 4
        halo = meta if k == 0 else x[b, NT * k - NH:NT * k]
        m = dict(shared)
        m["xs"] = np.ascontiguousarray(np.concatenate([halo, x[b, NT * k:NT * (k + 1)]], 0))
        m["hm"] = np.full((128, 1), 1.0 if k == 0 else 0.0, np.float32)
        cmv = np.zeros((4, 3), np.float32)
        for j in range(k):
            cmv[j, k - 1 - j] = 1.0
        m["cm"] = np.ascontiguousarray(np.broadcast_to(cmv.reshape(1, 12), (128, 12)))
        maps.append(m)
    return maps


_NC_CACHE = {}


def kernel(**inputs):
    if "nc" not in _NC_CACHE:
        _NC_CACHE["nc"] = build(False, 3)
    nc = _NC_CACHE["nc"]
    maps = _prep(inputs, 3)
    res = run_bass_kernel_spmd(nc, maps, core_ids=list(range(8)))
    out = np.empty((2, 8192, D), np.float32)
    for c in range(8):
        b, k = c // 4, c % 4
        out[b, NT * k:NT * (k + 1)] = res.results[c]["out"]
    return out
```

```python
import math
from contextlib import ExitStack

import numpy as np
import concourse.bass as bass
import concourse.mybir as mybir
from concourse.bass_utils import run_bass_kernel_spmd

F32 = mybir.dt.float32
BF16 = mybir.dt.bfloat16
I32 = mybir.dt.int32
ALU = mybir.AluOpType
AF = mybir.ActivationFunctionType
AX = mybir.AxisListType

D = 2048
KC = 16
NT = 2048
NH = 16
NTOK = NT + NH
TCH = 8
NCH = NTOK // TCH
NE = 64
CAP = 128
EPS = 1e-6
MAGIC = 12582912.0
TWO_PI = 2.0 * math.pi
DEBUG = {}


class Sched:
    ENGS = ("pe", "act", "dve", "pool", "sp")
    NDMA = 14

    def __init__(self, nc, stack):
        self.nc = nc
        self.ops = {e: [] for e in self.ENGS}
        self.cnt = {e: 0 for e in self.ENGS}
        self.sem = {e: stack.enter_context(nc.semaphore("s_" + e)) for e in self.ENGS}
        self.dsem = {
            q: [stack.enter_context(nc.semaphore(f"d_{q}{i}")) for i in range(self.NDMA)]
            for q in ("sp", "pool")
        }
        self.ccsem = stack.enter_context(nc.semaphore("cc"))
        self.dcnt = {"sp": 0, "pool": 0}
        self.known = {e: {} for e in self.ENGS}
        self.last_w = {}
        self.readers = {}
        self.out_tokens = []
        self.all_tokens = {}
        self.block = stack.enter_context(nc.Block())
        self.eobj = {"pe": nc.tensor, "act": nc.scalar, "dve": nc.vector, "pool": nc.gpsimd, "sp": nc.sync}

    def _emit(self, eng, waits, fn, sem, inc):
        e = self.eobj[eng]
        for (s_, v) in waits:
            e.wait_ge(s_, v)
        if fn is not None:
            fn(e).then_inc(sem, inc)

    def _deps(self, eng, reads, writes):
        toks = []
        for k in reads:
            w = self.last_w.get(k)
            if w is not None:
                toks.append(w)
        for k in writes:
            w = self.last_w.get(k)
            if w is not None:
                toks.append(w)
            toks.extend(self.readers.get(k, ()))
        waits = {}
        for (sem, val, src) in toks:
            if src == "pe" and eng == "pe":
                continue
            key = id(sem)
            if self.known[eng].get(key, 0) >= val:
                continue
            if key not in waits or waits[key][1] < val:
                waits[key] = (sem, val)
        for key, (sem, val) in waits.items():
            self.known[eng][key] = val
        return list(waits.values())

    def _record(self, tok, reads, writes):
        self.all_tokens[id(tok[0])] = (tok[0], max(tok[1], self.all_tokens.get(id(tok[0]), (None, 0))[1]))
        for k in writes:
            self.last_w[k] = tok
            self.readers[k] = []
        for k in reads:
            self.readers.setdefault(k, []).append(tok)

    def op(self, eng, fn, reads=(), writes=()):
        waits = self._deps(eng, reads, writes)
        self.cnt[eng] += 1
        tok = (self.sem[eng], self.cnt[eng], eng)
        self._emit(eng, waits, fn, self.sem[eng], 1)
        self._record(tok, reads, writes)
        return tok

    def dma(self, fn, reads=(), writes=(), q="sp", is_out=False):
        waits = self._deps(q, reads, writes)
        i = self.dcnt[q]
        self.dcnt[q] += 1
        slot = i % self.NDMA
        rnd = i // self.NDMA
        sem = self.dsem[q][slot]
        if rnd > 0:
            key = id(sem)
            if self.known[q].get(key, 0) < 16 * rnd:
                waits.append((sem, 16 * rnd))
                self.known[q][key] = 16 * rnd
        tok = (sem, 16 * (rnd + 1), "dma")
        self._emit(q, waits, fn, sem, 16)
        self._record(tok, reads, writes)
        if is_out:
            self.out_tokens.append(tok)
        return tok

    def collective(self, fn, reads=(), writes=()):
        waits = self._deps("pool", reads, writes)
        tok = (self.ccsem, 1, "dma")
        self._emit("pool", waits, fn, self.ccsem, 1)
        self._record(tok, reads, writes)
        return tok

    def barrier(self):
        for e in self.ENGS:
            waits = []
            for key, (sem, val) in self.all_tokens.items():
                if self.known[e].get(key, 0) < val:
                    if e == "pe" and sem is self.sem["pe"]:
                        continue
                    waits.append((sem, val))
                    self.known[e][key] = val
            if waits:
                self._emit(e, waits, None, None, 0)
        self.last_w = {}
        self.readers = {}

    def emit(self):
        final = {}
        for (sem, val, _) in self.out_tokens:
            if id(sem) not in final or final[id(sem)][1] < val:
                final[id(sem)] = (sem, val)
        for (s_, v) in final.values():
            self.eobj["sp"].wait_ge(s_, v)


def hi(ap):
    return ap.bitcast(BF16)[:, 1::2]


def build(debug=False, stage=3, dev=None):
    dev = dev or {}
    NBLK = dev.get("nblk", 4)
    nc = bass.Bass("TRN2", target_bir_lowering=False)

    def din(name, shape, dt=F32):
        return nc.dram_tensor(name, list(shape), dt, kind="ExternalInput").ap()

    def dscr(name, shape, dt=F32):
        return nc.dram_tensor(name, list(shape), dt, kind="Internal").ap()

    xs = din("xs", [NTOK, D])
    hm_d = din("hm", [128, 1])
    cm_d = din("cm", [128, 12])
    w_in = din("w_in", [D, 6144])
    pool_w = din("pool_w", [4, 256, 256])
    w_glu = din("w_glu", [1024, 1024])
    w_bp = din("w_bp", [1024, D])
    w_bs = din("w_bs", [1024, D])
    w_out = din("w_out", [D, D])
    if stage >= 3:
        w_gate = din("w_gate", [NE, D, 512])
        w_up = din("w_up", [NE, D, 512])
        w_down = din("w_down", [NE, 512, D])
    gains_d = din("gains", [128, 3, D])
    cols_d = din("cols", [128, 3, 8])
    wr_d = din("wr", [128, KC, 72])
    br_d = din("br", [128, 72])
    aL1_d = din("aL1", [128, 3, 8, 64])
    bL1_d = din("bL1", [128, 2, 8, 64])
    aL2_d = din("aL2", [128, 3, 32])
    bL2_d = din("bL2", [128, 2, 32, 16])
    cL2_d = din("cL2", [128, 2, 32, 16])
    cst_d = din("cst", [128, 1024])
    out_d = nc.dram_tensor("out", [NT, D], F32, kind="ExternalOutput").ap()

    cc_in = dscr("cc_in", [128, 64])
    cc_out = dscr("cc_out", [4 * 128, 64])
    hs = dscr("hs", [NT, D])
    xg = dscr("xg", [NE * CAP + 128, D], BF16)
    ysc = dscr("ysc", [NE * CAP + 128, D])
    sTd = dscr("sTd", [128, 8 * NT], BF16)
    with ExitStack() as st:
        S = Sched(nc, st)

        def dump(name, tile_, shape, dt, key):
            if not debug:
                return
            d_ap = nc.dram_tensor("dbg_" + name, list(shape), dt, kind="ExternalOutput").ap()
            S.dma(lambda e: e.dma_start(out=d_ap, in_=tile_[:]), [key], [], is_out=True)

        def V(fn, r=(), w=()): return S.op("dve", fn, r, w)
        def A(fn, r=(), w=()): return S.op("act", fn, r, w)
        def G(fn, r=(), w=()): return S.op("pool", fn, r, w)
        def PE(fn, r=(), w=()): return S.op("pe", fn, r, w)
        def DM(fn, r=(), w=(), **kw): return S.dma(fn, r, w, **kw)

        def T(stk, name, shape, dt=F32):
            return stk.enter_context(nc.sbuf_tensor("sb_" + name, list(shape), dt))

        ps = [st.enter_context(nc.psum_tensor(f"ps{i}", [128, 512], F32)) for i in range(8)]
        ps_rr = [0]

        def nbank():
            i = ps_rr[0]
            ps_rr[0] = (i + 1) % 8
            return i

        cst = T(st, "cst", [128, 1024])
        cstb = T(st, "cstb", [128, 512], BF16)
        cols = T(st, "cols", [128, 3, 8])
        hm = T(st, "hm", [128, 1])
        cm = T(st, "cm", [128, 12])
        DM(lambda e: e.dma_start(out=cst[:], in_=cst_d), w=["cst"])
        DM(lambda e: e.dma_start(out=cols[:], in_=cols_d), w=["cols"])
        DM(lambda e: e.dma_start(out=hm[:], in_=hm_d), w=["hm"])
        DM(lambda e: e.dma_start(out=cm[:], in_=cm_d), w=["cm"])
        V(lambda e: e.tensor_copy(out=cstb[:, 0:384], in_=cst[:, 0:384]), ["cst"], ["cstb"])
        ident32 = cst[:, 0:128]
        identb = cstb[:, 0:128]
        trib = cstb[:, 128:256]
        onesb = cstb[:, 256:384]
        iota_c = cst[:, 384:384 + 259]
        iota_e = cst[:, 648:712]
        mg2 = [cst[:, 712:713], cst[:, 713:714]]
        mqd = [cst[:, 714 + j:715 + j] for j in range(4)]
        mh2 = [cst[:, 718:719], cst[:, 719:720]]
        iota_p = cst[:, 720:721]

        gw = T(st, "gw", [128, 16, 2])
        gidx = T(st, "gidx", [128, 16, 2], I32)
        zs = None

        def load_norm_T(stk_tiles, row0, nrows, gain, uT_ap, col0, tag):
            xt, ub, ssq, kx, ku = stk_tiles
            DM(lambda e: e.dma_start(out=xt[:nrows, :], in_=xs[row0:row0 + nrows, :]), [], [kx])
            A(lambda e: e.activation(out=ub[:nrows, :], in_=xt[:nrows, :], func=AF.Square, accum_out=ssq[:nrows, 0:1]), [kx], [ku, ku + "s"])
            V(lambda e: e.tensor_scalar(out=ssq[:nrows, 1:2], in0=ssq[:nrows, 0:1], scalar1=1.0 / D, scalar2=EPS,
                                        op0=ALU.mult, op1=ALU.add), [ku + "s"], [ku + "s"])
            A(lambda e: e.activation(out=ssq[:nrows, 1:2], in_=ssq[:nrows, 1:2], func=AF.Sqrt), [ku + "s"], [ku + "s"])
            V(lambda e: e.reciprocal(out=ssq[:nrows, 2:3], in_=ssq[:nrows, 1:2]), [ku + "s"], [ku + "s"])
            V(lambda e: e.scalar_tensor_tensor(out=ub[:nrows, :], in0=xt[:nrows, :], scalar=ssq[:nrows, 2:3],
                                               in1=gain[:nrows, :], op0=ALU.mult, op1=ALU.mult),
              [kx, ku + "s", "gains", ku], [ku])
            for half in range(2):
                b = nbank()
                pk = "ps%d" % b
                psb = ps[b][:].bitcast(BF16)

                def tr(e, half=half, psb=psb):
                    ins = None
                    for j in range(8):
                        kc = half * 8 + j
                        ins = e.transpose(out=psb[:, j * 128:j * 128 + nrows], in_=ub[:nrows, kc * 128:(kc + 1) * 128],
                                          identity=identb[:nrows, :nrows])
                    return ins
                PE(tr, [ku, "cstb"], [pk])
                eng = A if half == 0 else V
                if half == 0:
                    A(lambda e, half=half, psb=psb: e.activation(
                        out=uT_ap[:, half * 8:half * 8 + 8, col0:col0 + nrows],
                        in_=psb[:, 0:1024].rearrange("p (k t) -> p k t", t=128)[:, :, 0:nrows], func=AF.Copy), [pk], tag if isinstance(tag, list) else [tag])
                else:
                    V(lambda e, half=half, psb=psb: e.tensor_copy(
                        out=uT_ap[:, half * 8:half * 8 + 8, col0:col0 + nrows],
                        in_=psb[:, 0:1024].rearrange("p (k t) -> p k t", t=128)[:, :, 0:nrows]), [pk], tag if isinstance(tag, list) else [tag])


        if dev.get("skip_ssm"):
            sT_in = nc.dram_tensor("sT_in", [128, 8, NT], BF16, kind="ExternalInput").ap()
            with nc.allow_non_contiguous_dma(reason="dev"):
                pass
            DM(lambda e: e.dma_start(out=sTd.rearrange("p (a t) -> p a t", a=8), in_=sT_in), [], ["sTd"])
        else:
            ssm_stack = ExitStack()
            sT = T(ssm_stack, "sT", [128, 8, NT], BF16)
            zs = T(ssm_stack, "zs", [128, 8, 8, NCH], BF16)

            with ExitStack() as a1:
                gmix = T(a1, "gmix", [128, D])
                DM(lambda e: e.dma_start(out=gmix[:], in_=gains_d[:, 0, :]), w=["gains"])
                wss = T(a1, "wss", [128, KC, 1024])
                w_in_v = w_in.rearrange("(k p) n -> p k n", p=128)
                for j in range(8):
                    DM(lambda e, j=j: e.dma_start(out=wss[:, 2 * j:2 * j + 2, :], in_=w_in_v[:, 2 * j:2 * j + 2, 1024:2048]), [], ["wss"])
                xts = [T(a1, "xt%d" % i, [128, D]) for i in range(2)]
                ubs = [T(a1, "ub%d" % i, [128, D], BF16) for i in range(2)]
                sqs = [T(a1, "sq%d" % i, [128, 4]) for i in range(2)]
                uTs = [T(a1, "uT%d" % i, [128, KC, 512], BF16) for i in range(2)]
                tile_i = [0]

                def norm_tiles():
                    i = tile_i[0] % 2
                    tile_i[0] += 1
                    return (xts[i], ubs[i], sqs[i], "xt%d" % i, "ub%d" % i)

                blocks = [(0, NH)] + [(NH + 512 * b, 512) for b in range(4)]
                for bi, (c0, n) in enumerate(blocks):
                    uT = uTs[bi % 2]
                    tag = "uT%d" % (bi % 2)
                    for t0 in range(0, n, 128):
                        nr = min(128, n - t0)
                        load_norm_T(norm_tiles(), c0 + t0, nr, gmix, uT, t0, tag)
                    for m in range(8):
                        b = nbank()
                        pk = "ps%d" % b

                        def zmm(e, m=m, b=b, uT=uT, n=n):
                            ins = None
                            for kc in range(KC):
                                ins = e.matmul(ps[b][:, 0:n], lhsT=hi(wss[:, kc, m * 128:(m + 1) * 128]), rhs=uT[:, kc, 0:n],
                                               start=(kc == 0), stop=(kc == KC - 1))
                            return ins
                        PE(zmm, ["wss", tag], [pk])
                        ch0 = c0 // TCH
                        nchk = n // TCH
                        src = ps[b][:, 0:n].rearrange("p (c s) -> p s c", s=TCH)
                        dst = zs[:, m, :, ch0:ch0 + nchk]
                        if bi == 0:
                            V(lambda e, src=src, dst=dst: e.tensor_scalar(out=dst, in0=src, scalar1=hm[:, 0:1], scalar2=None,
                                                                          op0=ALU.mult), [pk, "hm"], ["zs"])
                        elif m % 2 == 0:
                            A(lambda e, src=src, dst=dst: e.activation(out=dst, in_=src, func=AF.Copy), [pk], ["zs"])
                        else:
                            V(lambda e, src=src, dst=dst: e.tensor_copy(out=dst, in_=src), [pk], ["zs"])
            S.barrier()
            Ptab = T(ssm_stack, "Ptab", [128, 8, 8, 2, 128], BF16)
            Qtab = T(ssm_stack, "Qtab", [128, 32, 9, 2, 32], BF16)
            Ktab = T(ssm_stack, "Ktab", [128, 8, 8, 128], BF16)
            sml = T(ssm_stack, "sml", [128, 8, 32])
            Fst = T(ssm_stack, "Fst", [128, 32, 2])
            Xc = T(ssm_stack, "Xc", [128, 32, 2])

            t1 = ExitStack()
            if True:
                aL1 = T(t1, "aL1", [128, 3, 256])
                bL1 = T(t1, "bL1", [128, 2, 256])
                w1 = T(t1, "w1", [128, 12, 256])
                pw = T(t1, "pw", [128, 2, 2, 256])
                PP = T(t1, "PP", [128, 2, 2, 256])

                def lam_block(pref, akey, a_re, a_im, ldt, W, n, mults):
                    r = [pref + "w"]
                    k = pref + "w"
                    A(lambda e: e.activation(out=W[:, 4, :n], in_=ldt, func=AF.Exp), r + [akey], [k])
                    V(lambda e: e.tensor_tensor(out=W[:, 5, :n], in0=a_re, in1=W[:, 4, :n], op=ALU.mult), r + [akey], [k])
                    V(lambda e: e.tensor_tensor(out=W[:, 6, :n], in0=a_im, in1=W[:, 4, :n], op=ALU.mult), r + [akey], [k])

                    def expi(kk, o_mag, o_re, o_im):
                        A(lambda e: e.activation(out=W[:, 7, :n], in_=W[:, 5, :n], func=AF.Exp, scale=float(kk)), r, [k])
                        for (off, dst) in ((0.0, 8), (0.25, 9)):
                            V(lambda e, off=off, dst=dst: e.tensor_scalar(out=W[:, dst, :n], in0=W[:, 6, :n], scalar1=float(kk) / TWO_PI,
                                                                          scalar2=off, op0=ALU.mult, op1=ALU.add), r, [k])
                            V(lambda e, dst=dst: e.tensor_scalar(out=W[:, 10, :n], in0=W[:, dst, :n], scalar1=MAGIC, scalar2=None,
                                                                 op0=ALU.add), r, [k])
                            V(lambda e, dst=dst: e.tensor_scalar(out=W[:, 10, :n], in0=W[:, 10, :n], scalar1=-MAGIC, scalar2=None,
                                                                 op0=ALU.add), r, [k])
                            V(lambda e, dst=dst: e.tensor_tensor(out=W[:, dst, :n], in0=W[:, dst, :n], in1=W[:, 10, :n],
                                                                 op=ALU.subtract), r, [k])
                            A(lambda e, dst=dst: e.activation(out=W[:, dst, :n], in_=W[:, dst, :n], func=AF.Sin, scale=TWO_PI), r, [k])
                        if o_mag is not None:
                            V(lambda e: e.tensor_copy(out=o_mag, in_=W[:, 7, :n]), r, [k, pref + "o"])
                        V(lambda e: e.tensor_tensor(out=o_re, in0=W[:, 7, :n], in1=W[:, 9, :n], op=ALU.mult), r, [k, pref + "o"])
                        V(lambda e: e.tensor_tensor(out=o_im, in0=W[:, 7, :n], in1=W[:, 8, :n], op=ALU.mult), r, [k, pref + "o"])

                    expi(1, None, W[:, 0, :n], W[:, 1, :n])
                    for (kk, om, ore, oim) in mults:
                        expi(kk, om, ore, oim)
                    lam_block.expi = expi
                    V(lambda e: e.tensor_tensor(out=W[:, 7, :n], in0=a_re, in1=a_re, op=ALU.mult), r + [akey], [k])
                    V(lambda e: e.tensor_tensor(out=W[:, 8, :n], in0=a_im, in1=a_im, op=ALU.mult), r + [akey], [k])
                    V(lambda e: e.tensor_tensor(out=W[:, 7, :n], in0=W[:, 7, :n], in1=W[:, 8, :n], op=ALU.add), r, [k])
                    V(lambda e: e.reciprocal(out=W[:, 7, :n], in_=W[:, 7, :n]), r, [k])
                    V(lambda e: e.tensor_scalar(out=W[:, 8, :n], in0=W[:, 0, :n], scalar1=-1.0, scalar2=None, op0=ALU.add), r, [k])
                    V(lambda e: e.tensor_tensor(out=W[:, 9, :n], in0=W[:, 8, :n], in1=a_re, op=ALU.mult), r + [akey], [k])
                    V(lambda e: e.tensor_tensor(out=W[:, 10, :n], in0=W[:, 1, :n], in1=a_im, op=ALU.mult), r + [akey], [k])
                    V(lambda e: e.tensor_tensor(out=W[:, 9, :n], in0=W[:, 9, :n], in1=W[:, 10, :n], op=ALU.add), r, [k])
                    V(lambda e: e.tensor_tensor(out=W[:, 2, :n], in0=W[:, 9, :n], in1=W[:, 7, :n], op=ALU.mult), r, [k])
                    V(lambda e: e.tensor_tensor(out=W[:, 9, :n], in0=W[:, 1, :n], in1=a_re, op=ALU.mult), r + [akey], [k])
                    V(lambda e: e.tensor_tensor(out=W[:, 10, :n], in0=W[:, 8, :n], in1=a_im, op=ALU.mult), r + [akey], [k])
                    V(lambda e: e.tensor_tensor(out=W[:, 9, :n], in0=W[:, 9, :n], in1=W[:, 10, :n], op=ALU.subtract), r, [k])
                    V(lambda e: e.tensor_tensor(out=W[:, 3, :n], in0=W[:, 9, :n], in1=W[:, 7, :n], op=ALU.mult), r, [k])

                def cmul(o_re, o_im, a_re, a_im, b_re, b_im, t1_, t2_, rk, wk, neg_im=False):
                    V(lambda e: e.tensor_tensor(out=t1_, in0=a_re, in1=b_re, op=ALU.mult), rk, wk)
                    V(lambda e: e.tensor_tensor(out=t2_, in0=a_im, in1=b_im, op=ALU.mult), rk, wk)
                    V(lambda e: e.tensor_tensor(out=o_re, in0=t1_, in1=t2_, op=ALU.subtract), rk, wk)
                    V(lambda e: e.tensor_tensor(out=t1_, in0=a_re, in1=b_im, op=ALU.mult), rk, wk)
                    V(lambda e: e.tensor_tensor(out=t2_, in0=a_im, in1=b_re, op=ALU.mult), rk, wk)
                    if neg_im:
                        V(lambda e: e.scalar_tensor_tensor(out=o_im, in0=t1_, scalar=-1.0, in1=t2_, op0=ALU.mult, op1=ALU.subtract), rk, wk)
                    else:
                        V(lambda e: e.tensor_tensor(out=o_im, in0=t1_, in1=t2_, op=ALU.add), rk, wk)

                Bb = T(t1, "Bb", [128, 2, 256])
                tA = T(t1, "tA", [128, 2, 256])
                tB = T(t1, "tB", [128, 2, 256])
                for hb in range(2):
                    DM(lambda e, hb=hb: e.dma_start(out=aL1[:].rearrange("p a (g n) -> p a g n", g=4), in_=aL1_d[:, :, 4 * hb:4 * hb + 4, :]), [], ["aL1"])
                    DM(lambda e, hb=hb: e.dma_start(out=bL1[:].rearrange("p a (g n) -> p a g n", g=4), in_=bL1_d[:, :, 4 * hb:4 * hb + 4, :]), [], ["bL1"])
                    lam_block("L1", "aL1", aL1[:, 0, :], aL1[:, 1, :], aL1[:, 2, :], w1, 256, [])
                    expi1 = lam_block.expi
                    KL1 = ["L1w", "L1o", "bL1"]
                    cmul(Bb[:, 0, :], Bb[:, 1, :], w1[:, 2, :], w1[:, 3, :], bL1[:, 0, :], bL1[:, 1, :],
                         w1[:, 10, :], w1[:, 11, :], KL1, ["L1w", "Bb"])
                    bb_re = Bb[:, 0:1, :].to_broadcast([128, 2, 256])
                    bb_im = Bb[:, 1:2, :].to_broadcast([128, 2, 256])
                    for kq in range(4):
                        for j in range(2):
                            kk_ = 2 * kq + j
                            if kk_ == 0:
                                V(lambda e: e.memset(pw[:, 0, 0, :], 1.0), ["PP"], ["L1o"])
                                V(lambda e: e.memset(pw[:, 1, 0, :], 0.0), ["PP"], ["L1o"])
                            else:
                                expi1(kk_, None, pw[:, 0, j, :], pw[:, 1, j, :])
                        cmul(PP[:, 0], PP[:, 1], pw[:, 0], pw[:, 1], bb_re, bb_im, tA[:], tB[:], KL1 + ["tAB", "Bb"], ["PP", "tAB"])
                        for ri in range(2):
                            for g2 in range(2):
                                V(lambda e, ri=ri, g2=g2, kq=kq, hb=hb: e.tensor_scalar(
                                    out=Ptab[:, 4 * hb:4 * hb + 4, 2 * kq:2 * kq + 2, ri, g2 * 64:(g2 + 1) * 64],
                                    in0=PP[:, ri].rearrange("p k (g n) -> p g k n", g=4),
                                    scalar1=mg2[g2], scalar2=None, op0=ALU.mult), ["PP", "cst"], ["Ptab"])
            S.barrier()
            t1.close()

            with ExitStack() as t2:
                aL2 = T(t2, "aL2", [128, 3, 32])
                bL2 = T(t2, "bL2", [128, 2, 512])
                cL2 = T(t2, "cL2", [128, 2, 512])
                DM(lambda e: e.dma_start(out=aL2[:], in_=aL2_d), w=["aL2"])
                DM(lambda e: e.dma_start(out=bL2[:], in_=bL2_d.rearrange("p a q h -> p a (q h)")), w=["bL2"])
                DM(lambda e: e.dma_start(out=cL2[:], in_=cL2_d.rearrange("p a q h -> p a (q h)")), w=["cL2"])
                w2 = T(t2, "w2", [128, 12, 32])
                pw2 = T(t2, "pw2", [128, 2, 9, 32])
                V(lambda e: e.memset(pw2[:, 0, 0, :], 1.0), [], ["L2o"])
                V(lambda e: e.memset(pw2[:, 1, 0, :], 0.0), [], ["L2o"])
                mults = [(kk, None, pw2[:, 0, kk, :], pw2[:, 1, kk, :]) for kk in range(1, 8)]
                mults.append((8, sml[:, 0, :], pw2[:, 0, 8, :], pw2[:, 1, 8, :]))
                mults.append((2048, None, sml[:, 2, :], sml[:, 3, :]))
                lam_block("L2", "aL2", aL2[:, 0, :], aL2[:, 1, :], aL2[:, 2, :], w2, 32, mults)
                KL2 = ["L2w", "L2o", "bL2", "cL2"]
                V(lambda e: e.tensor_scalar(out=sml[:, 1, :], in0=w2[:, 6, :], scalar1=8.0 / TWO_PI, scalar2=None, op0=ALU.mult), KL2, ["L2o"])
                V(lambda e: e.tensor_scalar(out=w2[:, 10, :], in0=sml[:, 1, :], scalar1=MAGIC, scalar2=None, op0=ALU.add), KL2, ["L2w"])
                V(lambda e: e.tensor_scalar(out=w2[:, 10, :], in0=w2[:, 10, :], scalar1=-MAGIC, scalar2=None, op0=ALU.add), KL2, ["L2w"])
                V(lambda e: e.tensor_tensor(out=sml[:, 1, :], in0=sml[:, 1, :], in1=w2[:, 10, :], op=ALU.subtract), KL2, ["L2o"])
                BB2 = T(t2, "BB2", [128, 2, 512])
                tC = T(t2, "tC", [128, 512])
                tD = T(t2, "tD", [128, 512])
                cf_re = w2[:, 2, :].unsqueeze(2).to_broadcast([128, 32, 16])
                cf_im = w2[:, 3, :].unsqueeze(2).to_broadcast([128, 32, 16])
                v3 = lambda ap: ap.rearrange("p (q h) -> p q h", h=16)
                cmul(v3(BB2[:, 0, :]), v3(BB2[:, 1, :]), cf_re, cf_im, v3(bL2[:, 0, :]), v3(bL2[:, 1, :]),
                     v3(tC[:]), v3(tD[:]), KL2 + ["tCD"], ["BB2", "tCD"])
                BBp = T(t2, "BBp", [128, 32, 2, 32], BF16)
                for ri in range(2):
                    for g2 in range(2):
                        V(lambda e, ri=ri, g2=g2: e.tensor_scalar(
                            out=BBp[:, :, ri, g2 * 16:(g2 + 1) * 16], in0=v3(BB2[:, ri, :]),
                            scalar1=mh2[g2], scalar2=None, op0=ALU.mult), ["BB2", "cst"], ["BBp"])
                CL = T(t2, "CL", [128, 2, 3, 512])
                tE = T(t2, "tE", [128, 3, 512])
                tF = T(t2, "tF", [128, 3, 512])
                v4 = lambda ap: ap.rearrange("p k (q h) -> p k q h", h=16)
                c_re = v3(cL2[:, 0, :]).unsqueeze(1).to_broadcast([128, 3, 32, 16])
                c_im = v3(cL2[:, 1, :]).unsqueeze(1).to_broadcast([128, 3, 32, 16])
                for k3 in range(3):
                    p_re = pw2[:, 0, 3 * k3:3 * k3 + 3, :].unsqueeze(3).to_broadcast([128, 3, 32, 16])
                    p_im = pw2[:, 1, 3 * k3:3 * k3 + 3, :].unsqueeze(3).to_broadcast([128, 3, 32, 16])
                    cmul(v4(CL[:, 0]), v4(CL[:, 1]), c_re, c_im, p_re, p_im, v4(tE[:]), v4(tF[:]), KL2 + ["tEF"], ["CL", "tEF"], neg_im=True)
                    for ri in range(2):
                        for g2 in range(2):
                            V(lambda e, ri=ri, g2=g2, k3=k3: e.tensor_scalar(
                                out=Qtab[:, :, 3 * k3:3 * k3 + 3, ri, g2 * 16:(g2 + 1) * 16],
                                in0=v4(CL[:, ri]).rearrange("p k q h -> p q k h"),
                                scalar1=mh2[g2], scalar2=None, op0=ALU.mult), ["CL", "cst"], ["Qtab"])
                for gb in range(8):
                    b = nbank()
                    pk = "ps%d" % b

                    def kmm(e, gb=gb, b=b):
                        ins = None
                        for qd in range(4):
                            q = 4 * gb + qd
                            for ri in range(2):
                                ins = e.matmul(ps[b][32 * qd:32 * qd + 32, 0:256], lhsT=BBp[:, q, ri, :],
                                               rhs=Qtab[:, q, 0:8, ri, :], start=(ri == 0), stop=(ri == 1),
                                               tile_position=(0, 32 * qd))
                        return ins
                    PE(kmm, ["BBp", "Qtab"], [pk])
                    for qd in range(4):
                        V(lambda e, gb=gb, b=b, qd=qd: e.tensor_scalar(
                            out=Ktab[:, gb, :, 32 * qd:32 * qd + 32],
                            in0=ps[b][:, 0:256].rearrange("p (t c) -> p t c", c=32),
                            scalar1=mqd[qd], scalar2=None, op0=ALU.mult), [pk, "cst"], ["Ktab"])

            dump("zs", zs, [128, 8, 8, NCH], BF16, "zs")
            dump("Ptab", Ptab, [128, 8, 8, 2, 128], BF16, "Ptab")
            dump("Qtab", Qtab, [128, 32, 9, 2, 32], BF16, "Qtab")
            dump("Ktab", Ktab, [128, 8, 8, 128], BF16, "Ktab")
            dump("sml", sml, [128, 8, 32], F32, "L2o")
            S.barrier()

            with ExitStack() as a3:
                Sq = T(a3, "Sq", [128, 4, 2, NCH])
                St = T(a3, "St", [128, 4, 2, NCH])
                Xt = Sq
                Wcs = T(a3, "Wcs", [128, 2, 4, NCH + 1])
                Rt = T(a3, "Rt", [128, NCH])
                tm = T(a3, "tm", [128, 2, 4, NCH + 1])
                Xb = T(a3, "Xb", [128, 4, 2, NCH], BF16)
                yt = T(a3, "yt", [128, 1024])
                y2 = T(a3, "y2", [128, 1024])
                Fall = T(a3, "Fall", [128, 4, 64])
                Gh = T(a3, "Gh", [128, 4, 64])

                def chunk_states(blk, with_carry):
                    q0 = 4 * blk
                    for ql in range(4):
                        for ri in range(2):
                            b = nbank()
                            pk = "ps%d" % b

                            def smm(e, ql=ql, ri=ri, b=b):
                                ins = None
                                for s in range(TCH):
                                    ins = e.matmul(ps[b][:, 0:NCH], lhsT=Ptab[32 * ql:32 * ql + 32, blk, 7 - s, ri, :],
                                                   rhs=zs[32 * ql:32 * ql + 32, blk, s, :], start=(s == 0), stop=(s == TCH - 1),
                                                   tile_position=(32 * ql, 0))
                                return ins
                            PE(smm, ["Ptab", "zs"], [pk])
                            if ri == 0:
                                A(lambda e, ql=ql, ri=ri, b=b: e.activation(out=Sq[:, ql, ri, :], in_=ps[b][:, 0:NCH], func=AF.Copy), [pk], ["Sq"])
                            else:
                                V(lambda e, ql=ql, ri=ri, b=b: e.tensor_copy(out=Sq[:, ql, ri, :], in_=ps[b][:, 0:NCH]), [pk], ["Sq"])
                    if with_carry:
                        V(lambda e: e.tensor_tensor(out=Sq[:, :, :, 1], in0=Sq[:, :, :, 1], in1=Xc[:, q0:q0 + 4, :], op=ALU.add), ["Sq", "Xc"], ["Sq"])
                    fr = sml[:, 1, q0:q0 + 4].unsqueeze(2).to_broadcast([128, 4, NCH + 1])
                    io = iota_c.unsqueeze(1).to_broadcast([128, 4, NCH + 1])
                    V(lambda e: e.tensor_tensor(out=tm[:, 0], in0=fr, in1=io, op=ALU.mult), ["sml", "cst", "tm"], ["tm"])
                    for (j, off) in ((1, 0.0), (0, 0.25)):
                        V(lambda e, off=off: e.tensor_scalar(out=tm[:, 1], in0=tm[:, 0], scalar1=off, scalar2=MAGIC, op0=ALU.add, op1=ALU.add), ["tm"], ["tm"])
                        V(lambda e: e.tensor_scalar(out=tm[:, 1], in0=tm[:, 1], scalar1=-MAGIC, scalar2=None, op0=ALU.add), ["tm"], ["tm"])
                        V(lambda e, off=off: e.scalar_tensor_tensor(out=tm[:, 1], in0=tm[:, 0], scalar=off, in1=tm[:, 1], op0=ALU.add, op1=ALU.subtract), ["tm"], ["tm"])
                        A(lambda e, j=j: e.activation(out=Wcs[:, j], in_=tm[:, 1], func=AF.Sin, scale=TWO_PI), ["tm"], ["Wcs"])
                    cw = Wcs[:, 0, :, 1:NCH + 1]
                    sw = Wcs[:, 1, :, 1:NCH + 1]
                    t_a = tm[:, 0, :, 0:NCH]
                    t_b = tm[:, 1, :, 0:NCH]
                    kk = ["Sq", "Wcs", "tm"]
                    V(lambda e: e.tensor_tensor(out=t_a, in0=Sq[:, :, 0, :], in1=cw, op=ALU.mult), kk, ["tm"])
                    V(lambda e: e.tensor_tensor(out=t_b, in0=Sq[:, :, 1, :], in1=sw, op=ALU.mult), kk, ["tm"])
                    V(lambda e: e.tensor_tensor(out=St[:, :, 0, :], in0=t_a, in1=t_b, op=ALU.add), ["tm"], ["St"])
                    V(lambda e: e.tensor_tensor(out=t_a, in0=Sq[:, :, 1, :], in1=cw, op=ALU.mult), kk, ["tm"])
                    V(lambda e: e.tensor_tensor(out=t_b, in0=Sq[:, :, 0, :], in1=sw, op=ALU.mult), kk, ["tm"])
                    V(lambda e: e.tensor_tensor(out=St[:, :, 1, :], in0=t_a, in1=t_b, op=ALU.subtract), ["tm"], ["St"])
                    for ql in range(4):
                        V(lambda e, ql=ql: e.tensor_copy(out=Rt[:], in_=sml[:, 0, q0 + ql:q0 + ql + 1].to_broadcast([128, NCH])), ["sml", "Rt"], ["Rt"])
                        for ri in range(2):
                            V(lambda e, ri=ri, ql=ql: e.tensor_tensor_scan(out=Xt[:, ql, ri, :], data0=Rt[:], data1=St[:, ql, ri, :],
                                                                           initial=0.0, op0=ALU.mult, op1=ALU.add), ["Rt", "St", "Sq"], ["Sq"])

                for blk in range(8):
                    chunk_states(blk, False)
                    q0 = 4 * blk
                    c258 = Wcs[:, 0, :, NCH]
                    s258 = Wcs[:, 1, :, NCH]
                    xr = Xt[:, :, 0, NCH - 1]
                    xi = Xt[:, :, 1, NCH - 1]
                    ta = tm[:, 0, :, 0]
                    tb = tm[:, 1, :, 0]
                    kk = ["Sq", "Wcs", "tm"]
                    V(lambda e, xr=xr, c258=c258, ta=ta: e.tensor_tensor(out=ta, in0=xr, in1=c258, op=ALU.mult), kk, ["tm"])
                    V(lambda e, xi=xi, s258=s258, tb=tb: e.tensor_tensor(out=tb, in0=xi, in1=s258, op=ALU.mult), kk, ["tm"])
                    V(lambda e, q0=q0, ta=ta, tb=tb: e.tensor_tensor(out=Fst[:, q0:q0 + 4, 0], in0=ta, in1=tb, op=ALU.subtract), ["tm"], ["Fst"])
                    V(lambda e, xr=xr, s258=s258, ta=ta: e.tensor_tensor(out=ta, in0=xr, in1=s258, op=ALU.mult), kk, ["tm"])
                    V(lambda e, xi=xi, c258=c258, tb=tb: e.tensor_tensor(out=tb, in0=xi, in1=c258, op=ALU.mult), kk, ["tm"])
                    V(lambda e, q0=q0, ta=ta, tb=tb: e.tensor_tensor(out=Fst[:, q0:q0 + 4, 1], in0=ta, in1=tb, op=ALU.add), ["tm"], ["Fst"])
                DM(lambda e: e.dma_start(out=cc_in, in_=Fst[:].rearrange("p q r -> p (q r)")), ["Fst"], ["cc_in"])
                S.collective(lambda e: e.collective_compute("AllGather", ALU.bypass, replica_groups=[[0, 1, 2, 3], [4, 5, 6, 7]],
                                                            ins=[cc_in], outs=[cc_out]), ["cc_in"], ["cc_out"])
                DM(lambda e: e.dma_start(out=Fall[:], in_=cc_out.rearrange("(j p) f -> p j f", p=128)), ["cc_out"], ["Fall"])
                for p_ in range(3):
                    V(lambda e, p_=p_: e.tensor_scalar(out=Gh[:, p_, :], in0=Fall[:, 0, :], scalar1=cm[:, p_:p_ + 1], scalar2=None, op0=ALU.mult), ["Fall", "cm"], ["Gh"])
                    for j in range(1, 4):
                        V(lambda e, p_=p_, j=j: e.scalar_tensor_tensor(out=Gh[:, p_, :], in0=Fall[:, j, :], scalar=cm[:, 3 * j + p_:3 * j + p_ + 1],
                                                                       in1=Gh[:, p_, :], op0=ALU.mult, op1=ALU.add), ["Fall", "cm", "Gh"], ["Gh"])
                gv = lambda p_, ri: Gh[:, p_, :].rearrange("p (q r) -> p q r", r=2)[:, :, ri]
                lre = sml[:, 2, :]
                lim = sml[:, 3, :]
                ta = tm[:, 0, 0, 0:32]
                tb = tm[:, 1, 0, 0:32]
                acc_re = Gh[:, 3, 0:32]
                acc_im = Gh[:, 3, 32:64]
                kk = ["Gh", "sml", "tm"]

                def horner(src_re, src_im, add_p, o_re, o_im):
                    V(lambda e: e.tensor_tensor(out=ta, in0=src_re, in1=lre, op=ALU.mult), kk, ["tm"])
                    V(lambda e: e.tensor_tensor(out=tb, in0=src_im, in1=lim, op=ALU.mult), kk, ["tm"])
                    V(lambda e: e.tensor_tensor(out=ta, in0=ta, in1=tb, op=ALU.subtract), ["tm"], ["tm"])
                    V(lambda e: e.tensor_tensor(out=tb, in0=src_re, in1=lim, op=ALU.mult), kk, ["tm"])
                    V(lambda e: e.tensor_tensor(out=o_re, in0=ta, in1=gv(add_p, 0), op=ALU.add), kk, ["Gh", "Xc"])
                    V(lambda e: e.tensor_tensor(out=ta, in0=src_im, in1=lre, op=ALU.mult), kk, ["tm"])
                    V(lambda e: e.tensor_tensor(out=tb, in0=tb, in1=ta, op=ALU.add), ["tm"], ["tm"])
                    V(lambda e: e.tensor_tensor(out=o_im, in0=tb, in1=gv(add_p, 1), op=ALU.add), kk, ["Gh", "Xc"])
                horner(gv(2, 0), gv(2, 1), 1, acc_re, acc_im)
                horner(acc_re, acc_im, 0, Xc[:, :, 0], Xc[:, :, 1])
                dump("Fst", Fst, [128, 32, 2], F32, "Fst")
                dump("Xc", Xc, [128, 32, 2], F32, "Xc")

                for blk in range(8):
                    chunk_states(blk, True)
                    cw = Wcs[:, 0, :, 1:NCH]
                    sw = Wcs[:, 1, :, 1:NCH]
                    xr = Xt[:, :, 0, 0:NCH - 1]
                    xi = Xt[:, :, 1, 0:NCH - 1]
                    t_a = tm[:, 0, :, 0:NCH - 1]
                    t_b = tm[:, 1, :, 0:NCH - 1]
                    kk = ["Sq", "Wcs", "tm"]
                    V(lambda e, xr=xr, cw=cw, t_a=t_a: e.tensor_tensor(out=t_a, in0=xr, in1=cw, op=ALU.mult), kk, ["tm"])
                    V(lambda e, xi=xi, sw=sw, t_b=t_b: e.tensor_tensor(out=t_b, in0=xi, in1=sw, op=ALU.mult), kk, ["tm"])
                    V(lambda e, t_a=t_a, t_b=t_b: e.tensor_tensor(out=Xb[:, :, 0, 1:NCH], in0=t_a, in1=t_b, op=ALU.subtract), ["tm"], ["Xb"])
                    V(lambda e, xr=xr, sw=sw, t_a=t_a: e.tensor_tensor(out=t_a, in0=xr, in1=sw, op=ALU.mult), kk, ["tm"])
                    V(lambda e, xi=xi, cw=cw, t_b=t_b: e.tensor_tensor(out=t_b, in0=xi, in1=cw, op=ALU.mult), kk, ["tm"])
                    V(lambda e, t_a=t_a, t_b=t_b: e.tensor_tensor(out=Xb[:, :, 1, 1:NCH], in0=t_a, in1=t_b, op=ALU.add), ["tm"], ["Xb"])
                    banks = [nbank() for _ in range(4)]
                    pks = ["ps%d" % b for b in banks]

                    def omm(e, blk=blk, banks=banks):
                        ins = None
                        for j in range(4):
                            pb = ps[banks[j]]
                            first = True
                            for tau in range(0, 2 * j + 2):
                                s_lo = max(2 * j, tau)
                                ns = 2 * j + 2 - s_lo
                                o0 = (s_lo - 2 * j) * 256
                                ins = e.matmul(pb[:, o0:o0 + ns * 256], lhsT=Ktab[:, blk, tau, :],
                                               rhs=zs[:, blk, s_lo - tau:s_lo - tau + ns, 2:NCH],
                                               start=first, stop=False)
                                first = False
                            for sl in range(2):
                                s = 2 * j + sl
                                for ql in range(4):
                                    for ri in range(2):
                                        ins = e.matmul(pb[32 * ql:32 * ql + 32, sl * 256:(sl + 1) * 256],
                                                       lhsT=Qtab[:, 4 * blk + ql, s + 1, ri, :], rhs=Xb[:, ql, ri, 2:NCH],
                                                       start=False, stop=(sl == 1 and ql == 3 and ri == 1),
                                                       tile_position=(0, 32 * ql))
                        return ins
                    PE(omm, ["Ktab", "zs", "Qtab", "Xb"], pks)
                    for hf in range(2):
                        for jj in range(2):
                            j = 2 * hf + jj
                            V(lambda e, j=j, jj=jj, blk=blk, banks=banks: e.scalar_tensor_tensor(
                                out=yt[:, jj * 512:(jj + 1) * 512].rearrange("p (s c) -> p s c", s=2),
                                in0=zs[:, blk, 2 * j:2 * j + 2, 2:NCH], scalar=cols[:, 0, blk:blk + 1],
                                in1=ps[banks[j]][:, 0:512].rearrange("p (s c) -> p s c", s=2), op0=ALU.mult, op1=ALU.add),
                              ["zs", "cols", pks[j]], ["yt"])
                        A(lambda e: e.activation(out=y2[:], in_=yt[:], func=AF.Square), ["yt"], ["y2"])
                        V(lambda e: e.tensor_scalar(out=y2[:], in0=y2[:], scalar1=0.044715, scalar2=1.0, op0=ALU.mult, op1=ALU.add), ["y2"], ["y2"])
                        V(lambda e: e.tensor_tensor(out=y2[:], in0=y2[:], in1=yt[:], op=ALU.mult), ["y2", "yt"], ["y2"])
                        A(lambda e: e.activation(out=y2[:], in_=y2[:], func=AF.Sigmoid, scale=1.5957691216057308), ["y2"], ["y2"])
                        V(lambda e, blk=blk, hf=hf: e.tensor_tensor(
                            out=sT[:, blk, :].rearrange("p (c s) -> p s c", s=TCH)[:, 4 * hf:4 * hf + 4, :],
                            in0=yt[:].rearrange("p (s c) -> p s c", s=4),
                            in1=y2[:].rearrange("p (s c) -> p s c", s=4), op=ALU.mult), ["yt", "y2"], ["sT"])
            dump("sT", sT, [128, 8, NT], BF16, "sT")
            DM(lambda e: e.dma_start(out=sTd, in_=sT[:].rearrange("p a t -> p (a t)")), ["sT"], ["sTd"])
            S.barrier()
            ssm_stack.close()


        w_in_v = w_in.rearrange("(k p) n -> p k n", p=128)
        pbk = ExitStack()
        gain = T(pbk, "gain", [128, D])
        XH = T(pbk, "XH", [128, 4, D])
        ubs = [T(pbk, "vb%d" % i, [128, D], BF16) for i in range(2)]
        sqs = [T(pbk, "sqb%d" % i, [128, 4]) for i in range(2)]
        uT = XH[:, 2:4, :].rearrange("p a b -> p (a b)").bitcast(BF16).rearrange("p (k t) -> p k t", t=512)
        UTK = ["xh2", "xh3"]
        zp = T(pbk, "zp", [128, 8, NH + 512], BF16)
        pa = T(pbk, "pa", [128, NH + 512])
        pb_ = T(pbk, "pb", [128, NH + 512])
        dT = T(pbk, "dT", [128, 8, 512], BF16)
        s2T = T(pbk, "s2T", [128, 8, 512], BF16)
        mgT = T(pbk, "mgT", [128, KC, 512], BF16)
        NRING = 8
        sTb = T(pbk, "sTb", [128, 8, 512], BF16)
        ring = [T(pbk, "ring%d" % i, [128, 2048]) for i in range(NRING)]
        ring_i = [0]
        sg = [T(pbk, "sg%d" % i, [128, 512]) for i in range(3)]
        vT32 = T(pbk, "vT32", [128, KC, 128])
        wr = T(pbk, "wr", [128, KC, 72])
        br = T(pbk, "br", [128, 72])
        lg = T(pbk, "lg", [128, 72])
        rt = T(pbk, "rt", [128, 12, 64])
        rs = T(pbk, "rs", [128, 16])
        base = T(pbk, "base", [128, 64])
        mb = T(pbk, "mb", [128, 64], BF16)
        DM(lambda e: e.dma_start(out=wr[:], in_=wr_d), [], ["wr"])
        DM(lambda e: e.dma_start(out=br[:], in_=br_d), [], ["br"])
        V(lambda e: e.memset(base[:], 0.0), [], ["base"])
        V(lambda e: e.memset(ubs[0][:], 0.0), [], ["vb0"])
        for r in range((NE * CAP + 128) // 128):
            DM(lambda e, r=r: e.dma_start(out=xg[r * 128:(r + 1) * 128, :], in_=ubs[0][:]), ["vb0"], ["xg"])

        def ring_load(src_ap, shape_view):
            i = ring_i[0] % NRING
            ring_i[0] += 1
            key = "ring%d" % i
            dst = ring[i]
            DM(lambda e: e.dma_start(out=shape_view(dst), in_=src_ap), [], [key])
            return dst, key

        def w_piece(w_v, kc0, nkc, c0, ncols):
            dst, key = ring_load(w_v[:, kc0:kc0 + nkc, c0:c0 + ncols],
                                 lambda d: d[:, 0:nkc * ncols].rearrange("p (k n) -> p k n", n=ncols))
            return (lambda kl, c, n: hi(dst[:, kl * ncols + c:kl * ncols + c + n])), key

        nt_i = [0]

        def ntile():
            i = nt_i[0] % 2
            nt_i[0] += 1
            return (XH[:, i, :], ubs[i], sqs[i], "xh%d" % i, "vb%d" % i)

        pool_w_v = pool_w.rearrange("g (k p) n -> p g k n", p=128)
        w_glu_v = w_glu.rearrange("(k p) n -> p k n", p=128)
        w_bp_v = w_bp.rearrange("(k p) n -> p k n", p=128)
        w_bs_v = w_bs.rearrange("(k p) n -> p k n", p=128)
        w_out_v = w_out.rearrange("(k p) n -> p k n", p=128)
        ev_i = [0]

        def evac_copy(dst, src, rk, wk):
            ev_i[0] += 1
            if ev_i[0] % 2 == 0:
                A(lambda e: e.activation(out=dst, in_=src, func=AF.Copy), rk, wk)
            else:
                V(lambda e: e.tensor_copy(out=dst, in_=src), rk, wk)

        def zpool_cols(ncol, col_off, uT_ap, utk):
            for mp in range(4):
                getters = [w_piece(w_in_v, 8 * hk, 8, 256 * mp, 256) for hk in range(2)]
                for mm in range(2):
                    m = 2 * mp + mm
                    b = nbank()
                    pk = "ps%d" % b

                    def zmm(e, mm=mm, b=b):
                        ins = None
                        for kc in range(KC):
                            g_, _ = getters[kc // 8]
                            ins = e.matmul(ps[b][:, 0:ncol], lhsT=g_(kc % 8, mm * 128, 128), rhs=uT_ap[:, kc, 0:ncol],
                                           start=(kc == 0), stop=(kc == KC - 1))
                        return ins
                    PE(zmm, [getters[0][1], getters[1][1]] + utk, [pk])
                    evac_copy(zp[:, m, col_off:col_off + ncol], ps[b][:, 0:ncol], [pk], ["zp"])

        for tb in range(NBLK):
            tok0 = 512 * tb
            DM(lambda e, tok0=tok0: e.dma_start(out=sTb[:], in_=sTd.rearrange("p (a t) -> p a t", a=8)[:, :, tok0:tok0 + 512]), ["sTd"], ["sTb"])
            DM(lambda e: e.dma_start(out=gain[:], in_=gains_d[:, 0, :]), [], ["gains"])
            if tb == 0:
                load_norm_T(ntile(), 0, NH, gain, uT, 0, UTK)
                zpool_cols(NH, 0, uT, UTK)
            else:
                G(lambda e: e.tensor_copy(out=zp[:, :, 0:NH], in_=zp[:, :, 512:512 + NH]), ["zp"], ["zp"])
            for tt in range(4):
                load_norm_T(ntile(), NH + tok0 + 128 * tt, 128, gain, uT, 128 * tt, UTK)
            zpool_cols(512, NH, uT, UTK)
            for m in range(8):
                steps = m // 2 + 1
                srcs = [zp[:, m, :], pa[:], pb_[:], pa[:], pb_[:]]
                keys = ["zp", "pa", "pb", "pa", "pb"]
                for sidx in range(steps):
                    sh = 1 << sidx
                    lo = 2 * sh - 1
                    G(lambda e, sidx=sidx, sh=sh, lo=lo, srcs=srcs: e.tensor_tensor(
                        out=srcs[sidx + 1][:, lo:NH + 512], in0=srcs[sidx][:, lo:NH + 512], in1=srcs[sidx][:, lo - sh:NH + 512 - sh],
                        op=ALU.add), [keys[sidx]], [keys[sidx + 1]])
                V(lambda e, m=m, steps=steps, srcs=srcs: e.scalar_tensor_tensor(
                    out=dT[:, m, :], in0=srcs[steps][:, NH:NH + 512], scalar=1.0 / (1 << steps), in1=zp[:, m, NH:NH + 512],
                    op0=ALU.mult, op1=ALU.subtract), [keys[steps], "zp"], ["dT"])
            for gi in range(4):
                dst, key = ring_load(pool_w_v[:, gi, :, :], lambda d: d[:, 0:512].rearrange("p (k n) -> p k n", n=256))
                bks = [nbank(), nbank()]

                def pmm(e, gi=gi, dst=dst, bks=bks):
                    ins = None
                    for mo in range(2):
                        for kc in range(2):
                            ins = e.matmul(ps[bks[mo]][:, 0:512], lhsT=hi(dst[:, kc * 256 + mo * 128:kc * 256 + mo * 128 + 128]),
                                           rhs=dT[:, 2 * gi + kc, :], start=(kc == 0), stop=(kc == 1))
                    return ins
                PE(pmm, [key, "dT"], ["ps%d" % bks[0], "ps%d" % bks[1]])
                for mo in range(2):
                    m = 2 * gi + mo
                    A(lambda e, m=m, mo=mo, bks=bks: e.activation(out=dT[:, m, :], in_=ps[bks[mo]][:, 0:512], func=AF.Copy,
                                                                   scale=cols[:, 1, m:m + 1]), ["ps%d" % bks[mo], "cols"], ["dT"])
            for mp in range(4):
                g_, key = w_piece(w_glu_v, 0, 8, 256 * mp, 256)
                for mm in range(2):
                    mo = 2 * mp + mm
                    b = nbank()
                    pk = "ps%d" % b

                    def gmm(e, mm=mm, b=b, g_=g_):
                        ins = None
                        for kc in range(8):
                            ins = e.matmul(ps[b][:, 0:512], lhsT=g_(kc, mm * 128, 128), rhs=sTb[:, kc, :],
                                           start=(kc == 0), stop=(kc == 7))
                        return ins
                    PE(gmm, [key, "sTb"], [pk])
                    A(lambda e, mo=mo, b=b: e.activation(out=sg[0][:], in_=ps[b][:, 0:512], func=AF.Sigmoid,
                                                         bias=cols[:, 2, mo:mo + 1], scale=1.0), [pk, "cols"], ["sg0"])
                    V(lambda e, mo=mo: e.tensor_tensor(out=s2T[:, mo, :], in0=sTb[:, mo, :], in1=sg[0][:], op=ALU.mult),
                      ["sg0", "sTb"], ["s2T"])
            for jp in range(8):
                bk = [nbank() for _ in range(8)]
                pks = ["ps%d" % b for b in bk]
                gbp, kbp = w_piece(w_bp_v, 0, 8, 256 * jp, 256)

                def m_yp(e, bk=bk, gbp=gbp):
                    ins = None
                    for jj in range(2):
                        for kc in range(8):
                            ins = e.matmul(ps[bk[jj]][:, 0:512], lhsT=gbp(kc, jj * 128, 128), rhs=dT[:, kc, :], start=(kc == 0), stop=(kc == 7))
                    return ins
                PE(m_yp, [kbp, "dT"], pks[0:2])
                gbs, kbs = w_piece(w_bs_v, 0, 8, 256 * jp, 256)

                def m_ys(e, bk=bk, gbs=gbs):
                    ins = None
                    for jj in range(2):
                        for kc in range(8):
                            ins = e.matmul(ps[bk[2 + jj]][:, 0:512], lhsT=gbs(kc, jj * 128, 128), rhs=s2T[:, kc, :], start=(kc == 0), stop=(kc == 7))
                    return ins
                PE(m_ys, [kbs, "s2T"], pks[2:4])
                for gsel in range(2):
                    gg = [w_piece(w_in_v, 8 * hk, 8, 2048 * (gsel + 1) + 256 * jp, 256) for hk in range(2)]

                    def m_g(e, bk=bk, gg=gg, gsel=gsel):
                        ins = None
                        for jj in range(2):
                            for kc in range(KC):
                                ins = e.matmul(ps[bk[4 + 2 * gsel + jj]][:, 0:512], lhsT=gg[kc // 8][0](kc % 8, jj * 128, 128), rhs=uT[:, kc, :],
                                               start=(kc == 0), stop=(kc == KC - 1))
                        return ins
                    PE(m_g, [gg[0][1], gg[1][1]] + UTK, pks[4 + 2 * gsel:6 + 2 * gsel])
                for jj in range(2):
                    j = 2 * jp + jj
                    A(lambda e, bk=bk, jj=jj: e.activation(out=sg[0][:], in_=ps[bk[4 + jj]][:, 0:512], func=AF.Sigmoid), [pks[4 + jj]], ["sg0"])
                    A(lambda e, bk=bk, jj=jj: e.activation(out=sg[1][:], in_=ps[bk[6 + jj]][:, 0:512], func=AF.Sigmoid), [pks[6 + jj]], ["sg1"])
                    V(lambda e, bk=bk, jj=jj: e.tensor_tensor(out=sg[0][:], in0=sg[0][:], in1=ps[bk[jj]][:, 0:512], op=ALU.mult), ["sg0", pks[jj]], ["sg0"])
                    V(lambda e, bk=bk, jj=jj: e.tensor_tensor(out=sg[1][:], in0=sg[1][:], in1=ps[bk[2 + jj]][:, 0:512], op=ALU.mult), ["sg1", pks[2 + jj]], ["sg1"])
                    G(lambda e, j=j: e.tensor_tensor(out=mgT[:, j, :], in0=sg[0][:], in1=sg[1][:], op=ALU.add), ["sg0", "sg1"], ["mgT"])
            if tb == 0:
                dump("zp", zp, [128, 8, NH + 512], BF16, "zp")
                dump("dT", dT, [128, 8, 512], BF16, "dT")
                dump("s2T", s2T, [128, 8, 512], BF16, "s2T")
                dump("mgT", mgT, [128, KC, 512], BF16, "mgT")
            for tt in range(4):
                DM(lambda e, tt=tt: e.dma_start(out=XH[:, tt, :], in_=xs[NH + tok0 + 128 * tt:NH + tok0 + 128 * (tt + 1), :]),
                   [], ["xh%d" % tt])
            DM(lambda e: e.dma_start(out=gain[:], in_=gains_d[:, 1, :]), [], ["gains"])
            for nb in range(4):
                bk = [nbank() for _ in range(4)]
                pks = ["ps%d" % b for b in bk]
                pieces = [w_piece(w_out_v, 4 * kq, 4, 512 * nb, 512) for kq in range(4)]

                def omm2(e, bk=bk, pieces=pieces):
                    ins = None
                    for kq in range(4):
                        for tt in range(4):
                            for kl in range(4):
                                kc = 4 * kq + kl
                                ins = e.matmul(ps[bk[tt]][:, 0:512], lhsT=mgT[:, kc, 128 * tt:128 * (tt + 1)],
                                               rhs=pieces[kq][0](kl, 0, 512), start=(kc == 0), stop=(kc == KC - 1))
                    return ins
                PE(omm2, [p_[1] for p_ in pieces] + ["mgT"], pks)
                for tt in range(4):
                    V(lambda e, tt=tt, nb=nb, bk=bk: e.tensor_tensor(out=XH[:, tt, 512 * nb:512 * (nb + 1)], in0=XH[:, tt, 512 * nb:512 * (nb + 1)],
                                                                     in1=ps[bk[tt]][:, 0:512], op=ALU.add), [pks[tt], "xh%d" % tt], ["xh%d" % tt])
            for tt in range(4):
                ti = 4 * tb + tt
                hk = "xh%d" % tt
                ht = XH[:, tt, :]
                vb = ubs[tt % 2]
                vk = "vb%d" % (tt % 2)
                sq = sqs[tt % 2]
                sk = "sqb%d" % (tt % 2)
                DM(lambda e, ti=ti, ht=ht: e.dma_start(out=hs[128 * ti:128 * (ti + 1), :], in_=ht), [hk], ["hs"])
                A(lambda e, ht=ht, vb=vb, sq=sq: e.activation(out=vb[:], in_=ht, func=AF.Square, accum_out=sq[:, 0:1]), [hk], [vk, sk])
                V(lambda e, sq=sq: e.tensor_scalar(out=sq[:, 1:2], in0=sq[:, 0:1], scalar1=1.0 / D, scalar2=EPS, op0=ALU.mult, op1=ALU.add), [sk], [sk])
                A(lambda e, sq=sq: e.activation(out=sq[:, 1:2], in_=sq[:, 1:2], func=AF.Sqrt), [sk], [sk])
                V(lambda e, sq=sq: e.reciprocal(out=sq[:, 2:3], in_=sq[:, 1:2]), [sk], [sk])
                V(lambda e, ht=ht, sq=sq: e.scalar_tensor_tensor(out=ht, in0=ht, scalar=sq[:, 2:3], in1=gain[:], op0=ALU.mult, op1=ALU.mult),
                  [hk, sk, "gains"], [hk])
                G(lambda e, ht=ht, vb=vb: e.tensor_copy(out=vb[:], in_=ht), [hk], [vk])
                for qd in range(4):
                    b = nbank()
                    pk = "ps%d" % b

                    def vtr(e, qd=qd, b=b, ht=ht):
                        ins = None
                        for j in range(4):
                            kc = 4 * qd + j
                            ins = e.transpose(out=ps[b][:, j * 128:(j + 1) * 128], in_=ht[:, kc * 128:(kc + 1) * 128], identity=ident32)
                        return ins
                    PE(vtr, [hk, "cst"], [pk])
                    evac_copy(vT32[:, 4 * qd:4 * qd + 4, :], ps[b][:, 0:512].rearrange("p (k t) -> p k t", t=128), [pk], ["vT32"])
                b = nbank()
                pk = "ps%d" % b

                def rmm(e, b=b):
                    ins = None
                    for kc in range(KC):
                        ins = e.matmul(ps[b][:, 0:72], lhsT=vT32[:, kc, :], rhs=wr[:, kc, :], start=(kc == 0), stop=(kc == KC - 1))
                    return ins
                PE(rmm, ["vT32", "wr"], [pk])
                V(lambda e, b=b: e.tensor_tensor(out=lg[:], in0=ps[b][:, 0:72], in1=br[:], op=ALU.add), [pk, "br"], ["lg"])
                R = lambda i: rt[:, i, :]
                K_ = ["rt", "rs", "lg"]
                W_ = ["rt", "rs"]
                V(lambda e: e.reduce_max(out=rs[:, 0:1], in_=lg[:, 0:8], axis=AX.X), K_, W_)
                V(lambda e: e.tensor_scalar(out=R(0)[:, 0:8], in0=lg[:, 0:8], scalar1=rs[:, 0:1], scalar2=None, op0=ALU.is_equal), K_, W_)
                V(lambda e: e.tensor_scalar(out=rs[:, 1:2], in0=rs[:, 0:1], scalar1=-1.0, scalar2=None, op0=ALU.mult), K_, W_)
                A(lambda e: e.activation(out=R(1)[:, 0:8], in_=lg[:, 0:8], func=AF.Exp, bias=rs[:, 1:2], scale=1.0, accum_out=rs[:, 2:3]), K_, W_)
                V(lambda e: e.reciprocal(out=rs[:, 3:4], in_=rs[:, 2:3]), K_, W_)
                V(lambda e: e.tensor_scalar(out=R(1)[:, 0:8], in0=R(0)[:, 0:8], scalar1=1e30, scalar2=-1e30, op0=ALU.mult, op1=ALU.add), K_, W_)
                V(lambda e: e.tensor_tensor(out=R(2).rearrange("p (g x) -> p g x", x=8), in0=lg[:, 8:72].rearrange("p (g x) -> p g x", x=8),
                                            in1=R(1)[:, 0:8].unsqueeze(2).to_broadcast([128, 8, 8]), op=ALU.add), K_, W_)
                V(lambda e: e.reduce_max(out=rs[:, 4:5], in_=R(2), axis=AX.X), K_, W_)
                V(lambda e: e.tensor_scalar(out=R(3), in0=R(2), scalar1=rs[:, 4:5], scalar2=None, op0=ALU.is_equal), K_, W_)
                V(lambda e: e.scalar_tensor_tensor(out=R(4), in0=R(3), scalar=-1e30, in1=R(2), op0=ALU.mult, op1=ALU.add), K_, W_)
                V(lambda e: e.reduce_max(out=rs[:, 5:6], in_=R(4), axis=AX.X), K_, W_)
                V(lambda e: e.tensor_scalar(out=R(5), in0=R(4), scalar1=rs[:, 5:6], scalar2=None, op0=ALU.is_equal), K_, W_)
                V(lambda e: e.tensor_tensor(out=rs[:, 6:7], in0=rs[:, 5:6], in1=rs[:, 4:5], op=ALU.subtract), K_, W_)
                A(lambda e: e.activation(out=rs[:, 6:7], in_=rs[:, 6:7], func=AF.Exp), K_, W_)
                V(lambda e: e.tensor_scalar(out=rs[:, 6:7], in0=rs[:, 6:7], scalar1=1.0, scalar2=None, op0=ALU.add), K_, W_)
                V(lambda e: e.reciprocal(out=rs[:, 7:8], in_=rs[:, 6:7]), K_, W_)
                V(lambda e, ti=ti: e.tensor_tensor(out=gw[:, ti, 0:1], in0=rs[:, 7:8], in1=rs[:, 3:4], op=ALU.mult), K_, W_ + ["gw"])
                V(lambda e, ti=ti: e.tensor_tensor(out=gw[:, ti, 1:2], in0=rs[:, 3:4], in1=gw[:, ti, 0:1], op=ALU.subtract), K_ + ["gw"], W_ + ["gw"])
                V(lambda e: e.tensor_tensor(out=mb[:], in0=R(3), in1=R(5), op=ALU.add), K_, ["mb"])
                b = nbank()
                pk = "ps%d" % b

                def cmm(e, b=b):
                    e.matmul(ps[b][:, 0:64], lhsT=trib, rhs=mb[:], start=True, stop=True)
                    return e.matmul(ps[b][:, 64:128], lhsT=onesb, rhs=mb[:], start=True, stop=True)
                PE(cmm, ["mb", "cstb"], [pk])
                V(lambda e, b=b: e.tensor_tensor(out=R(6), in0=ps[b][:, 0:64], in1=base[:], op=ALU.add), [pk, "base"] + K_, W_)
                V(lambda e, b=b: e.tensor_tensor(out=base[:], in0=base[:], in1=ps[b][:, 64:128], op=ALU.add), [pk, "base"] + K_, ["base"])
                for k_ in range(2):
                    oh = R(3) if k_ == 0 else R(5)
                    V(lambda e, oh=oh: e.tensor_tensor(out=R(7), in0=oh, in1=R(6), op=ALU.mult), K_, W_)
                    V(lambda e: e.reduce_sum(out=rs[:, 8:9], in_=R(7), axis=AX.X), K_, W_)
                    V(lambda e: e.tensor_scalar(out=rs[:, 8:9], in0=rs[:, 8:9], scalar1=float(CAP - 1), scalar2=None, op0=ALU.min), K_, W_)
                    V(lambda e, oh=oh: e.tensor_tensor(out=R(7), in0=oh, in1=iota_e, op=ALU.mult), K_ + ["cst"], W_)
                    V(lambda e: e.reduce_sum(out=rs[:, 9:10], in_=R(7), axis=AX.X), K_, W_)
                    V(lambda e: e.scalar_tensor_tensor(out=rs[:, 10:11], in0=rs[:, 9:10], scalar=float(CAP), in1=rs[:, 8:9],
                                                       op0=ALU.mult, op1=ALU.add), K_, W_)
                    V(lambda e, ti=ti, k_=k_: e.tensor_copy(out=gidx[:, ti, k_:k_ + 1], in_=rs[:, 10:11]), K_, ["gi"])
                    S.dma(lambda e, ti=ti, k_=k_, vb=vb: e.indirect_dma_start(
                        out=xg, out_offset=bass.IndirectOffsetOnAxis(ap=gidx[:, ti, k_:k_ + 1], axis=0), in_=vb[:], in_offset=None),
                        ["gi", vk, "xg"], ["xg%d" % (ti * 2 + k_)], q="pool")
        dump("gw", gw, [128, 16, 2], F32, "gw")
        dump("lg", lg, [128, 72], F32, "lg")
        S.barrier()
        pbk.close()

        if stage >= 3:
            pc = ExitStack()
            NR2 = 8
            ring2 = [T(pc, "wrg%d" % i, [128, 4096]) for i in range(NR2)]
            r2_i = [0]
            Xe = [T(pc, "Xe%d" % i, [128, D], BF16) for i in range(2)]
            XeT = [T(pc, "XeT%d" % i, [128, KC, 128], BF16) for i in range(2)]
            hg = T(pc, "hg", [128, 512])
            hh = T(pc, "hh", [128, 512], BF16)
            hT = T(pc, "hT", [128, 4, 128], BF16)
            Ye = [T(pc, "Ye%d" % i, [128, D]) for i in range(2)]

            def piece2(src_ap, nk, ncols):
                i = r2_i[0] % NR2
                r2_i[0] += 1
                key = "wrg%d" % i
                dst = ring2[i]
                DM(lambda e: e.dma_start(out=dst[:, 0:nk * ncols].rearrange("p (k n) -> p k n", n=ncols), in_=src_ap), [], [key])
                return (lambda kl, c, n: hi(dst[:, kl * ncols + c:kl * ncols + c + n])), key

            for ex in range(NE):
                xe = Xe[ex % 2]
                xk = "Xe%d" % (ex % 2)
                xT = XeT[ex % 2]
                xtk = "XeT%d" % (ex % 2)
                DM(lambda e, ex=ex, xe=xe: e.dma_start(out=xe[:], in_=xg[ex * CAP:(ex + 1) * CAP, :]),
                   ["xg"] + ["xg%d" % i for i in range(32)], [xk])
                wg_v = w_gate[ex].rearrange("(k p) f -> p k f", p=128)
                wu_v = w_up[ex].rearrange("(k p) f -> p k f", p=128)
                wd_v = w_down[ex].rearrange("(k p) n -> p k n", p=128)
                pg_ = [piece2(wg_v[:, 8 * h_:8 * h_ + 8, :], 8, 512) for h_ in range(2)]
                pu_ = [piece2(wu_v[:, 8 * h_:8 * h_ + 8, :], 8, 512) for h_ in range(2)]
                pd_ = [piece2(wd_v[:, :, 1024 * h_:1024 * (h_ + 1)], 4, 1024) for h_ in range(2)]
                for half in range(2):
                    b = nbank()
                    pk = "ps%d" % b
                    psb = ps[b][:].bitcast(BF16)

                    def xtr(e, half=half, psb=psb, xe=xe):
                        ins = None
                        for j in range(8):
                            kc = half * 8 + j
                            ins = e.transpose(out=psb[:, j * 128:(j + 1) * 128], in_=xe[:, kc * 128:(kc + 1) * 128], identity=identb)
                        return ins
                    PE(xtr, [xk, "cstb"], [pk])
                    evac_copy(xT[:, half * 8:half * 8 + 8, :], psb[:, 0:1024].rearrange("p (k t) -> p k t", t=128), [pk], [xtk])
                bg, bu = nbank(), nbank()

                def gu(e, bg=bg, bu=bu, xT=xT, pg_=pg_, pu_=pu_):
                    ins = None
                    for kc in range(KC):
                        ins = e.matmul(ps[bg][:, 0:512], lhsT=xT[:, kc, :], rhs=pg_[kc // 8][0](kc % 8, 0, 512), start=(kc == 0), stop=(kc == KC - 1))
                    for kc in range(KC):
                        ins = e.matmul(ps[bu][:, 0:512], lhsT=xT[:, kc, :], rhs=pu_[kc // 8][0](kc % 8, 0, 512), start=(kc == 0), stop=(kc == KC - 1))
                    return ins
                PE(gu, [xtk] + [p_[1] for p_ in pg_ + pu_], ["ps%d" % bg, "ps%d" % bu])
                A(lambda e, bg=bg: e.activation(out=hg[:], in_=ps[bg][:, 0:512], func=AF.Silu), ["ps%d" % bg], ["hg"])
                V(lambda e, bu=bu: e.tensor_tensor(out=hh[:], in0=hg[:], in1=ps[bu][:, 0:512], op=ALU.mult), ["hg", "ps%d" % bu], ["hh"])
                b = nbank()
                pk = "ps%d" % b
                psb = ps[b][:].bitcast(BF16)

                def htr(e, psb=psb):
                    ins = None
                    for j in range(4):
                        ins = e.transpose(out=psb[:, j * 128:(j + 1) * 128], in_=hh[:, j * 128:(j + 1) * 128], identity=identb)
                    return ins
                PE(htr, ["hh", "cstb"], [pk])
                evac_copy(hT[:], psb[:, 0:512].rearrange("p (k t) -> p k t", t=128), [pk], ["hT"])
                ye = Ye[ex % 2]
                yk = "Ye%d" % (ex % 2)
                for nb in range(4):
                    b = nbank()
                    pk = "ps%d" % b

                    def dmm(e, nb=nb, b=b, pd_=pd_):
                        ins = None
                        for kc in range(4):
                            ins = e.matmul(ps[b][:, 0:512], lhsT=hT[:, kc, :], rhs=pd_[nb // 2][0](kc, (nb % 2) * 512, 512),
                                           start=(kc == 0), stop=(kc == 3))
                        return ins
                    PE(dmm, ["hT", pd_[nb // 2][1]], [pk])
                    evac_copy(ye[:, nb * 512:(nb + 1) * 512], ps[b][:, 0:512], [pk], [yk])
                DM(lambda e, ex=ex, ye=ye: e.dma_start(out=ysc[ex * CAP:(ex + 1) * CAP, :], in_=ye[:]), [yk], ["ysc"])
            S.barrier()
            pc.close()

        pd = ExitStack()
        gfin = T(pd, "gfin", [128, D])
        DM(lambda e: e.dma_start(out=gfin[:], in_=gains_d[:, 2, :]), [], ["gfin"])
        hts = [T(pd, "hD%d" % i, [128, D]) for i in range(2)]
        g1s = [T(pd, "g1_%d" % i, [128, D]) for i in range(2)]
        g2s = [T(pd, "g2_%d" % i, [128, D]) for i in range(2)]
        jnk = T(pd, "jnk", [128, D], BF16)
        sqd = [T(pd, "sqd%d" % i, [128, 4]) for i in range(2)]
        for ti in range(4 * NBLK):
            i = ti % 2
            ht, g1, g2, sq = hts[i], g1s[i], g2s[i], sqd[i]
            hk, k1, k2, sk = "hD%d" % i, "g1_%d" % i, "g2_%d" % i, "sqd%d" % i
            DM(lambda e, ti=ti, ht=ht: e.dma_start(out=ht[:], in_=hs[128 * ti:128 * (ti + 1), :]), ["hs"], [hk])
            if stage >= 3:
                S.dma(lambda e, ti=ti, g1=g1: e.indirect_dma_start(out=g1[:], out_offset=None, in_=ysc,
                      in_offset=bass.IndirectOffsetOnAxis(ap=gidx[:, ti, 0:1], axis=0)), ["ysc", "gi"], [k1], q="pool")
                S.dma(lambda e, ti=ti, g2=g2: e.indirect_dma_start(out=g2[:], out_offset=None, in_=ysc,
                      in_offset=bass.IndirectOffsetOnAxis(ap=gidx[:, ti, 1:2], axis=0)), ["ysc", "gi"], [k2], q="pool")
                V(lambda e, ti=ti, ht=ht, g1=g1: e.scalar_tensor_tensor(out=ht[:], in0=g1[:], scalar=gw[:, ti, 0:1], in1=ht[:],
                                                                        op0=ALU.mult, op1=ALU.add), [hk, k1, "gw"], [hk])
                V(lambda e, ti=ti, ht=ht, g2=g2: e.scalar_tensor_tensor(out=ht[:], in0=g2[:], scalar=gw[:, ti, 1:2], in1=ht[:],
                                                                        op0=ALU.mult, op1=ALU.add), [hk, k2, "gw"], [hk])
            A(lambda e, ht=ht, sq=sq: e.activation(out=jnk[:], in_=ht[:], func=AF.Square, accum_out=sq[:, 0:1]), [hk], ["jnk", sk])
            V(lambda e, sq=sq: e.tensor_scalar(out=sq[:, 1:2], in0=sq[:, 0:1], scalar1=1.0 / D, scalar2=EPS, op0=ALU.mult, op1=ALU.add), [sk], [sk])
            A(lambda e, sq=sq: e.activation(out=sq[:, 1:2], in_=sq[:, 1:2], func=AF.Sqrt), [sk], [sk])
            V(lambda e, sq=sq: e.reciprocal(out=sq[:, 2:3], in_=sq[:, 1:2]), [sk], [sk])
            V(lambda e, ht=ht, sq=sq: e.scalar_tensor_tensor(out=ht[:], in0=ht[:], scalar=sq[:, 2:3], in1=gfin[:], op0=ALU.mult, op1=ALU.mult),
              [hk, sk, "gfin"], [hk])
            DM(lambda e, ti=ti, ht=ht: e.dma_start(out=out_d[128 * ti:128 * (ti + 1), :], in_=ht[:]), [hk], ["out"], is_out=True)
        S.barrier()
        pd.close()
        S.emit()
    return nc


def build_rest(nc, st, S, L):
    pass


def _consts():
    c = np.zeros((128, 1024), np.float32)
    c[:, 0:128] = np.eye(128, dtype=np.float32)
    k = np.arange(128)
    c[:, 128:256] = (k[:, None] < k[None, :]).astype(np.float32)
    c[:, 256:384] = 1.0
    c[:, 384:384 + 259] = np.arange(259, dtype=np.float32)[None, :]
    c[:, 648:712] = np.arange(64, dtype=np.float32)[None, :]
    for j in range(2):
        c[:, 712 + j] = ((k // 16) % 2 == j)
        c[:, 718 + j] = (k // 64 == j)
    for j in range(4):
        c[:, 714 + j] = (k // 32 == j)
    c[:, 720] = k
    return c


def _prep(inp, stage=3):
    f = lambda a: np.ascontiguousarray(np.asarray(a, dtype=np.float32))
    x = f(inp["x"]); meta = f(inp["meta"])
    a_re = f(inp["ssm_a_re"])[0]; a_im = f(inp["ssm_a_im"])[0]; ldt = f(inp["ssm_log_dt"])[0]
    b_re = f(inp["ssm_b_re"])[0]; b_im = f(inp["ssm_b_im"])[0]
    c_re = f(inp["ssm_c_re"])[0]; c_im = f(inp["ssm_c_im"])[0]
    shared = {}
    shared["w_in"] = f(inp["w_in"])[0]
    shared["pool_w"] = f(inp["pool_w"])[0]
    shared["w_glu"] = f(inp["w_glu"])[0]
    shared["w_bp"] = f(inp["w_branch_pool"])[0]
    shared["w_bs"] = f(inp["w_branch_ssm"])[0]
    shared["w_out"] = f(inp["w_out"])[0]
    if stage >= 3:
        shared["w_gate"] = f(inp["w_gate"])[0]
        shared["w_up"] = f(inp["w_up"])[0]
        shared["w_down"] = f(inp["w_down"])[0]
    g = np.stack([f(inp["norm_mix"])[0], f(inp["norm_ffn"])[0], f(inp["norm_final"])], 0)
    shared["gains"] = np.ascontiguousarray(np.broadcast_to(g[None], (128, 3, D)))
    cols = np.stack([f(inp["ssm_d"])[0].reshape(8, 128).T, f(inp["pool_scale"])[0].reshape(8, 128).T,
                     f(inp["b_glu"])[0].reshape(8, 128).T], 1)
    shared["cols"] = np.ascontiguousarray(cols)
    wr = np.concatenate([f(inp["w_router_group"])[0], f(inp["w_router_expert"])[0]], 1)
    shared["wr"] = np.ascontiguousarray(wr.reshape(KC, 128, 72).transpose(1, 0, 2))
    br = np.concatenate([f(inp["b_router_group"])[0], f(inp["b_router_expert"])[0]], 0)
    shared["br"] = np.ascontiguousarray(np.broadcast_to(br[None], (128, 72)))
    def L1(a):
        t = a.reshape(8, 8, 64)
        t = np.broadcast_to(t[:, :, None, :], (8, 8, 16, 64))
        return t.transpose(1, 2, 0, 3).reshape(128, 8, 64)
    ldt_b = np.broadcast_to(ldt[:, None], (64, 64))
    shared["aL1"] = np.ascontiguousarray(np.stack([L1(a_re), L1(a_im), L1(ldt_b)], 1))
    def B1(b):
        t = b.reshape(8, 8, 64, 16)
        return t.transpose(1, 3, 0, 2).reshape(128, 8, 64)
    shared["bL1"] = np.ascontiguousarray(np.stack([B1(b_re), B1(b_im)], 1))
    def L2(a):
        return a.reshape(32, 2, 64).transpose(1, 2, 0).reshape(128, 32)
    shared["aL2"] = np.ascontiguousarray(np.stack([L2(a_re), L2(a_im), L2(ldt_b)], 1))
    def B2(b):
        return b.reshape(32, 2, 64, 16).transpose(1, 2, 0, 3).reshape(128, 32, 16)
    shared["bL2"] = np.ascontiguousarray(np.stack([B2(b_re), B2(b_im)], 1))
    def C2(c):
        return c.reshape(32, 2, 16, 64).transpose(1, 3, 0, 2).reshape(128, 32, 16)
    shared["cL2"] = np.ascontiguousarray(np.stack([C2(c_re), C2(c_im)], 1))
    shared["cst"] = _consts()
    maps = []
    for c in range(8):
        b, k = c // 4, c % 4
        halo = meta if k == 0 else x[b, NT * k - NH:NT * k]
        m = dict(shared)
        m["xs"] = np.ascontiguousarray(np.concatenate([halo, x[b, NT * k:NT * (k + 1)]], 0))
        m["hm"] = np.full((128, 1), 1.0 if k == 0 else 0.0, np.float32)
        cmv = np.zeros((4, 3), np.float32)
        for j in range(k):
            cmv[j, k - 1 - j] = 1.0
        m["cm"] = np.ascontiguousarray(np.broadcast_to(cmv.reshape(1, 12), (128, 12)))
        maps.append(m)
    return maps


_NC_CACHE = {}


def kernel(**inputs):
    if "nc" not in _NC_CACHE:
        _NC_CACHE["nc"] = build(False, 3)
    nc = _NC_CACHE["nc"]
    maps = _prep(inputs, 3)
    res = run_bass_kernel_spmd(nc, maps, core_ids=list(range(8)))
    out = np.empty((2, 8192, D), np.float32)
    for c in range(8):
        b, k = c // 4, c % 4
        out[b, NT * k:NT * (k + 1)] = res.results[c]["out"]
    return out
```

```python
import math
from contextlib import ExitStack

import numpy as np
import concourse.bass as bass
import concourse.mybir as mybir
from concourse.bass_utils import run_bass_kernel_spmd

F32 = mybir.dt.float32
BF16 = mybir.dt.bfloat16
I32 = mybir.dt.int32
ALU = mybir.AluOpType
AF = mybir.ActivationFunctionType
AX = mybir.AxisListType

D = 2048
KC = 16
NT = 2048
NH = 16
NTOK = NT + NH
TCH = 8
NCH = NTOK // TCH
NE = 64
CAP = 128
EPS = 1e-6
MAGIC = 12582912.0
TWO_PI = 2.0 * math.pi
DEBUG = {}


class Sched:
    ENGS = ("pe", "act", "dve", "pool", "sp")
    NDMA = 14

    def __init__(self, nc, stack):
        self.nc = nc
        self.ops = {e: [] for e in self.ENGS}
        self.cnt = {e: 0 for e in self.ENGS}
        self.sem = {e: stack.enter_context(nc.semaphore("s_" + e)) for e in self.ENGS}
        self.dsem = {
            q: [stack.enter_context(nc.semaphore(f"d_{q}{i}")) for i in range(self.NDMA)]
            for q in ("sp", "pool")
        }
        self.ccsem = stack.enter_context(nc.semaphore("cc"))
        self.dcnt = {"sp": 0, "pool": 0}
        self.known = {e: {} for e in self.ENGS}
        self.last_w = {}
        self.readers = {}
        self.out_tokens = []
        self.all_tokens = {}
        self.block = stack.enter_context(nc.Block())
        self.eobj = {"pe": nc.tensor, "act": nc.scalar, "dve": nc.vector, "pool": nc.gpsimd, "sp": nc.sync}

    def _emit(self, eng, waits, fn, sem, inc):
        e = self.eobj[eng]
        for (s_, v) in waits:
            e.wait_ge(s_, v)
        if fn is not None:
            fn(e).then_inc(sem, inc)

    def _deps(self, eng, reads, writes):
        toks = []
        for k in reads:
            w = self.last_w.get(k)
            if w is not None:
                toks.append(w)
        for k in writes:
            w = self.last_w.get(k)
            if w is not None:
                toks.append(w)
            toks.extend(self.readers.get(k, ()))
        waits = {}
        for (sem, val, src) in toks:
            if src == "pe" and eng == "pe":
                continue
            key = id(sem)
            if self.known[eng].get(key, 0) >= val:
                continue
            if key not in waits or waits[key][1] < val:
                waits[key] = (sem, val)
        for key, (sem, val) in waits.items():
            self.known[eng][key] = val
        return list(waits.values())

    def _record(self, tok, reads, writes):
        self.all_tokens[id(tok[0])] = (tok[0], max(tok[1], self.all_tokens.get(id(tok[0]), (None, 0))[1]))
        for k in writes:
            self.last_w[k] = tok
            self.readers[k] = []
        for k in reads:
            self.readers.setdefault(k, []).append(tok)

    def op(self, eng, fn, reads=(), writes=()):
        waits = self._deps(eng, reads, writes)
        self.cnt[eng] += 1
        tok = (self.sem[eng], self.cnt[eng], eng)
        self._emit(eng, waits, fn, self.sem[eng], 1)
        self._record(tok, reads, writes)
        return tok

    def dma(self, fn, reads=(), writes=(), q="sp", is_out=False):
        waits = self._deps(q, reads, writes)
        i = self.dcnt[q]
        self.dcnt[q] += 1
        slot = i % self.NDMA
        rnd = i // self.NDMA
        sem = self.dsem[q][slot]
        if rnd > 0:
            key = id(sem)
            if self.known[q].get(key, 0) < 16 * rnd:
                waits.append((sem, 16 * rnd))
                self.known[q][key] = 16 * rnd
        tok = (sem, 16 * (rnd + 1), "dma")
        self._emit(q, waits, fn, sem, 16)
        self._record(tok, reads, writes)
        if is_out:
            self.out_tokens.append(tok)
        return tok

    def collective(self, fn, reads=(), writes=()):
        waits = self._deps("pool", reads, writes)
        tok = (self.ccsem, 1, "dma")
        self._emit("pool", waits, fn, self.ccsem, 1)
        self._record(tok, reads, writes)
        return tok

    def barrier(self):
        for e in self.ENGS:
            waits = []
            for key, (sem, val) in self.all_tokens.items():
                if self.known[e].get(key, 0) < val:
                    if e == "pe" and sem is self.sem["pe"]:
                        continue
                    waits.append((sem, val))
                    self.known[e][key] = val
            if waits:
                self._emit(e, waits, None, None, 0)
        self.last_w = {}
        self.readers = {}

    def emit(self):
        final = {}
        for (sem, val, _) in self.out_tokens:
            if id(sem) not in final or final[id(sem)][1] < val:
                final[id(sem)] = (sem, val)
        for (s_, v) in final.values():
            self.eobj["sp"].wait_ge(s_, v)


def hi(ap):
    return ap.bitcast(BF16)[:, 1::2]


def build(debug=False, stage=3, dev=None):
    dev = dev or {}
    NBLK = dev.get("nblk", 4)
    nc = bass.Bass("TRN2", target_bir_lowering=False)

    def din(name, shape, dt=F32):
        return nc.dram_tensor(name, list(shape), dt, kind="ExternalInput").ap()

    def dscr(name, shape, dt=F32):
        return nc.dram_tensor(name, list(shape), dt, kind="Internal").ap()

    xs = din("xs", [NTOK, D])
    hm_d = din("hm", [128, 1])
    cm_d = din("cm", [128, 12])
    w_in = din("w_in", [D, 6144])
    pool_w = din("pool_w", [4, 256, 256])
    w_glu = din("w_glu", [1024, 1024])
    w_bp = din("w_bp", [1024, D])
    w_bs = din("w_bs", [1024, D])
    w_out = din("w_out", [D, D])
    if stage >= 3:
        w_gate = din("w_gate", [NE, D, 512])
        w_up = din("w_up", [NE, D, 512])
        w_down = din("w_down", [NE, 512, D])
    gains_d = din("gains", [128, 3, D])
    cols_d = din("cols", [128, 3, 8])
    wr_d = din("wr", [128, KC, 72])
    br_d = din("br", [128, 72])
    aL1_d = din("aL1", [128, 3, 8, 64])
    bL1_d = din("bL1", [128, 2, 8, 64])
    aL2_d = din("aL2", [128, 3, 32])
    bL2_d = din("bL2", [128, 2, 32, 16])
    cL2_d = din("cL2", [128, 2, 32, 16])
    cst_d = din("cst", [128, 1024])
    out_d = nc.dram_tensor("out", [NT, D], F32, kind="ExternalOutput").ap()

    cc_in = dscr("cc_in", [128, 64])
    cc_out = dscr("cc_out", [4 * 128, 64])
    hs = dscr("hs", [NT, D])
    xg = dscr("xg", [NE * CAP + 128, D], BF16)
    ysc = dscr("ysc", [NE * CAP + 128, D])
    sTd = dscr("sTd", [128, 8 * NT], BF16)
    with ExitStack() as st:
        S = Sched(nc, st)

        def dump(name, tile_, shape, dt, key):
            if not debug:
                return
            d_ap = nc.dram_tensor("dbg_" + name, list(shape), dt, kind="ExternalOutput").ap()
            S.dma(lambda e: e.dma_start(out=d_ap, in_=tile_[:]), [key], [], is_out=True)

        def V(fn, r=(), w=()): return S.op("dve", fn, r, w)
        def A(fn, r=(), w=()): return S.op("act", fn, r, w)
        def G(fn, r=(), w=()): return S.op("pool", fn, r, w)
        def PE(fn, r=(), w=()): return S.op("pe", fn, r, w)
        def DM(fn, r=(), w=(), **kw): return S.dma(fn, r, w, **kw)

        def T(stk, name, shape, dt=F32):
            return stk.enter_context(nc.sbuf_tensor("sb_" + name, list(shape), dt))

        ps = [st.enter_context(nc.psum_tensor(f"ps{i}", [128, 512], F32)) for i in range(8)]
        ps_rr = [0]

        def nbank():
            i = ps_rr[0]
            ps_rr[0] = (i + 1) % 8
            return i

        cst = T(st, "cst", [128, 1024])
        cstb = T(st, "cstb", [128, 512], BF16)
        cols = T(st, "cols", [128, 3, 8])
        hm = T(st, "hm", [128, 1])
        cm = T(st, "cm", [128, 12])
        DM(lambda e: e.dma_start(out=cst[:], in_=cst_d), w=["cst"])
        DM(lambda e: e.dma_start(out=cols[:], in_=cols_d), w=["cols"])
        DM(lambda e: e.dma_start(out=hm[:], in_=hm_d), w=["hm"])
        DM(lambda e: e.dma_start(out=cm[:], in_=cm_d), w=["cm"])
        V(lambda e: e.tensor_copy(out=cstb[:, 0:384], in_=cst[:, 0:384]), ["cst"], ["cstb"])
        ident32 = cst[:, 0:128]
        identb = cstb[:, 0:128]
        trib = cstb[:, 128:256]
        onesb = cstb[:, 256:384]
        iota_c = cst[:, 384:384 + 259]
        iota_e = cst[:, 648:712]
        mg2 = [cst[:, 712:713], cst[:, 713:714]]
        mqd = [cst[:, 714 + j:715 + j] for j in range(4)]
        mh2 = [cst[:, 718:719], cst[:, 719:720]]
        iota_p = cst[:, 720:721]

        gw = T(st, "gw", [128, 16, 2])
        gidx = T(st, "gidx", [128, 16, 2], I32)
        zs = None

        def load_norm_T(stk_tiles, row0, nrows, gain, uT_ap, col0, tag):
            xt, ub, ssq, kx, ku = stk_tiles
            DM(lambda e: e.dma_start(out=xt[:nrows, :], in_=xs[row0:row0 + nrows, :]), [], [kx])
            A(lambda e: e.activation(out=ub[:nrows, :], in_=xt[:nrows, :], func=AF.Square, accum_out=ssq[:nrows, 0:1]), [kx], [ku, ku + "s"])
            V(lambda e: e.tensor_scalar(out=ssq[:nrows, 1:2], in0=ssq[:nrows, 0:1], scalar1=1.0 / D, scalar2=EPS,
                                        op0=ALU.mult, op1=ALU.add), [ku + "s"], [ku + "s"])
            A(lambda e: e.activation(out=ssq[:nrows, 1:2], in_=ssq[:nrows, 1:2], func=AF.Sqrt), [ku + "s"], [ku + "s"])
            V(lambda e: e.reciprocal(out=ssq[:nrows, 2:3], in_=ssq[:nrows, 1:2]), [ku + "s"], [ku + "s"])
            V(lambda e: e.scalar_tensor_tensor(out=ub[:nrows, :], in0=xt[:nrows, :], scalar=ssq[:nrows, 2:3],
                                               in1=gain[:nrows, :], op0=ALU.mult, op1=ALU.mult),
              [kx, ku + "s", "gains", ku], [ku])
            for half in range(2):
                b = nbank()
                pk = "ps%d" % b
                psb = ps[b][:].bitcast(BF16)

                def tr(e, half=half, psb=psb):
                    ins = None
                    for j in range(8):
                        kc = half * 8 + j
                        ins = e.transpose(out=psb[:, j * 128:j * 128 + nrows], in_=ub[:nrows, kc * 128:(kc + 1) * 128],
                                          identity=identb[:nrows, :nrows])
                    return ins
                PE(tr, [ku, "cstb"], [pk])
                eng = A if half == 0 else V
                if half == 0:
                    A(lambda e, half=half, psb=psb: e.activation(
                        out=uT_ap[:, half * 8:half * 8 + 8, col0:col0 + nrows],
                        in_=psb[:, 0:1024].rearrange("p (k t) -> p k t", t=128)[:, :, 0:nrows], func=AF.Copy), [pk], tag if isinstance(tag, list) else [tag])
                else:
                    V(lambda e, half=half, psb=psb: e.tensor_copy(
                        out=uT_ap[:, half * 8:half * 8 + 8, col0:col0 + nrows],
                        in_=psb[:, 0:1024].rearrange("p (k t) -> p k t", t=128)[:, :, 0:nrows]), [pk], tag if isinstance(tag, list) else [tag])


        if dev.get("skip_ssm"):
            sT_in = nc.dram_tensor("sT_in", [128, 8, NT], BF16, kind="ExternalInput").ap()
            with nc.allow_non_contiguous_dma(reason="dev"):
                pass
            DM(lambda e: e.dma_start(out=sTd.rearrange("p (a t) -> p a t", a=8), in_=sT_in), [], ["sTd"])
        else:
            ssm_stack = ExitStack()
            sT = T(ssm_stack, "sT", [128, 8, NT], BF16)
            zs = T(ssm_stack, "zs", [128, 8, 8, NCH], BF16)

            with ExitStack() as a1:
                gmix = T(a1, "gmix", [128, D])
                DM(lambda e: e.dma_start(out=gmix[:], in_=gains_d[:, 0, :]), w=["gains"])
                wss = T(a1, "wss", [128, KC, 1024])
                w_in_v = w_in.rearrange("(k p) n -> p k n", p=128)
                for j in range(8):
                    DM(lambda e, j=j: e.dma_start(out=wss[:, 2 * j:2 * j + 2, :], in_=w_in_v[:, 2 * j:2 * j + 2, 1024:2048]), [], ["wss"])
                xts = [T(a1, "xt%d" % i, [128, D]) for i in range(2)]
                ubs = [T(a1, "ub%d" % i, [128, D], BF16) for i in range(2)]
                sqs = [T(a1, "sq%d" % i, [128, 4]) for i in range(2)]
                uTs = [T(a1, "uT%d" % i, [128, KC, 512], BF16) for i in range(2)]
                tile_i = [0]

                def norm_tiles():
                    i = tile_i[0] % 2
                    tile_i[0] += 1
                    return (xts[i], ubs[i], sqs[i], "xt%d" % i, "ub%d" % i)

                blocks = [(0, NH)] + [(NH + 512 * b, 512) for b in range(4)]
                for bi, (c0, n) in enumerate(blocks):
                    uT = uTs[bi % 2]
                    tag = "uT%d" % (bi % 2)
                    for t0 in range(0, n, 128):
                        nr = min(128, n - t0)
                        load_norm_T(norm_tiles(), c0 + t0, nr, gmix, uT, t0, tag)
                    for m in range(8):
                        b = nbank()
                        pk = "ps%d" % b

                        def zmm(e, m=m, b=b, uT=uT, n=n):
                            ins = None
                            for kc in range(KC):
                                ins = e.matmul(ps[b][:, 0:n], lhsT=hi(wss[:, kc, m * 128:(m + 1) * 128]), rhs=uT[:, kc, 0:n],
                                               start=(kc == 0), stop=(kc == KC - 1))
                            return ins
                        PE(zmm, ["wss", tag], [pk])
                        ch0 = c0 // TCH
                        nchk = n // TCH
                        src = ps[b][:, 0:n].rearrange("p (c s) -> p s c", s=TCH)
                        dst = zs[:, m, :, ch0:ch0 + nchk]
                        if bi == 0:
                            V(lambda e, src=src, dst=dst: e.tensor_scalar(out=dst, in0=src, scalar1=hm[:, 0:1], scalar2=None,
                                                                          op0=ALU.mult), [pk, "hm"], ["zs"])
                        elif m % 2 == 0:
                            A(lambda e, src=src, dst=dst: e.activation(out=dst, in_=src, func=AF.Copy), [pk], ["zs"])
                        else:
                            V(lambda e, src=src, dst=dst: e.tensor_copy(out=dst, in_=src), [pk], ["zs"])
            S.barrier()
            Ptab = T(ssm_stack, "Ptab", [128, 8, 8, 2, 128], BF16)
            Qtab = T(ssm_stack, "Qtab", [128, 32, 9, 2, 32], BF16)
            Ktab = T(ssm_stack, "Ktab", [128, 8, 8, 128], BF16)
            sml = T(ssm_stack, "sml", [128, 8, 32])
            Fst = T(ssm_stack, "Fst", [128, 32, 2])
            Xc = T(ssm_stack, "Xc", [128, 32, 2])

            t1 = ExitStack()
            if True:
                aL1 = T(t1, "aL1", [128, 3, 256])
                bL1 = T(t1, "bL1", [128, 2, 256])
                w1 = T(t1, "w1", [128, 12, 256])
                pw = T(t1, "pw", [128, 2, 2, 256])
                PP = T(t1, "PP", [128, 2, 2, 256])

                def lam_block(pref, akey, a_re, a_im, ldt, W, n, mults):
                    r = [pref + "w"]
                    k = pref + "w"
                    A(lambda e: e.activation(out=W[:, 4, :n], in_=ldt, func=AF.Exp), r + [akey], [k])
                    V(lambda e: e.tensor_tensor(out=W[:, 5, :n], in0=a_re, in1=W[:, 4, :n], op=ALU.mult), r + [akey], [k])
                    V(lambda e: e.tensor_tensor(out=W[:, 6, :n], in0=a_im, in1=W[:, 4, :n], op=ALU.mult), r + [akey], [k])

                    def expi(kk, o_mag, o_re, o_im):
                        A(lambda e: e.activation(out=W[:, 7, :n], in_=W[:, 5, :n], func=AF.Exp, scale=float(kk)), r, [k])
                        for (off, dst) in ((0.0, 8), (0.25, 9)):
                            V(lambda e, off=off, dst=dst: e.tensor_scalar(out=W[:, dst, :n], in0=W[:, 6, :n], scalar1=float(kk) / TWO_PI,
                                                                          scalar2=off, op0=ALU.mult, op1=ALU.add), r, [k])
                            V(lambda e, dst=dst: e.tensor_scalar(out=W[:, 10, :n], in0=W[:, dst, :n], scalar1=MAGIC, scalar2=None,
                                                                 op0=ALU.add), r, [k])
                            V(lambda e, dst=dst: e.tensor_scalar(out=W[:, 10, :n], in0=W[:, 10, :n], scalar1=-MAGIC, scalar2=None,
                                                                 op0=ALU.add), r, [k])
                            V(lambda e, dst=dst: e.tensor_tensor(out=W[:, dst, :n], in0=W[:, dst, :n], in1=W[:, 10, :n],
                                                                 op=ALU.subtract), r, [k])
                            A(lambda e, dst=dst: e.activation(out=W[:, dst, :n], in_=W[:, dst, :n], func=AF.Sin, scale=TWO_PI), r, [k])
                        if o_mag is not None:
                            V(lambda e: e.tensor_copy(out=o_mag, in_=W[:, 7, :n]), r, [k, pref + "o"])
                        V(lambda e: e.tensor_tensor(out=o_re, in0=W[:, 7, :n], in1=W[:, 9, :n], op=ALU.mult), r, [k, pref + "o"])
                        V(lambda e: e.tensor_tensor(out=o_im, in0=W[:, 7, :n], in1=W[:, 8, :n], op=ALU.mult), r, [k, pref + "o"])

                    expi(1, None, W[:, 0, :n], W[:, 1, :n])
                    for (kk, om, ore, oim) in mults:
                        expi(kk, om, ore, oim)
                    lam_block.expi = expi
                    V(lambda e: e.tensor_tensor(out=W[:, 7, :n], in0=a_re, in1=a_re, op=ALU.mult), r + [akey], [k])
                    V(lambda e: e.tensor_tensor(out=W[:, 8, :n], in0=a_im, in1=a_im, op=ALU.mult), r + [akey], [k])
                    V(lambda e: e.tensor_tensor(out=W[:, 7, :n], in0=W[:, 7, :n], in1=W[:, 8, :n], op=ALU.add), r, [k])
                    V(lambda e: e.reciprocal(out=W[:, 7, :n], in_=W[:, 7, :n]), r, [k])
                    V(lambda e: e.tensor_scalar(out=W[:, 8, :n], in0=W[:, 0, :n], scalar1=-1.0, scalar2=None, op0=ALU.add), r, [k])
                    V(lambda e: e.tensor_tensor(out=W[:, 9, :n], in0=W[:, 8, :n], in1=a_re, op=ALU.mult), r + [akey], [k])
                    V(lambda e: e.tensor_tensor(out=W[:, 10, :n], in0=W[:, 1, :n], in1=a_im, op=ALU.mult), r + [akey], [k])
                    V(lambda e: e.tensor_tensor(out=W[:, 9, :n], in0=W[:, 9, :n], in1=W[:, 10, :n], op=ALU.add), r, [k])
                    V(lambda e: e.tensor_tensor(out=W[:, 2, :n], in0=W[:, 9, :n], in1=W[:, 7, :n], op=ALU.mult), r, [k])
                    V(lambda e: e.tensor_tensor(out=W[:, 9, :n], in0=W[:, 1, :n], in1=a_re, op=ALU.mult), r + [akey], [k])
                    V(lambda e: e.tensor_tensor(out=W[:, 10, :n], in0=W[:, 8, :n], in1=a_im, op=ALU.mult), r + [akey], [k])
                    V(lambda e: e.tensor_tensor(out=W[:, 9, :n], in0=W[:, 9, :n], in1=W[:, 10, :n], op=ALU.subtract), r, [k])
                    V(lambda e: e.tensor_tensor(out=W[:, 3, :n], in0=W[:, 9, :n], in1=W[:, 7, :n], op=ALU.mult), r, [k])

                def cmul(o_re, o_im, a_re, a_im, b_re, b_im, t1_, t2_, rk, wk, neg_im=False):
                    V(lambda e: e.tensor_tensor(out=t1_, in0=a_re, in1=b_re, op=ALU.mult), rk, wk)
                    V(lambda e: e.tensor_tensor(out=t2_, in0=a_im, in1=b_im, op=ALU.mult), rk, wk)
                    V(lambda e: e.tensor_tensor(out=o_re, in0=t1_, in1=t2_, op=ALU.subtract), rk, wk)
                    V(lambda e: e.tensor_tensor(out=t1_, in0=a_re, in1=b_im, op=ALU.mult), rk, wk)
                    V(lambda e: e.tensor_tensor(out=t2_, in0=a_im, in1=b_re, op=ALU.mult), rk, wk)
                    if neg_im:
                        V(lambda e: e.scalar_tensor_tensor(out=o_im, in0=t1_, scalar=-1.0, in1=t2_, op0=ALU.mult, op1=ALU.subtract), rk, wk)
                    else:
                        V(lambda e: e.tensor_tensor(out=o_im, in0=t1_, in1=t2_, op=ALU.add), rk, wk)

                Bb = T(t1, "Bb", [128, 2, 256])
                tA = T(t1, "tA", [128, 2, 256])
                tB = T(t1, "tB", [128, 2, 256])
                for hb in range(2):
                    DM(lambda e, hb=hb: e.dma_start(out=aL1[:].rearrange("p a (g n) -> p a g n", g=4), in_=aL1_d[:, :, 4 * hb:4 * hb + 4, :]), [], ["aL1"])
                    DM(lambda e, hb=hb: e.dma_start(out=bL1[:].rearrange("p a (g n) -> p a g n", g=4), in_=bL1_d[:, :, 4 * hb:4 * hb + 4, :]), [], ["bL1"])
                    lam_block("L1", "aL1", aL1[:, 0, :], aL1[:, 1, :], aL1[:, 2, :], w1, 256, [])
                    expi1 = lam_block.expi
                    KL1 = ["L1w", "L1o", "bL1"]
                    cmul(Bb[:, 0, :], Bb[:, 1, :], w1[:, 2, :], w1[:, 3, :], bL1[:, 0, :], bL1[:, 1, :],
                         w1[:, 10, :], w1[:, 11, :], KL1, ["L1w", "Bb"])
                    bb_re = Bb[:, 0:1, :].to_broadcast([128, 2, 256])
                    bb_im = Bb[:, 1:2, :].to_broadcast([128, 2, 256])
                    for kq in range(4):
                        for j in range(2):
                            kk_ = 2 * kq + j
                            if kk_ == 0:
                                V(lambda e: e.memset(pw[:, 0, 0, :], 1.0), ["PP"], ["L1o"])
                                V(lambda e: e.memset(pw[:, 1, 0, :], 0.0), ["PP"], ["L1o"])
                            else:
                                expi1(kk_, None, pw[:, 0, j, :], pw[:, 1, j, :])
                        cmul(PP[:, 0], PP[:, 1], pw[:, 0], pw[:, 1], bb_re, bb_im, tA[:], tB[:], KL1 + ["tAB", "Bb"], ["PP", "tAB"])
                        for ri in range(2):
                            for g2 in range(2):
                                V(lambda e, ri=ri, g2=g2, kq=kq, hb=hb: e.tensor_scalar(
                                    out=Ptab[:, 4 * hb:4 * hb + 4, 2 * kq:2 * kq + 2, ri, g2 * 64:(g2 + 1) * 64],
                                    in0=PP[:, ri].rearrange("p k (g n) -> p g k n", g=4),
                                    scalar1=mg2[g2], scalar2=None, op0=ALU.mult), ["PP", "cst"], ["Ptab"])
            S.barrier()
            t1.close()

            with ExitStack() as t2:
                aL2 = T(t2, "aL2", [128, 3, 32])
                bL2 = T(t2, "bL2", [128, 2, 512])
                cL2 = T(t2, "cL2", [128, 2, 512])
                DM(lambda e: e.dma_start(out=aL2[:], in_=aL2_d), w=["aL2"])
                DM(lambda e: e.dma_start(out=bL2[:], in_=bL2_d.rearrange("p a q h -> p a (q h)")), w=["bL2"])
                DM(lambda e: e.dma_start(out=cL2[:], in_=cL2_d.rearrange("p a q h -> p a (q h)")), w=["cL2"])
                w2 = T(t2, "w2", [128, 12, 32])
                pw2 = T(t2, "pw2", [128, 2, 9, 32])
                V(lambda e: e.memset(pw2[:, 0, 0, :], 1.0), [], ["L2o"])
                V(lambda e: e.memset(pw2[:, 1, 0, :], 0.0), [], ["L2o"])
                mults = [(kk, None, pw2[:, 0, kk, :], pw2[:, 1, kk, :]) for kk in range(1, 8)]
                mults.append((8, sml[:, 0, :], pw2[:, 0, 8, :], pw2[:, 1, 8, :]))
                mults.append((2048, None, sml[:, 2, :], sml[:, 3, :]))
                lam_block("L2", "aL2", aL2[:, 0, :], aL2[:, 1, :], aL2[:, 2, :], w2, 32, mults)
                KL2 = ["L2w", "L2o", "bL2", "cL2"]
                V(lambda e: e.tensor_scalar(out=sml[:, 1, :], in0=w2[:, 6, :], scalar1=8.0 / TWO_PI, scalar2=None, op0=ALU.mult), KL2, ["L2o"])
                V(lambda e: e.tensor_scalar(out=w2[:, 10, :], in0=sml[:, 1, :], scalar1=MAGIC, scalar2=None, op0=ALU.add), KL2, ["L2w"])
                V(lambda e: e.tensor_scalar(out=w2[:, 10, :], in0=w2[:, 10, :], scalar1=-MAGIC, scalar2=None, op0=ALU.add), KL2, ["L2w"])
                V(lambda e: e.tensor_tensor(out=sml[:, 1, :], in0=sml[:, 1, :], in1=w2[:, 10, :], op=ALU.subtract), KL2, ["L2o"])
                BB2 = T(t2, "BB2", [128, 2, 512])
                tC = T(t2, "tC", [128, 512])
                tD = T(t2, "tD", [128, 512])
                cf_re = w2[:, 2, :].unsqueeze(2).to_broadcast([128, 32, 16])
                cf_im = w2[:, 3, :].unsqueeze(2).to_broadcast([128, 32, 16])
                v3 = lambda ap: ap.rearrange("p (q h) -> p q h", h=16)
                cmul(v3(BB2[:, 0, :]), v3(BB2[:, 1, :]), cf_re, cf_im, v3(bL2[:, 0, :]), v3(bL2[:, 1, :]),
                     v3(tC[:]), v3(tD[:]), KL2 + ["tCD"], ["BB2", "tCD"])
                BBp = T(t2, "BBp", [128, 32, 2, 32], BF16)
                for ri in range(2):
                    for g2 in range(2):
                        V(lambda e, ri=ri, g2=g2: e.tensor_scalar(
                            out=BBp[:, :, ri, g2 * 16:(g2 + 1) * 16], in0=v3(BB2[:, ri, :]),
                            scalar1=mh2[g2], scalar2=None, op0=ALU.mult), ["BB2", "cst"], ["BBp"])
                CL = T(t2, "CL", [128, 2, 3, 512])
                tE = T(t2, "tE", [128, 3, 512])
                tF = T(t2, "tF", [128, 3, 512])
                v4 = lambda ap: ap.rearrange("p k (q h) -> p k q h", h=16)
                c_re = v3(cL2[:, 0, :]).unsqueeze(1).to_broadcast([128, 3, 32, 16])
                c_im = v3(cL2[:, 1, :]).unsqueeze(1).to_broadcast([128, 3, 32, 16])
                for k3 in range(3):
                    p_re = pw2[:, 0, 3 * k3:3 * k3 + 3, :].unsqueeze(3).to_broadcast([128, 3, 32, 16])
                    p_im = pw2[:, 1, 3 * k3:3 * k3 + 3, :].unsqueeze(3).to_broadcast([128, 3, 32, 16])
                    cmul(v4(CL[:, 0]), v4(CL[:, 1]), c_re, c_im, p_re, p_im, v4(tE[:]), v4(tF[:]), KL2 + ["tEF"], ["CL", "tEF"], neg_im=True)
                    for ri in range(2):
                        for g2 in range(2):
                            V(lambda e, ri=ri, g2=g2, k3=k3: e.tensor_scalar(
                                out=Qtab[:, :, 3 * k3:3 * k3 + 3, ri, g2 * 16:(g2 + 1) * 16],
                                in0=v4(CL[:, ri]).rearrange("p k q h -> p q k h"),
                                scalar1=mh2[g2], scalar2=None, op0=ALU.mult), ["CL", "cst"], ["Qtab"])
                for gb in range(8):
                    b = nbank()
                    pk = "ps%d" % b

                    def kmm(e, gb=gb, b=b):
                        ins = None
                        for qd in range(4):
                            q = 4 * gb + qd
                            for ri in range(2):
                                ins = e.matmul(ps[b][32 * qd:32 * qd + 32, 0:256], lhsT=BBp[:, q, ri, :],
                                               rhs=Qtab[:, q, 0:8, ri, :], start=(ri == 0), stop=(ri == 1),
                                               tile_position=(0, 32 * qd))
                        return ins
                    PE(kmm, ["BBp", "Qtab"], [pk])
                    for qd in range(4):
                        V(lambda e, gb=gb, b=b, qd=qd: e.tensor_scalar(
                            out=Ktab[:, gb, :, 32 * qd:32 * qd + 32],
                            in0=ps[b][:, 0:256].rearrange("p (t c) -> p t c", c=32),
                            scalar1=mqd[qd], scalar2=None, op0=ALU.mult), [pk, "cst"], ["Ktab"])

            dump("zs", zs, [128, 8, 8, NCH], BF16, "zs")
            dump("Ptab", Ptab, [128, 8, 8, 2, 128], BF16, "Ptab")
            dump("Qtab", Qtab, [128, 32, 9, 2, 32], BF16, "Qtab")
            dump("Ktab", Ktab, [128, 8, 8, 128], BF16, "Ktab")
            dump("sml", sml, [128, 8, 32], F32, "L2o")
            S.barrier()

            with ExitStack() as a3:
                Sq = T(a3, "Sq", [128, 4, 2, NCH])
                St = T(a3, "St", [128, 4, 2, NCH])
                Xt = Sq
                Wcs = T(a3, "Wcs", [128, 2, 4, NCH + 1])
                Rt = T(a3, "Rt", [128, NCH])
                tm = T(a3, "tm", [128, 2, 4, NCH + 1])
                Xb = T(a3, "Xb", [128, 4, 2, NCH], BF16)
                yt = T(a3, "yt", [128, 1024])
                y2 = T(a3, "y2", [128, 1024])
                Fall = T(a3, "Fall", [128, 4, 64])
                Gh = T(a3, "Gh", [128, 4, 64])

                def chunk_states(blk, with_carry):
                    q0 = 4 * blk
                    for ql in range(4):
                        for ri in range(2):
                            b = nbank()
                            pk = "ps%d" % b

                            def smm(e, ql=ql, ri=ri, b=b):
                                ins = None
                                for s in range(TCH):
                                    ins = e.matmul(ps[b][:, 0:NCH], lhsT=Ptab[32 * ql:32 * ql + 32, blk, 7 - s, ri, :],
                                                   rhs=zs[32 * ql:32 * ql + 32, blk, s, :], start=(s == 0), stop=(s == TCH - 1),
                                                   tile_position=(32 * ql, 0))
                                return ins
                            PE(smm, ["Ptab", "zs"], [pk])
                            if ri == 0:
                                A(lambda e, ql=ql, ri=ri, b=b: e.activation(out=Sq[:, ql, ri, :], in_=ps[b][:, 0:NCH], func=AF.Copy), [pk], ["Sq"])
                            else:
                                V(lambda e, ql=ql, ri=ri, b=b: e.tensor_copy(out=Sq[:, ql, ri, :], in_=ps[b][:, 0:NCH]), [pk], ["Sq"])
                    if with_carry:
                        V(lambda e: e.tensor_tensor(out=Sq[:, :, :, 1], in0=Sq[:, :, :, 1], in1=Xc[:, q0:q0 + 4, :], op=ALU.add), ["Sq", "Xc"], ["Sq"])
                    fr = sml[:, 1, q0:q0 + 4].unsqueeze(2).to_broadcast([128, 4, NCH + 1])
                    io = iota_c.unsqueeze(1).to_broadcast([128, 4, NCH + 1])
                    V(lambda e: e.tensor_tensor(out=tm[:, 0], in0=fr, in1=io, op=ALU.mult), ["sml", "cst", "tm"], ["tm"])
                    for (j, off) in ((1, 0.0), (0, 0.25)):
                        V(lambda e, off=off: e.tensor_scalar(out=tm[:, 1], in0=tm[:, 0], scalar1=off, scalar2=MAGIC, op0=ALU.add, op1=ALU.add), ["tm"], ["tm"])
                        V(lambda e: e.tensor_scalar(out=tm[:, 1], in0=tm[:, 1], scalar1=-MAGIC, scalar2=None, op0=ALU.add), ["tm"], ["tm"])
                        V(lambda e, off=off: e.scalar_tensor_tensor(out=tm[:, 1], in0=tm[:, 0], scalar=off, in1=tm[:, 1], op0=ALU.add, op1=ALU.subtract), ["tm"], ["tm"])
                        A(lambda e, j=j: e.activation(out=Wcs[:, j], in_=tm[:, 1], func=AF.Sin, scale=TWO_PI), ["tm"], ["Wcs"])
                    cw = Wcs[:, 0, :, 1:NCH + 1]
                    sw = Wcs[:, 1, :, 1:NCH + 1]
                    t_a = tm[:, 0, :, 0:NCH]
                    t_b = tm[:, 1, :, 0:NCH]
                    kk = ["Sq", "Wcs", "tm"]
                    V(lambda e: e.tensor_tensor(out=t_a, in0=Sq[:, :, 0, :], in1=cw, op=ALU.mult), kk, ["tm"])
                    V(lambda e: e.tensor_tensor(out=t_b, in0=Sq[:, :, 1, :], in1=sw, op=ALU.mult), kk, ["tm"])
                    V(lambda e: e.tensor_tensor(out=St[:, :, 0, :], in0=t_a, in1=t_b, op=ALU.add), ["tm"], ["St"])
                    V(lambda e: e.tensor_tensor(out=t_a, in0=Sq[:, :, 1, :], in1=cw, op=ALU.mult), kk, ["tm"])
                    V(lambda e: e.tensor_tensor(out=t_b, in0=Sq[:, :, 0, :], in1=sw, op=ALU.mult), kk, ["tm"])
                    V(lambda e: e.tensor_tensor(out=St[:, :, 1, :], in0=t_a, in1=t_b, op=ALU.subtract), ["tm"], ["St"])
                    for ql in range(4):
                        V(lambda e, ql=ql: e.tensor_copy(out=Rt[:], in_=sml[:, 0, q0 + ql:q0 + ql + 1].to_broadcast([128, NCH])), ["sml", "Rt"], ["Rt"])
                        for ri in range(2):
                            V(lambda e, ri=ri, ql=ql: e.tensor_tensor_scan(out=Xt[:, ql, ri, :], data0=Rt[:], data1=St[:, ql, ri, :],
                                                                           initial=0.0, op0=ALU.mult, op1=ALU.add), ["Rt", "St", "Sq"], ["Sq"])

                for blk in range(8):
                    chunk_states(blk, False)
                    q0 = 4 * blk
                    c258 = Wcs[:, 0, :, NCH]
                    s258 = Wcs[:, 1, :, NCH]
                    xr = Xt[:, :, 0, NCH - 1]
                    xi = Xt[:, :, 1, NCH - 1]
                    ta = tm[:, 0, :, 0]
                    tb = tm[:, 1, :, 0]
                    kk = ["Sq", "Wcs", "tm"]
                    V(lambda e, xr=xr, c258=c258, ta=ta: e.tensor_tensor(out=ta, in0=xr, in1=c258, op=ALU.mult), kk, ["tm"])
                    V(lambda e, xi=xi, s258=s258, tb=tb: e.tensor_tensor(out=tb, in0=xi, in1=s258, op=ALU.mult), kk, ["tm"])
                    V(lambda e, q0=q0, ta=ta, tb=tb: e.tensor_tensor(out=Fst[:, q0:q0 + 4, 0], in0=ta, in1=tb, op=ALU.subtract), ["tm"], ["Fst"])
                    V(lambda e, xr=xr, s258=s258, ta=ta: e.tensor_tensor(out=ta, in0=xr, in1=s258, op=ALU.mult), kk, ["tm"])
                    V(lambda e, xi=xi, c258=c258, tb=tb: e.tensor_tensor(out=tb, in0=xi, in1=c258, op=ALU.mult), kk, ["tm"])
                    V(lambda e, q0=q0, ta=ta, tb=tb: e.tensor_tensor(out=Fst[:, q0:q0 + 4, 1], in0=ta, in1=tb, op=ALU.add), ["tm"], ["Fst"])
                DM(lambda e: e.dma_start(out=cc_in, in_=Fst[:].rearrange("p q r -> p (q r)")), ["Fst"], ["cc_in"])
                S.collective(lambda e: e.collective_compute("AllGather", ALU.bypass, replica_groups=[[0, 1, 2, 3], [4, 5, 6, 7]],
                                                            ins=[cc_in], outs=[cc_out]), ["cc_in"], ["cc_out"])
                DM(lambda e: e.dma_start(out=Fall[:], in_=cc_out.rearrange("(j p) f -> p j f", p=128)), ["cc_out"], ["Fall"])
                for p_ in range(3):
                    V(lambda e, p_=p_: e.tensor_scalar(out=Gh[:, p_, :], in0=Fall[:, 0, :], scalar1=cm[:, p_:p_ + 1], scalar2=None, op0=ALU.mult), ["Fall", "cm"], ["Gh"])
                    for j in range(1, 4):
                        V(lambda e, p_=p_, j=j: e.scalar_tensor_tensor(out=Gh[:, p_, :], in0=Fall[:, j, :], scalar=cm[:, 3 * j + p_:3 * j + p_ + 1],
                                                                       in1=Gh[:, p_, :], op0=ALU.mult, op1=ALU.add), ["Fall", "cm", "Gh"], ["Gh"])
                gv = lambda p_, ri: Gh[:, p_, :].rearrange("p (q r) -> p q r", r=2)[:, :, ri]
                lre = sml[:, 2, :]
                lim = sml[:, 3, :]
                ta = tm[:, 0, 0, 0:32]
                tb = tm[:, 1, 0, 0:32]
                acc_re = Gh[:, 3, 0:32]
                acc_im = Gh[:, 3, 32:64]
                kk = ["Gh", "sml", "tm"]

                def horner(src_re, src_im, add_p, o_re, o_im):
                    V(lambda e: e.tensor_tensor(out=ta, in0=src_re, in1=lre, op=ALU.mult), kk, ["tm"])
                    V(lambda e: e.tensor_tensor(out=tb, in0=src_im, in1=lim, op=ALU.mult), kk, ["tm"])
                    V(lambda e: e.tensor_tensor(out=ta, in0=ta, in1=tb, op=ALU.subtract), ["tm"], ["tm"])
                    V(lambda e: e.tensor_tensor(out=tb, in0=src_re, in1=lim, op=ALU.mult), kk, ["tm"])
                    V(lambda e: e.tensor_tensor(out=o_re, in0=ta, in1=gv(add_p, 0), op=ALU.add), kk, ["Gh", "Xc"])
                    V(lambda e: e.tensor_tensor(out=ta, in0=src_im, in1=lre, op=ALU.mult), kk, ["tm"])
                    V(lambda e: e.tensor_tensor(out=tb, in0=tb, in1=ta, op=ALU.add), ["tm"], ["tm"])
                    V(lambda e: e.tensor_tensor(out=o_im, in0=tb, in1=gv(add_p, 1), op=ALU.add), kk, ["Gh", "Xc"])
                horner(gv(2, 0), gv(2, 1), 1, acc_re, acc_im)
                horner(acc_re, acc_im, 0, Xc[:, :, 0], Xc[:, :, 1])
                dump("Fst", Fst, [128, 32, 2], F32, "Fst")
                dump("Xc", Xc, [128, 32, 2], F32, "Xc")

                for blk in range(8):
                    chunk_states(blk, True)
                    cw = Wcs[:, 0, :, 1:NCH]
                    sw = Wcs[:, 1, :, 1:NCH]
                    xr = Xt[:, :, 0, 0:NCH - 1]
                    xi = Xt[:, :, 1, 0:NCH - 1]
                    t_a = tm[:, 0, :, 0:NCH - 1]
                    t_b = tm[:, 1, :, 0:NCH - 1]
                    kk = ["Sq", "Wcs", "tm"]
                    V(lambda e, xr=xr, cw=cw, t_a=t_a: e.tensor_tensor(out=t_a, in0=xr, in1=cw, op=ALU.mult), kk, ["tm"])
                    V(lambda e, xi=xi, sw=sw, t_b=t_b: e.tensor_tensor(out=t_b, in0=xi, in1=sw, op=ALU.mult), kk, ["tm"])
                    V(lambda e, t_a=t_a, t_b=t_b: e.tensor_tensor(out=Xb[:, :, 0, 1:NCH], in0=t_a, in1=t_b, op=ALU.subtract), ["tm"], ["Xb"])
                    V(lambda e, xr=xr, sw=sw, t_a=t_a: e.tensor_tensor(out=t_a, in0=xr, in1=sw, op=ALU.mult), kk, ["tm"])
                    V(lambda e, xi=xi, cw=cw, t_b=t_b: e.tensor_tensor(out=t_b, in0=xi, in1=cw, op=ALU.mult), kk, ["tm"])
                    V(lambda e, t_a=t_a, t_b=t_b: e.tensor_tensor(out=Xb[:, :, 1, 1:NCH], in0=t_a, in1=t_b, op=ALU.add), ["tm"], ["Xb"])
                    banks = [nbank() for _ in range(4)]
                    pks = ["ps%d" % b for b in banks]

                    def omm(e, blk=blk, banks=banks):
                        ins = None
                        for j in range(4):
                            pb = ps[banks[j]]
                            first = True
                            for tau in range(0, 2 * j + 2):
                                s_lo = max(2 * j, tau)
                                ns = 2 * j + 2 - s_lo
                                o0 = (s_lo - 2 * j) * 256
                                ins = e.matmul(pb[:, o0:o0 + ns * 256], lhsT=Ktab[:, blk, tau, :],
                                               rhs=zs[:, blk, s_lo - tau:s_lo - tau + ns, 2:NCH],
                                               start=first, stop=False)
                                first = False
                            for sl in range(2):
                                s = 2 * j + sl
                                for ql in range(4):
                                    for ri in range(2):
                                        ins = e.matmul(pb[32 * ql:32 * ql + 32, sl * 256:(sl + 1) * 256],
                                                       lhsT=Qtab[:, 4 * blk + ql, s + 1, ri, :], rhs=Xb[:, ql, ri, 2:NCH],
                                                       start=False, stop=(sl == 1 and ql == 3 and ri == 1),
                                                       tile_position=(0, 32 * ql))
                        return ins
                    PE(omm, ["Ktab", "zs", "Qtab", "Xb"], pks)
                    for hf in range(2):
                        for jj in range(2):
                            j = 2 * hf + jj
                            V(lambda e, j=j, jj=jj, blk=blk, banks=banks: e.scalar_tensor_tensor(
                                out=yt[:, jj * 512:(jj + 1) * 512].rearrange("p (s c) -> p s c", s=2),
                                in0=zs[:, blk, 2 * j:2 * j + 2, 2:NCH], scalar=cols[:, 0, blk:blk + 1],
                                in1=ps[banks[j]][:, 0:512].rearrange("p (s c) -> p s c", s=2), op0=ALU.mult, op1=ALU.add),
                              ["zs", "cols", pks[j]], ["yt"])
                        A(lambda e: e.activation(out=y2[:], in_=yt[:], func=AF.Square), ["yt"], ["y2"])
                        V(lambda e: e.tensor_scalar(out=y2[:], in0=y2[:], scalar1=0.044715, scalar2=1.0, op0=ALU.mult, op1=ALU.add), ["y2"], ["y2"])
                        V(lambda e: e.tensor_tensor(out=y2[:], in0=y2[:], in1=yt[:], op=ALU.mult), ["y2", "yt"], ["y2"])
                        A(lambda e: e.activation(out=y2[:], in_=y2[:], func=AF.Sigmoid, scale=1.5957691216057308), ["y2"], ["y2"])
                        V(lambda e, blk=blk, hf=hf: e.tensor_tensor(
                            out=sT[:, blk, :].rearrange("p (c s) -> p s c", s=TCH)[:, 4 * hf:4 * hf + 4, :],
                            in0=yt[:].rearrange("p (s c) -> p s c", s=4),
                            in1=y2[:].rearrange("p (s c) -> p s c", s=4), op=ALU.mult), ["yt", "y2"], ["sT"])
            dump("sT", sT, [128, 8, NT], BF16, "sT")
            DM(lambda e: e.dma_start(out=sTd, in_=sT[:].rearrange("p a t -> p (a t)")), ["sT"], ["sTd"])
            S.barrier()
            ssm_stack.close()


        w_in_v = w_in.rearrange("(k p) n -> p k n", p=128)
        pbk = ExitStack()
        gain = T(pbk, "gain", [128, D])
        XH = T(pbk, "XH", [128, 4, D])
        ubs = [T(pbk, "vb%d" % i, [128, D], BF16) for i in range(2)]
        sqs = [T(pbk, "sqb%d" % i, [128, 4]) for i in range(2)]
        uT = XH[:, 2:4, :].rearrange("p a b -> p (a b)").bitcast(BF16).rearrange("p (k t) -> p k t", t=512)
        UTK = ["xh2", "xh3"]
        zp = T(pbk, "zp", [128, 8, NH + 512], BF16)
        pa = T(pbk, "pa", [128, NH + 512])
        pb_ = T(pbk, "pb", [128, NH + 512])
        dT = T(pbk, "dT", [128, 8, 512], BF16)
        s2T = T(pbk, "s2T", [128, 8, 512], BF16)
        mgT = T(pbk, "mgT", [128, KC, 512], BF16)
        NRING = 8
        sTb = T(pbk, "sTb", [128, 8, 512], BF16)
        ring = [T(pbk, "ring%d" % i, [128, 2048]) for i in range(NRING)]
        ring_i = [0]
        sg = [T(pbk, "sg%d" % i, [128, 512]) for i in range(3)]
        vT32 = T(pbk, "vT32", [128, KC, 128])
        wr = T(pbk, "wr", [128, KC, 72])
        br = T(pbk, "br", [128, 72])
        lg = T(pbk, "lg", [128, 72])
        rt = T(pbk, "rt", [128, 12, 64])
        rs = T(pbk, "rs", [128, 16])
        base = T(pbk, "base", [128, 64])
        mb = T(pbk, "mb", [128, 64], BF16)
        DM(lambda e: e.dma_start(out=wr[:], in_=wr_d), [], ["wr"])
        DM(lambda e: e.dma_start(out=br[:], in_=br_d), [], ["br"])
        V(lambda e: e.memset(base[:], 0.0), [], ["base"])
        V(lambda e: e.memset(ubs[0][:], 0.0), [], ["vb0"])
        for r in range((NE * CAP + 128) // 128):
            DM(lambda e, r=r: e.dma_start(out=xg[r * 128:(r + 1) * 128, :], in_=ubs[0][:]), ["vb0"], ["xg"])

        def ring_load(src_ap, shape_view):
            i = ring_i[0] % NRING
            ring_i[0] += 1
            key = "ring%d" % i
            dst = ring[i]
            DM(lambda e: e.dma_start(out=shape_view(dst), in_=src_ap), [], [key])
            return dst, key

        def w_piece(w_v, kc0, nkc, c0, ncols):
            dst, key = ring_load(w_v[:, kc0:kc0 + nkc, c0:c0 + ncols],
                                 lambda d: d[:, 0:nkc * ncols].rearrange("p (k n) -> p k n", n=ncols))
            return (lambda kl, c, n: hi(dst[:, kl * ncols + c:kl * ncols + c + n])), key

        nt_i = [0]

        def ntile():
            i = nt_i[0] % 2
            nt_i[0] += 1
            return (XH[:, i, :], ubs[i], sqs[i], "xh%d" % i, "vb%d" % i)

        pool_w_v = pool_w.rearrange("g (k p) n -> p g k n", p=128)
        w_glu_v = w_glu.rearrange("(k p) n -> p k n", p=128)
        w_bp_v = w_bp.rearrange("(k p) n -> p k n", p=128)
        w_bs_v = w_bs.rearrange("(k p) n -> p k n", p=128)
        w_out_v = w_out.rearrange("(k p) n -> p k n", p=128)
        ev_i = [0]

        def evac_copy(dst, src, rk, wk):
            ev_i[0] += 1
            if ev_i[0] % 2 == 0:
                A(lambda e: e.activation(out=dst, in_=src, func=AF.Copy), rk, wk)
            else:
                V(lambda e: e.tensor_copy(out=dst, in_=src), rk, wk)

        def zpool_cols(ncol, col_off, uT_ap, utk):
            for mp in range(4):
                getters = [w_piece(w_in_v, 8 * hk, 8, 256 * mp, 256) for hk in range(2)]
                for mm in range(2):
                    m = 2 * mp + mm
                    b = nbank()
                    pk = "ps%d" % b

                    def zmm(e, mm=mm, b=b):
                        ins = None
                        for kc in range(KC):
                            g_, _ = getters[kc // 8]
                            ins = e.matmul(ps[b][:, 0:ncol], lhsT=g_(kc % 8, mm * 128, 128), rhs=uT_ap[:, kc, 0:ncol],
                                           start=(kc == 0), stop=(kc == KC - 1))
                        return ins
                    PE(zmm, [getters[0][1], getters[1][1]] + utk, [pk])
                    evac_copy(zp[:, m, col_off:col_off + ncol], ps[b][:, 0:ncol], [pk], ["zp"])

        for tb in range(NBLK):
            tok0 = 512 * tb
            DM(lambda e, tok0=tok0: e.dma_start(out=sTb[:], in_=sTd.rearrange("p (a t) -> p a t", a=8)[:, :, tok0:tok0 + 512]), ["sTd"], ["sTb"])
            DM(lambda e: e.dma_start(out=gain[:], in_=gains_d[:, 0, :]), [], ["gains"])
            if tb == 0:
                load_norm_T(ntile(), 0, NH, gain, uT, 0, UTK)
                zpool_cols(NH, 0, uT, UTK)
            else:
                G(lambda e: e.tensor_copy(out=zp[:, :, 0:NH], in_=zp[:, :, 512:512 + NH]), ["zp"], ["zp"])
            for tt in range(4):
                load_norm_T(ntile(), NH + tok0 + 128 * tt, 128, gain, uT, 128 * tt, UTK)
            zpool_cols(512, NH, uT, UTK)
            for m in range(8):
                steps = m // 2 + 1
                srcs = [zp[:, m, :], pa[:], pb_[:], pa[:], pb_[:]]
                keys = ["zp", "pa", "pb", "pa", "pb"]
                for sidx in range(steps):
                    sh = 1 << sidx
                    lo = 2 * sh - 1
                    G(lambda e, sidx=sidx, sh=sh, lo=lo, srcs=srcs: e.tensor_tensor(
                        out=srcs[sidx + 1][:, lo:NH + 512], in0=srcs[sidx][:, lo:NH + 512], in1=srcs[sidx][:, lo - sh:NH + 512 - sh],
                        op=ALU.add), [keys[sidx]], [keys[sidx + 1]])
                V(lambda e, m=m, steps=steps, srcs=srcs: e.scalar_tensor_tensor(
                    out=dT[:, m, :], in0=srcs[steps][:, NH:NH + 512], scalar=1.0 / (1 << steps), in1=zp[:, m, NH:NH + 512],
                    op0=ALU.mult, op1=ALU.subtract), [keys[steps], "zp"], ["dT"])
            for gi in range(4):
                dst, key = ring_load(pool_w_v[:, gi, :, :], lambda d: d[:, 0:512].rearrange("p (k n) -> p k n", n=256))
                bks = [nbank(), nbank()]

                def pmm(e, gi=gi, dst=dst, bks=bks):
                    ins = None
                    for mo in range(2):
                        for kc in range(2):
                            ins = e.matmul(ps[bks[mo]][:, 0:512], lhsT=hi(dst[:, kc * 256 + mo * 128:kc * 256 + mo * 128 + 128]),
                                           rhs=dT[:, 2 * gi + kc, :], start=(kc == 0), stop=(kc == 1))
                    return ins
                PE(pmm, [key, "dT"], ["ps%d" % bks[0], "ps%d" % bks[1]])
                for mo in range(2):
                    m = 2 * gi + mo
                    A(lambda e, m=m, mo=mo, bks=bks: e.activation(out=dT[:, m, :], in_=ps[bks[mo]][:, 0:512], func=AF.Copy,
                                                                   scale=cols[:, 1, m:m + 1]), ["ps%d" % bks[mo], "cols"], ["dT"])
            for mp in range(4):
                g_, key = w_piece(w_glu_v, 0, 8, 256 * mp, 256)
                for mm in range(2):
                    mo = 2 * mp + mm
                    b = nbank()
                    pk = "ps%d" % b

                    def gmm(e, mm=mm, b=b, g_=g_):
                        ins = None
                        for kc in range(8):
                            ins = e.matmul(ps[b][:, 0:512], lhsT=g_(kc, mm * 128, 128), rhs=sTb[:, kc, :],
                                           start=(kc == 0), stop=(kc == 7))
                        return ins
                    PE(gmm, [key, "sTb"], [pk])
                    A(lambda e, mo=mo, b=b: e.activation(out=sg[0][:], in_=ps[b][:, 0:512], func=AF.Sigmoid,
                                                         bias=cols[:, 2, mo:mo + 1], scale=1.0), [pk, "cols"], ["sg0"])
                    V(lambda e, mo=mo: e.tensor_tensor(out=s2T[:, mo, :], in0=sTb[:, mo, :], in1=sg[0][:], op=ALU.mult),
                      ["sg0", "sTb"], ["s2T"])
            for jp in range(8):
                bk = [nbank() for _ in range(8)]
                pks = ["ps%d" % b for b in bk]
                gbp, kbp = w_piece(w_bp_v, 0, 8, 256 * jp, 256)

                def m_yp(e, bk=bk, gbp=gbp):
                    ins = None
                    for jj in range(2):
                        for kc in range(8):
                            ins = e.matmul(ps[bk[jj]][:, 0:512], lhsT=gbp(kc, jj * 128, 128), rhs=dT[:, kc, :], start=(kc == 0), stop=(kc == 7))
                    return ins
                PE(m_yp, [kbp, "dT"], pks[0:2])
                gbs, kbs = w_piece(w_bs_v, 0, 8, 256 * jp, 256)

                def m_ys(e, bk=bk, gbs=gbs):
                    ins = None
                    for jj in range(2):
                        for kc in range(8):
                            ins = e.matmul(ps[bk[2 + jj]][:, 0:512], lhsT=gbs(kc, jj * 128, 128), rhs=s2T[:, kc, :], start=(kc == 0), stop=(kc == 7))
                    return ins
                PE(m_ys, [kbs, "s2T"], pks[2:4])
                for gsel in range(2):
                    gg = [w_piece(w_in_v, 8 * hk, 8, 2048 * (gsel + 1) + 256 * jp, 256) for hk in range(2)]

                    def m_g(e, bk=bk, gg=gg, gsel=gsel):
                        ins = None
                        for jj in range(2):
                            for kc in range(KC):
                                ins = e.matmul(ps[bk[4 + 2 * gsel + jj]][:, 0:512], lhsT=gg[kc // 8][0](kc % 8, jj * 128, 128), rhs=uT[:, kc, :],
                                               start=(kc == 0), stop=(kc == KC - 1))
                        return ins
                    PE(m_g, [gg[0][1], gg[1][1]] + UTK, pks[4 + 2 * gsel:6 + 2 * gsel])
                for jj in range(2):
                    j = 2 * jp + jj
                    A(lambda e, bk=bk, jj=jj: e.activation(out=sg[0][:], in_=ps[bk[4 + jj]][:, 0:512], func=AF.Sigmoid), [pks[4 + jj]], ["sg0"])
                    A(lambda e, bk=bk, jj=jj: e.activation(out=sg[1][:], in_=ps[bk[6 + jj]][:, 0:512], func=AF.Sigmoid), [pks[6 + jj]], ["sg1"])
                    V(lambda e, bk=bk, jj=jj: e.tensor_tensor(out=sg[0][:], in0=sg[0][:], in1=ps[bk[jj]][:, 0:512], op=ALU.mult), ["sg0", pks[jj]], ["sg0"])
                    V(lambda e, bk=bk, jj=jj: e.tensor_tensor(out=sg[1][:], in0=sg[1][:], in1=ps[bk[2 + jj]][:, 0:512], op=ALU.mult), ["sg1", pks[2 + jj]], ["sg1"])
                    G(lambda e, j=j: e.tensor_tensor(out=mgT[:, j, :], in0=sg[0][:], in1=sg[1][:], op=ALU.add), ["sg0", "sg1"], ["mgT"])
            if tb == 0:
                dump("zp", zp, [128, 8, NH + 512], BF16, "zp")
                dump("dT", dT, [128, 8, 512], BF16, "dT")
                dump("s2T", s2T, [128, 8, 512], BF16, "s2T")
                dump("mgT", mgT, [128, KC, 512], BF16, "mgT")
            for tt in range(4):
                DM(lambda e, tt=tt: e.dma_start(out=XH[:, tt, :], in_=xs[NH + tok0 + 128 * tt:NH + tok0 + 128 * (tt + 1), :]),
                   [], ["xh%d" % tt])
            DM(lambda e: e.dma_start(out=gain[:], in_=gains_d[:, 1, :]), [], ["gains"])
            for nb in range(4):
                bk = [nbank() for _ in range(4)]
                pks = ["ps%d" % b for b in bk]
                pieces = [w_piece(w_out_v, 4 * kq, 4, 512 * nb, 512) for kq in range(4)]

                def omm2(e, bk=bk, pieces=pieces):
                    ins = None
                    for kq in range(4):
                        for tt in range(4):
                            for kl in range(4):
                                kc = 4 * kq + kl
                                ins = e.matmul(ps[bk[tt]][:, 0:512], lhsT=mgT[:, kc, 128 * tt:128 * (tt + 1)],
                                               rhs=pieces[kq][0](kl, 0, 512), start=(kc == 0), stop=(kc == KC - 1))
                    return ins
                PE(omm2, [p_[1] for p_ in pieces] + ["mgT"], pks)
                for tt in range(4):
                    V(lambda e, tt=tt, nb=nb, bk=bk: e.tensor_tensor(out=XH[:, tt, 512 * nb:512 * (nb + 1)], in0=XH[:, tt, 512 * nb:512 * (nb + 1)],
                                                                     in1=ps[bk[tt]][:, 0:512], op=ALU.add), [pks[tt], "xh%d" % tt], ["xh%d" % tt])
            for tt in range(4):
                ti = 4 * tb + tt
                hk = "xh%d" % tt
                ht = XH[:, tt, :]
                vb = ubs[tt % 2]
                vk = "vb%d" % (tt % 2)
                sq = sqs[tt % 2]
                sk = "sqb%d" % (tt % 2)
                DM(lambda e, ti=ti, ht=ht: e.dma_start(out=hs[128 * ti:128 * (ti + 1), :], in_=ht), [hk], ["hs"])
                A(lambda e, ht=ht, vb=vb, sq=sq: e.activation(out=vb[:], in_=ht, func=AF.Square, accum_out=sq[:, 0:1]), [hk], [vk, sk])
                V(lambda e, sq=sq: e.tensor_scalar(out=sq[:, 1:2], in0=sq[:, 0:1], scalar1=1.0 / D, scalar2=EPS, op0=ALU.mult, op1=ALU.add), [sk], [sk])
                A(lambda e, sq=sq: e.activation(out=sq[:, 1:2], in_=sq[:, 1:2], func=AF.Sqrt), [sk], [sk])
                V(lambda e, sq=sq: e.reciprocal(out=sq[:, 2:3], in_=sq[:, 1:2]), [sk], [sk])
                V(lambda e, ht=ht, sq=sq: e.scalar_tensor_tensor(out=ht, in0=ht, scalar=sq[:, 2:3], in1=gain[:], op0=ALU.mult, op1=ALU.mult),
                  [hk, sk, "gains"], [hk])
                G(lambda e, ht=ht, vb=vb: e.tensor_copy(out=vb[:], in_=ht), [hk], [vk])
                for qd in range(4):
                    b = nbank()
                    pk = "ps%d" % b

                    def vtr(e, qd=qd, b=b, ht=ht):
                        ins = None
                        for j in range(4):
                            kc = 4 * qd + j
                            ins = e.transpose(out=ps[b][:, j * 128:(j + 1) * 128], in_=ht[:, kc * 128:(kc + 1) * 128], identity=ident32)
                        return ins
                    PE(vtr, [hk, "cst"], [pk])
                    evac_copy(vT32[:, 4 * qd:4 * qd + 4, :], ps[b][:, 0:512].rearrange("p (k t) -> p k t", t=128), [pk], ["vT32"])
                b = nbank()
                pk = "ps%d" % b

                def rmm(e, b=b):
                    ins = None
                    for kc in range(KC):
                        ins = e.matmul(ps[b][:, 0:72], lhsT=vT32[:, kc, :], rhs=wr[:, kc, :], start=(kc == 0), stop=(kc == KC - 1))
                    return ins
                PE(rmm, ["vT32", "wr"], [pk])
                V(lambda e, b=b: e.tensor_tensor(out=lg[:], in0=ps[b][:, 0:72], in1=br[:], op=ALU.add), [pk, "br"], ["lg"])
                R = lambda i: rt[:, i, :]
                K_ = ["rt", "rs", "lg"]
                W_ = ["rt", "rs"]
                V(lambda e: e.reduce_max(out=rs[:, 0:1], in_=lg[:, 0:8], axis=AX.X), K_, W_)
                V(lambda e: e.tensor_scalar(out=R(0)[:, 0:8], in0=lg[:, 0:8], scalar1=rs[:, 0:1], scalar2=None, op0=ALU.is_equal), K_, W_)
                V(lambda e: e.tensor_scalar(out=rs[:, 1:2], in0=rs[:, 0:1], scalar1=-1.0, scalar2=None, op0=ALU.mult), K_, W_)
                A(lambda e: e.activation(out=R(1)[:, 0:8], in_=lg[:, 0:8], func=AF.Exp, bias=rs[:, 1:2], scale=1.0, accum_out=rs[:, 2:3]), K_, W_)
                V(lambda e: e.reciprocal(out=rs[:, 3:4], in_=rs[:, 2:3]), K_, W_)
                V(lambda e: e.tensor_scalar(out=R(1)[:, 0:8], in0=R(0)[:, 0:8], scalar1=1e30, scalar2=-1e30, op0=ALU.mult, op1=ALU.add), K_, W_)
                V(lambda e: e.tensor_tensor(out=R(2).rearrange("p (g x) -> p g x", x=8), in0=lg[:, 8:72].rearrange("p (g x) -> p g x", x=8),
                                            in1=R(1)[:, 0:8].unsqueeze(2).to_broadcast([128, 8, 8]), op=ALU.add), K_, W_)
                V(lambda e: e.reduce_max(out=rs[:, 4:5], in_=R(2), axis=AX.X), K_, W_)
                V(lambda e: e.tensor_scalar(out=R(3), in0=R(2), scalar1=rs[:, 4:5], scalar2=None, op0=ALU.is_equal), K_, W_)
                V(lambda e: e.scalar_tensor_tensor(out=R(4), in0=R(3), scalar=-1e30, in1=R(2), op0=ALU.mult, op1=ALU.add), K_, W_)
                V(lambda e: e.reduce_max(out=rs[:, 5:6], in_=R(4), axis=AX.X), K_, W_)
                V(lambda e: e.tensor_scalar(out=R(5), in0=R(4), scalar1=rs[:, 5:6], scalar2=None, op0=ALU.is_equal), K_, W_)
                V(lambda e: e.tensor_tensor(out=rs[:, 6:7], in0=rs[:, 5:6], in1=rs[:, 4:5], op=ALU.subtract), K_, W_)
                A(lambda e: e.activation(out=rs[:, 6:7], in_=rs[:, 6:7], func=AF.Exp), K_, W_)
                V(lambda e: e.tensor_scalar(out=rs[:, 6:7], in0=rs[:, 6:7], scalar1=1.0, scalar2=None, op0=ALU.add), K_, W_)
                V(lambda e: e.reciprocal(out=rs[:, 7:8], in_=rs[:, 6:7]), K_, W_)
                V(lambda e, ti=ti: e.tensor_tensor(out=gw[:, ti, 0:1], in0=rs[:, 7:8], in1=rs[:, 3:4], op=ALU.mult), K_, W_ + ["gw"])
                V(lambda e, ti=ti: e.tensor_tensor(out=gw[:, ti, 1:2], in0=rs[:, 3:4], in1=gw[:, ti, 0:1], op=ALU.subtract), K_ + ["gw"], W_ + ["gw"])
                V(lambda e: e.tensor_tensor(out=mb[:], in0=R(3), in1=R(5), op=ALU.add), K_, ["mb"])
                b = nbank()
                pk = "ps%d" % b

                def cmm(e, b=b):
                    e.matmul(ps[b][:, 0:64], lhsT=trib, rhs=mb[:], start=True, stop=True)
                    return e.matmul(ps[b][:, 64:128], lhsT=onesb, rhs=mb[:], start=True, stop=True)
                PE(cmm, ["mb", "cstb"], [pk])
                V(lambda e, b=b: e.tensor_tensor(out=R(6), in0=ps[b][:, 0:64], in1=base[:], op=ALU.add), [pk, "base"] + K_, W_)
                V(lambda e, b=b: e.tensor_tensor(out=base[:], in0=base[:], in1=ps[b][:, 64:128], op=ALU.add), [pk, "base"] + K_, ["base"])
                for k_ in range(2):
                    oh = R(3) if k_ == 0 else R(5)
                    V(lambda e, oh=oh: e.tensor_tensor(out=R(7), in0=oh, in1=R(6), op=ALU.mult), K_, W_)
                    V(lambda e: e.reduce_sum(out=rs[:, 8:9], in_=R(7), axis=AX.X), K_, W_)
                    V(lambda e: e.tensor_scalar(out=rs[:, 8:9], in0=rs[:, 8:9], scalar1=float(CAP - 1), scalar2=None, op0=ALU.min), K_, W_)
                    V(lambda e, oh=oh: e.tensor_tensor(out=R(7), in0=oh, in1=iota_e, op=ALU.mult), K_ + ["cst"], W_)
                    V(lambda e: e.reduce_sum(out=rs[:, 9:10], in_=R(7), axis=AX.X), K_, W_)
                    V(lambda e: e.scalar_tensor_tensor(out=rs[:, 10:11], in0=rs[:, 9:10], scalar=float(CAP), in1=rs[:, 8:9],
                                                       op0=ALU.mult, op1=ALU.add), K_, W_)
                    V(lambda e, ti=ti, k_=k_: e.tensor_copy(out=gidx[:, ti, k_:k_ + 1], in_=rs[:, 10:11]), K_, ["gi"])
                    S.dma(lambda e, ti=ti, k_=k_, vb=vb: e.indirect_dma_start(
                        out=xg, out_offset=bass.IndirectOffsetOnAxis(ap=gidx[:, ti, k_:k_ + 1], axis=0), in_=vb[:], in_offset=None),
                        ["gi", vk, "xg"], ["xg%d" % (ti * 2 + k_)], q="pool")
        dump("gw", gw, [128, 16, 2], F32, "gw")
        dump("lg", lg, [128, 72], F32, "lg")
        S.barrier()
        pbk.close()

        if stage >= 3:
            pc = ExitStack()
            NR2 = 8
            ring2 = [T(pc, "wrg%d" % i, [128, 4096]) for i in range(NR2)]
            r2_i = [0]
            Xe = [T(pc, "Xe%d" % i, [128, D], BF16) for i in range(2)]
            XeT = [T(pc, "XeT%d" % i, [128, KC, 128], BF16) for i in range(2)]
            hg = T(pc, "hg", [128, 512])
            hh = T(pc, "hh", [128, 512], BF16)
            hT = T(pc, "hT", [128, 4, 128], BF16)
            Ye = [T(pc, "Ye%d" % i, [128, D]) for i in range(2)]

            def piece2(src_ap, nk, ncols):
                i = r2_i[0] % NR2
                r2_i[0] += 1
                key = "wrg%d" % i
                dst = ring2[i]
                DM(lambda e: e.dma_start(out=dst[:, 0:nk * ncols].rearrange("p (k n) -> p k n", n=ncols), in_=src_ap), [], [key])
                return (lambda kl, c, n: hi(dst[:, kl * ncols + c:kl * ncols + c + n])), key

            for ex in range(NE):
                xe = Xe[ex % 2]
                xk = "Xe%d" % (ex % 2)
                xT = XeT[ex % 2]
                xtk = "XeT%d" % (ex % 2)
                DM(lambda e, ex=ex, xe=xe: e.dma_start(out=xe[:], in_=xg[ex * CAP:(ex + 1) * CAP, :]),
                   ["xg"] + ["xg%d" % i for i in range(32)], [xk])
                wg_v = w_gate[ex].rearrange("(p k) f -> p k f", k=KC)
                wu_v = w_up[ex].rearrange("(p k) f -> p k f", k=KC)
                wd_v = w_down[ex].rearrange("(p k) n -> p k n", k=4)
                pg_ = [piece2(wg_v[:, 8 * h_:8 * h_ + 8, :], 8, 512) for h_ in range(2)]
                pu_ = [piece2(wu_v[:, 8 * h_:8 * h_ + 8, :], 8, 512) for h_ in range(2)]
                pd_ = [piece2(wd_v[:, 2 * h_:2 * h_ + 2, :], 2, 2048) for h_ in range(2)]
                for half in range(2):
                    b = nbank()
                    pk = "ps%d" % b
                    psb = ps[b][:].bitcast(BF16)

                    def xtr(e, half=half, psb=psb, xe=xe):
                        ins = None
                        for j in range(8):
                            kc = half * 8 + j
                            ins = e.transpose(out=psb[:, j * 128:(j + 1) * 128], in_=xe[:, kc:D:KC], identity=identb)
                        return ins
                    PE(xtr, [xk, "cstb"], [pk])
                    evac_copy(xT[:, half * 8:half * 8 + 8, :], psb[:, 0:1024].rearrange("p (k t) -> p k t", t=128), [pk], [xtk])
                bg, bu = nbank(), nbank()

                def gu(e, bg=bg, bu=bu, xT=xT, pg_=pg_, pu_=pu_):
                    ins = None
                    for kc in range(KC):
                        ins = e.matmul(ps[bg][:, 0:512], lhsT=xT[:, kc, :], rhs=pg_[kc // 8][0](kc % 8, 0, 512), start=(kc == 0), stop=(kc == KC - 1))
                    for kc in range(KC):
                        ins = e.matmul(ps[bu][:, 0:512], lhsT=xT[:, kc, :], rhs=pu_[kc // 8][0](kc % 8, 0, 512), start=(kc == 0), stop=(kc == KC - 1))
                    return ins
                PE(gu, [xtk] + [p_[1] for p_ in pg_ + pu_], ["ps%d" % bg, "ps%d" % bu])
                A(lambda e, bg=bg: e.activation(out=hg[:], in_=ps[bg][:, 0:512], func=AF.Silu), ["ps%d" % bg], ["hg"])
                V(lambda e, bu=bu: e.tensor_tensor(out=hh[:], in0=hg[:], in1=ps[bu][:, 0:512], op=ALU.mult), ["hg", "ps%d" % bu], ["hh"])
                b = nbank()
                pk = "ps%d" % b
                psb = ps[b][:].bitcast(BF16)

                def htr(e, psb=psb):
                    ins = None
                    for j in range(4):
                        ins = e.transpose(out=psb[:, j * 128:(j + 1) * 128], in_=hh[:, j:512:4], identity=identb)
                    return ins
                PE(htr, ["hh", "cstb"], [pk])
                evac_copy(hT[:], psb[:, 0:512].rearrange("p (k t) -> p k t", t=128), [pk], ["hT"])
                ye = Ye[ex % 2]
                yk = "Ye%d" % (ex % 2)
                for nb in range(4):
                    b = nbank()
                    pk = "ps%d" % b

                    def dmm(e, nb=nb, b=b, pd_=pd_):
                        ins = None
                        for kc in range(4):
                            ins = e.matmul(ps[b][:, 0:512], lhsT=hT[:, kc, :], rhs=pd_[kc // 2][0](kc % 2, nb * 512, 512),
                                           start=(kc == 0), stop=(kc == 3))
                        return ins
                    PE(dmm, ["hT", pd_[0][1], pd_[1][1]], [pk])
                    evac_copy(ye[:, nb * 512:(nb + 1) * 512], ps[b][:, 0:512], [pk], [yk])
                DM(lambda e, ex=ex, ye=ye: e.dma_start(out=ysc[ex * CAP:(ex + 1) * CAP, :], in_=ye[:]), [yk], ["ysc"])
            S.barrier()
            pc.close()

        pd = ExitStack()
        gfin = T(pd, "gfin", [128, D])
        DM(lambda e: e.dma_start(out=gfin[:], in_=gains_d[:, 2, :]), [], ["gfin"])
        hts = [T(pd, "hD%d" % i, [128, D]) for i in range(2)]
        g1s = [T(pd, "g1_%d" % i, [128, D]) for i in range(2)]
        g2s = [T(pd, "g2_%d" % i, [128, D]) for i in range(2)]
        jnk = T(pd, "jnk", [128, D], BF16)
        sqd = [T(pd, "sqd%d" % i, [128, 4]) for i in range(2)]
        for ti in range(4 * NBLK):
            i = ti % 2
            ht, g1, g2, sq = hts[i], g1s[i], g2s[i], sqd[i]
            hk, k1, k2, sk = "hD%d" % i, "g1_%d" % i, "g2_%d" % i, "sqd%d" % i
            DM(lambda e, ti=ti, ht=ht: e.dma_start(out=ht[:], in_=hs[128 * ti:128 * (ti + 1), :]), ["hs"], [hk])
            if stage >= 3:
                S.dma(lambda e, ti=ti, g1=g1: e.indirect_dma_start(out=g1[:], out_offset=None, in_=ysc,
                      in_offset=bass.IndirectOffsetOnAxis(ap=gidx[:, ti, 0:1], axis=0)), ["ysc", "gi"], [k1], q="pool")
                S.dma(lambda e, ti=ti, g2=g2: e.indirect_dma_start(out=g2[:], out_offset=None, in_=ysc,
                      in_offset=bass.IndirectOffsetOnAxis(ap=gidx[:, ti, 1:2], axis=0)), ["ysc", "gi"], [k2], q="pool")
                V(lambda e, ti=ti, ht=ht, g1=g1: e.scalar_tensor_tensor(out=ht[:], in0=g1[:], scalar=gw[:, ti, 0:1], in1=ht[:],
                                                                        op0=ALU.mult, op1=ALU.add), [hk, k1, "gw"], [hk])
                V(lambda e, ti=ti, ht=ht, g2=g2: e.scalar_tensor_tensor(out=ht[:], in0=g2[:], scalar=gw[:, ti, 1:2], in1=ht[:],
                                                                        op0=ALU.mult, op1=ALU.add), [hk, k2, "gw"], [hk])
            A(lambda e, ht=ht, sq=sq: e.activation(out=jnk[:], in_=ht[:], func=AF.Square, accum_out=sq[:, 0:1]), [hk], ["jnk", sk])
            V(lambda e, sq=sq: e.tensor_scalar(out=sq[:, 1:2], in0=sq[:, 0:1], scalar1=1.0 / D, scalar2=EPS, op0=ALU.mult, op1=ALU.add), [sk], [sk])
            A(lambda e, sq=sq: e.activation(out=sq[:, 1:2], in_=sq[:, 1:2], func=AF.Sqrt), [sk], [sk])
            V(lambda e, sq=sq: e.reciprocal(out=sq[:, 2:3], in_=sq[:, 1:2]), [sk], [sk])
            V(lambda e, ht=ht, sq=sq: e.scalar_tensor_tensor(out=ht[:], in0=ht[:], scalar=sq[:, 2:3], in1=gfin[:], op0=ALU.mult, op1=ALU.mult),
              [hk, sk, "gfin"], [hk])
            DM(lambda e, ti=ti, ht=ht: e.dma_start(out=out_d[128 * ti:128 * (ti + 1), :], in_=ht[:]), [hk], ["out"], is_out=True)
        S.barrier()
        pd.close()
        S.emit()
    return nc


def build_rest(nc, st, S, L):
    pass


def _consts():
    c = np.zeros((128, 1024), np.float32)
    c[:, 0:128] = np.eye(128, dtype=np.float32)
    k = np.arange(128)
    c[:, 128:256] = (k[:, None] < k[None, :]).astype(np.float32)
    c[:, 256:384] = 1.0
    c[:, 384:384 + 259] = np.arange(259, dtype=np.float32)[None, :]
    c[:, 648:712] = np.arange(64, dtype=np.float32)[None, :]
    for j in range(2):
        c[:, 712 + j] = ((k // 16) % 2 == j)
        c[:, 718 + j] = (k // 64 == j)
    for j in range(4):
        c[:, 714 + j] = (k // 32 == j)
    c[:, 720] = k
    return c


def _prep(inp, stage=3):
    f = lambda a: np.ascontiguousarray(np.asarray(a, dtype=np.float32))
    x = f(inp["x"]); meta = f(inp["meta"])
    a_re = f(inp["ssm_a_re"])[0]; a_im = f(inp["ssm_a_im"])[0]; ldt = f(inp["ssm_log_dt"])[0]
    b_re = f(inp["ssm_b_re"])[0]; b_im = f(inp["ssm_b_im"])[0]
    c_re = f(inp["ssm_c_re"])[0]; c_im = f(inp["ssm_c_im"])[0]
    shared = {}
    shared["w_in"] = f(inp["w_in"])[0]
    shared["pool_w"] = f(inp["pool_w"])[0]
    shared["w_glu"] = f(inp["w_glu"])[0]
    shared["w_bp"] = f(inp["w_branch_pool"])[0]
    shared["w_bs"] = f(inp["w_branch_ssm"])[0]
    shared["w_out"] = f(inp["w_out"])[0]
    if stage >= 3:
        shared["w_gate"] = f(inp["w_gate"])[0]
        shared["w_up"] = f(inp["w_up"])[0]
        shared["w_down"] = f(inp["w_down"])[0]
    g = np.stack([f(inp["norm_mix"])[0], f(inp["norm_ffn"])[0], f(inp["norm_final"])], 0)
    shared["gains"] = np.ascontiguousarray(np.broadcast_to(g[None], (128, 3, D)))
    cols = np.stack([f(inp["ssm_d"])[0].reshape(8, 128).T, f(inp["pool_scale"])[0].reshape(8, 128).T,
                     f(inp["b_glu"])[0].reshape(8, 128).T], 1)
    shared["cols"] = np.ascontiguousarray(cols)
    wr = np.concatenate([f(inp["w_router_group"])[0], f(inp["w_router_expert"])[0]], 1)
    shared["wr"] = np.ascontiguousarray(wr.reshape(KC, 128, 72).transpose(1, 0, 2))
    br = np.concatenate([f(inp["b_router_group"])[0], f(inp["b_router_expert"])[0]], 0)
    shared["br"] = np.ascontiguousarray(np.broadcast_to(br[None], (128, 72)))
    def L1(a):
        t = a.reshape(8, 8, 64)
        t = np.broadcast_to(t[:, :, None, :], (8, 8, 16, 64))
        return t.transpose(1, 2, 0, 3).reshape(128, 8, 64)
    ldt_b = np.broadcast_to(ldt[:, None], (64, 64))
    shared["aL1"] = np.ascontiguousarray(np.stack([L1(a_re), L1(a_im), L1(ldt_b)], 1))
    def B1(b):
        t = b.reshape(8, 8, 64, 16)
        return t.transpose(1, 3, 0, 2).reshape(128, 8, 64)
    shared["bL1"] = np.ascontiguousarray(np.stack([B1(b_re), B1(b_im)], 1))
    def L2(a):
        return a.reshape(32, 2, 64).transpose(1, 2, 0).reshape(128, 32)
    shared["aL2"] = np.ascontiguousarray(np.stack([L2(a_re), L2(a_im), L2(ldt_b)], 1))
    def B2(b):
        return b.reshape(32, 2, 64, 16).transpose(1, 2, 0, 3).reshape(128, 32, 16)
    shared["bL2"] = np.ascontiguousarray(np.stack([B2(b_re), B2(b_im)], 1))
    def C2(c):
        return c.reshape(32, 2, 16, 64).transpose(1, 3, 0, 2).reshape(128, 32, 16)
    shared["cL2"] = np.ascontiguousarray(np.stack([C2(c_re), C2(c_im)], 1))
    shared["cst"] = _consts()
    maps = []
    for c in range(8):
        b, k = c // 4, c % 4
        halo = meta if k == 0 else x[b, NT * k - NH:NT * k]
        m = dict(shared)
        m["xs"] = np.ascontiguousarray(np.concatenate([halo, x[b, NT * k:NT * (k + 1)]], 0))
        m["hm"] = np.full((128, 1), 1.0 if k == 0 else 0.0, np.float32)
        cmv = np.zeros((4, 3), np.float32)
        for j in range(k):
            cmv[j, k - 1 - j] = 1.0
        m["cm"] = np.ascontiguousarray(np.broadcast_to(cmv.reshape(1, 12), (128, 12)))
        maps.append(m)
    return maps


_NC_CACHE = {}


def kernel(**inputs):
    if "nc" not in _NC_CACHE:
        _NC_CACHE["nc"] = build(False, 3)
    nc = _NC_CACHE["nc"]
    maps = _prep(inputs, 3)
    res = run_bass_kernel_spmd(nc, maps, core_ids=list(range(8)))
    out = np.empty((2, 8192, D), np.float32)
    for c in range(8):
        b, k = c // 4, c % 4
        out[b, NT * k:NT * (k + 1)] = res.results[c]["out"]
    return out
```

```python
import math
from contextlib import ExitStack

import numpy as np
import concourse.bass as bass
import concourse.mybir as mybir
from concourse.bass_utils import run_bass_kernel_spmd

F32 = mybir.dt.float32
BF16 = mybir.dt.bfloat16
I32 = mybir.dt.int32
ALU = mybir.AluOpType
AF = mybir.ActivationFunctionType
AX = mybir.AxisListType

D = 2048
KC = 16
NT = 2048
NH = 16
NTOK = NT + NH
TCH = 8
NCH = NTOK // TCH
NE = 64
CAP = 128
EPS = 1e-6
MAGIC = 12582912.0
TWO_PI = 2.0 * math.pi
DEBUG = {}


class Sched:
    ENGS = ("pe", "act", "dve", "pool", "sp")
    NDMA = 14

    def __init__(self, nc, stack):
        self.nc = nc
        self.ops = {e: [] for e in self.ENGS}
        self.cnt = {e: 0 for e in self.ENGS}
        self.sem = {e: stack.enter_context(nc.semaphore("s_" + e)) for e in self.ENGS}
        self.dsem = {
            q: [stack.enter_context(nc.semaphore(f"d_{q}{i}")) for i in range(self.NDMA)]
            for q in ("sp", "pool")
        }
        self.ccsem = stack.enter_context(nc.semaphore("cc"))
        self.dcnt = {"sp": 0, "pool": 0}
        self.known = {e: {} for e in self.ENGS}
        self.last_w = {}
        self.readers = {}
        self.out_tokens = []
        self.all_tokens = {}
        self.block = stack.enter_context(nc.Block())
        self.eobj = {"pe": nc.tensor, "act": nc.scalar, "dve": nc.vector, "pool": nc.gpsimd, "sp": nc.sync}

    def _emit(self, eng, waits, fn, sem, inc):
        e = self.eobj[eng]
        for (s_, v) in waits:
            e.wait_ge(s_, v)
        if fn is not None:
            fn(e).then_inc(sem, inc)

    def _deps(self, eng, reads, writes):
        toks = []
        for k in reads:
            w = self.last_w.get(k)
            if w is not None:
                toks.append(w)
        for k in writes:
            w = self.last_w.get(k)
            if w is not None:
                toks.append(w)
            toks.extend(self.readers.get(k, ()))
        waits = {}
        for (sem, val, src) in toks:
            if src == "pe" and eng == "pe":
                continue
            key = id(sem)
            if self.known[eng].get(key, 0) >= val:
                continue
            if key not in waits or waits[key][1] < val:
                waits[key] = (sem, val)
        for key, (sem, val) in waits.items():
            self.known[eng][key] = val
        return list(waits.values())

    def _record(self, tok, reads, writes):
        self.all_tokens[id(tok[0])] = (tok[0], max(tok[1], self.all_tokens.get(id(tok[0]), (None, 0))[1]))
        for k in writes:
            self.last_w[k] = tok
            self.readers[k] = []
        for k in reads:
            self.readers.setdefault(k, []).append(tok)

    def op(self, eng, fn, reads=(), writes=()):
        waits = self._deps(eng, reads, writes)
        self.cnt[eng] += 1
        tok = (self.sem[eng], self.cnt[eng], eng)
        self._emit(eng, waits, fn, self.sem[eng], 1)
        self._record(tok, reads, writes)
        return tok

    def dma(self, fn, reads=(), writes=(), q="sp", is_out=False):
        waits = self._deps(q, reads, writes)
        i = self.dcnt[q]
        self.dcnt[q] += 1
        slot = i % self.NDMA
        rnd = i // self.NDMA
        sem = self.dsem[q][slot]
        if rnd > 0:
            key = id(sem)
            if self.known[q].get(key, 0) < 16 * rnd:
                waits.append((sem, 16 * rnd))
                self.known[q][key] = 16 * rnd
        tok = (sem, 16 * (rnd + 1), "dma")
        self._emit(q, waits, fn, sem, 16)
        self._record(tok, reads, writes)
        if is_out:
            self.out_tokens.append(tok)
        return tok

    def collective(self, fn, reads=(), writes=()):
        waits = self._deps("pool", reads, writes)
        tok = (self.ccsem, 1, "dma")
        self._emit("pool", waits, fn, self.ccsem, 1)
        self._record(tok, reads, writes)
        return tok

    def barrier(self):
        for e in self.ENGS:
            waits = []
            for key, (sem, val) in self.all_tokens.items():
                if self.known[e].get(key, 0) < val:
                    if e == "pe" and sem is self.sem["pe"]:
                        continue
                    waits.append((sem, val))
                    self.known[e][key] = val
            if waits:
                self._emit(e, waits, None, None, 0)
        self.last_w = {}
        self.readers = {}

    def emit(self):
        final = {}
        for (sem, val, _) in self.out_tokens:
            if id(sem) not in final or final[id(sem)][1] < val:
                final[id(sem)] = (sem, val)
        for (s_, v) in final.values():
            self.eobj["sp"].wait_ge(s_, v)


def hi(ap):
    return ap.bitcast(BF16)[:, 1::2]


def build(debug=False, stage=3, dev=None):
    dev = dev or {}
    NBLK = dev.get("nblk", 4)
    nc = bass.Bass("TRN2", target_bir_lowering=False)

    def din(name, shape, dt=F32):
        return nc.dram_tensor(name, list(shape), dt, kind="ExternalInput").ap()

    def dscr(name, shape, dt=F32):
        return nc.dram_tensor(name, list(shape), dt, kind="Internal").ap()

    xs = din("xs", [NTOK, D])
    hm_d = din("hm", [128, 1])
    cm_d = din("cm", [128, 12])
    w_in = din("w_in", [D, 6144])
    pool_w = din("pool_w", [4, 256, 256])
    w_glu = din("w_glu", [1024, 1024])
    w_bp = din("w_bp", [1024, D])
    w_bs = din("w_bs", [1024, D])
    w_out = din("w_out", [D, D])
    if stage >= 3:
        w_gate = din("w_gate", [NE, D, 512])
        w_up = din("w_up", [NE, D, 512])
        w_down = din("w_down", [NE, 512, D])
    gains_d = din("gains", [128, 3, D])
    cols_d = din("cols", [128, 3, 8])
    wr_d = din("wr", [128, KC, 72])
    br_d = din("br", [128, 72])
    aL1_d = din("aL1", [128, 3, 8, 64])
    bL1_d = din("bL1", [128, 2, 8, 64])
    aL2_d = din("aL2", [128, 3, 32])
    bL2_d = din("bL2", [128, 2, 32, 16])
    cL2_d = din("cL2", [128, 2, 32, 16])
    cst_d = din("cst", [128, 1024])
    out_d = nc.dram_tensor("out", [NT, D], F32, kind="ExternalOutput").ap()

    cc_in = dscr("cc_in", [128, 64])
    cc_out = dscr("cc_out", [4 * 128, 64])
    hs = dscr("hs", [NT, D])
    xg = dscr("xg", [NE * CAP + 128, D], BF16)
    ysc = dscr("ysc", [NE * CAP + 128, D])
    sTd = dscr("sTd", [128, 8 * NT], BF16)
    with ExitStack() as st:
        S = Sched(nc, st)

        def dump(name, tile_, shape, dt, key):
            if not debug:
                return
            d_ap = nc.dram_tensor("dbg_" + name, list(shape), dt, kind="ExternalOutput").ap()
            S.dma(lambda e: e.dma_start(out=d_ap, in_=tile_[:]), [key], [], is_out=True)

        def V(fn, r=(), w=()): return S.op("dve", fn, r, w)
        def A(fn, r=(), w=()): return S.op("act", fn, r, w)
        def G(fn, r=(), w=()): return S.op("pool", fn, r, w)
        def PE(fn, r=(), w=()): return S.op("pe", fn, r, w)
        def DM(fn, r=(), w=(), **kw): return S.dma(fn, r, w, **kw)

        def T(stk, name, shape, dt=F32):
            return stk.enter_context(nc.sbuf_tensor("sb_" + name, list(shape), dt))

        ps = [st.enter_context(nc.psum_tensor(f"ps{i}", [128, 512], F32)) for i in range(8)]
        ps_rr = [0]

        def nbank():
            i = ps_rr[0]
            ps_rr[0] = (i + 1) % 8
            return i

        cst = T(st, "cst", [128, 1024])
        cstb = T(st, "cstb", [128, 512], BF16)
        cols = T(st, "cols", [128, 3, 8])
        hm = T(st, "hm", [128, 1])
        cm = T(st, "cm", [128, 12])
        DM(lambda e: e.dma_start(out=cst[:], in_=cst_d), w=["cst"])
        DM(lambda e: e.dma_start(out=cols[:], in_=cols_d), w=["cols"])
        DM(lambda e: e.dma_start(out=hm[:], in_=hm_d), w=["hm"])
        DM(lambda e: e.dma_start(out=cm[:], in_=cm_d), w=["cm"])
        V(lambda e: e.tensor_copy(out=cstb[:, 0:384], in_=cst[:, 0:384]), ["cst"], ["cstb"])
        ident32 = cst[:, 0:128]
        identb = cstb[:, 0:128]
        trib = cstb[:, 128:256]
        onesb = cstb[:, 256:384]
        iota_c = cst[:, 384:384 + 259]
        iota_e = cst[:, 648:712]
        mg2 = [cst[:, 712:713], cst[:, 713:714]]
        mqd = [cst[:, 714 + j:715 + j] for j in range(4)]
        mh2 = [cst[:, 718:719], cst[:, 719:720]]
        iota_p = cst[:, 720:721]

        gw = T(st, "gw", [128, 16, 2])
        gidx = T(st, "gidx", [128, 16, 2], I32)
        zs = None

        def load_norm_T(stk_tiles, row0, nrows, gain, uT_ap, col0, tag):
            xt, ub, ssq, kx, ku = stk_tiles
            DM(lambda e: e.dma_start(out=xt[:nrows, :], in_=xs[row0:row0 + nrows, :]), [], [kx])
            A(lambda e: e.activation(out=ub[:nrows, :], in_=xt[:nrows, :], func=AF.Square, accum_out=ssq[:nrows, 0:1]), [kx], [ku, ku + "s"])
            V(lambda e: e.tensor_scalar(out=ssq[:nrows, 1:2], in0=ssq[:nrows, 0:1], scalar1=1.0 / D, scalar2=EPS,
                                        op0=ALU.mult, op1=ALU.add), [ku + "s"], [ku + "s"])
            A(lambda e: e.activation(out=ssq[:nrows, 1:2], in_=ssq[:nrows, 1:2], func=AF.Sqrt), [ku + "s"], [ku + "s"])
            V(lambda e: e.reciprocal(out=ssq[:nrows, 2:3], in_=ssq[:nrows, 1:2]), [ku + "s"], [ku + "s"])
            V(lambda e: e.scalar_tensor_tensor(out=ub[:nrows, :], in0=xt[:nrows, :], scalar=ssq[:nrows, 2:3],
                                               in1=gain[:nrows, :], op0=ALU.mult, op1=ALU.mult),
              [kx, ku + "s", "gains", ku], [ku])
            for half in range(2):
                b = nbank()
                pk = "ps%d" % b
                psb = ps[b][:].bitcast(BF16)

                def tr(e, half=half, psb=psb):
                    ins = None
                    for j in range(8):
                        kc = half * 8 + j
                        ins = e.transpose(out=psb[:, j * 128:j * 128 + nrows], in_=ub[:nrows, kc * 128:(kc + 1) * 128],
                                          identity=identb[:nrows, :nrows])
                    return ins
                PE(tr, [ku, "cstb"], [pk])
                eng = A if half == 0 else V
                if half == 0:
                    A(lambda e, half=half, psb=psb: e.activation(
                        out=uT_ap[:, half * 8:half * 8 + 8, col0:col0 + nrows],
                        in_=psb[:, 0:1024].rearrange("p (k t) -> p k t", t=128)[:, :, 0:nrows], func=AF.Copy), [pk], tag if isinstance(tag, list) else [tag])
                else:
                    V(lambda e, half=half, psb=psb: e.tensor_copy(
                        out=uT_ap[:, half * 8:half * 8 + 8, col0:col0 + nrows],
                        in_=psb[:, 0:1024].rearrange("p (k t) -> p k t", t=128)[:, :, 0:nrows]), [pk], tag if isinstance(tag, list) else [tag])


        if dev.get("skip_ssm"):
            sT_in = nc.dram_tensor("sT_in", [128, 8, NT], BF16, kind="ExternalInput").ap()
            with nc.allow_non_contiguous_dma(reason="dev"):
                pass
            DM(lambda e: e.dma_start(out=sTd.rearrange("p (a t) -> p a t", a=8), in_=sT_in), [], ["sTd"])
        else:
            ssm_stack = ExitStack()
            sT = T(ssm_stack, "sT", [128, 8, NT], BF16)
            zs = T(ssm_stack, "zs", [128, 8, 8, NCH], BF16)

            with ExitStack() as a1:
                gmix = T(a1, "gmix", [128, D])
                DM(lambda e: e.dma_start(out=gmix[:], in_=gains_d[:, 0, :]), w=["gains"])
                wss = T(a1, "wss", [128, KC, 1024])
                w_in_v = w_in.rearrange("(k p) n -> p k n", p=128)
                for j in range(8):
                    DM(lambda e, j=j: e.dma_start(out=wss[:, 2 * j:2 * j + 2, :], in_=w_in_v[:, 2 * j:2 * j + 2, 1024:2048]), [], ["wss"])
                xts = [T(a1, "xt%d" % i, [128, D]) for i in range(2)]
                ubs = [T(a1, "ub%d" % i, [128, D], BF16) for i in range(2)]
                sqs = [T(a1, "sq%d" % i, [128, 4]) for i in range(2)]
                uTs = [T(a1, "uT%d" % i, [128, KC, 512], BF16) for i in range(2)]
                tile_i = [0]

                def norm_tiles():
                    i = tile_i[0] % 2
                    tile_i[0] += 1
                    return (xts[i], ubs[i], sqs[i], "xt%d" % i, "ub%d" % i)

                blocks = [(0, NH)] + [(NH + 512 * b, 512) for b in range(4)]
                for bi, (c0, n) in enumerate(blocks):
                    uT = uTs[bi % 2]
                    tag = "uT%d" % (bi % 2)
                    for t0 in range(0, n, 128):
                        nr = min(128, n - t0)
                        load_norm_T(norm_tiles(), c0 + t0, nr, gmix, uT, t0, tag)
                    for m in range(8):
                        b = nbank()
                        pk = "ps%d" % b

                        def zmm(e, m=m, b=b, uT=uT, n=n):
                            ins = None
                            for kc in range(KC):
                                ins = e.matmul(ps[b][:, 0:n], lhsT=hi(wss[:, kc, m * 128:(m + 1) * 128]), rhs=uT[:, kc, 0:n],
                                               start=(kc == 0), stop=(kc == KC - 1))
                            return ins
                        PE(zmm, ["wss", tag], [pk])
                        ch0 = c0 // TCH
                        nchk = n // TCH
                        src = ps[b][:, 0:n].rearrange("p (c s) -> p s c", s=TCH)
                        dst = zs[:, m, :, ch0:ch0 + nchk]
                        if bi == 0:
                            V(lambda e, src=src, dst=dst: e.tensor_scalar(out=dst, in0=src, scalar1=hm[:, 0:1], scalar2=None,
                                                                          op0=ALU.mult), [pk, "hm"], ["zs"])
                        elif m % 2 == 0:
                            A(lambda e, src=src, dst=dst: e.activation(out=dst, in_=src, func=AF.Copy), [pk], ["zs"])
                        else:
                            V(lambda e, src=src, dst=dst: e.tensor_copy(out=dst, in_=src), [pk], ["zs"])
            S.barrier()
            Ptab = T(ssm_stack, "Ptab", [128, 8, 8, 2, 128], BF16)
            Qtab = T(ssm_stack, "Qtab", [128, 32, 9, 2, 32], BF16)
            Ktab = T(ssm_stack, "Ktab", [128, 8, 8, 128], BF16)
            sml = T(ssm_stack, "sml", [128, 8, 32])
            Fst = T(ssm_stack, "Fst", [128, 32, 2])
            Xc = T(ssm_stack, "Xc", [128, 32, 2])

            t1 = ExitStack()
            if True:
                aL1 = T(t1, "aL1", [128, 3, 256])
                bL1 = T(t1, "bL1", [128, 2, 256])
                w1 = T(t1, "w1", [128, 12, 256])
                pw = T(t1, "pw", [128, 2, 2, 256])
                PP = T(t1, "PP", [128, 2, 2, 256])

                def lam_block(pref, akey, a_re, a_im, ldt, W, n, mults):
                    r = [pref + "w"]
                    k = pref + "w"
                    A(lambda e: e.activation(out=W[:, 4, :n], in_=ldt, func=AF.Exp), r + [akey], [k])
                    V(lambda e: e.tensor_tensor(out=W[:, 5, :n], in0=a_re, in1=W[:, 4, :n], op=ALU.mult), r + [akey], [k])
                    V(lambda e: e.tensor_tensor(out=W[:, 6, :n], in0=a_im, in1=W[:, 4, :n], op=ALU.mult), r + [akey], [k])

                    def expi(kk, o_mag, o_re, o_im):
                        A(lambda e: e.activation(out=W[:, 7, :n], in_=W[:, 5, :n], func=AF.Exp, scale=float(kk)), r, [k])
                        for (off, dst) in ((0.0, 8), (0.25, 9)):
                            V(lambda e, off=off, dst=dst: e.tensor_scalar(out=W[:, dst, :n], in0=W[:, 6, :n], scalar1=float(kk) / TWO_PI,
                                                                          scalar2=off, op0=ALU.mult, op1=ALU.add), r, [k])
                            V(lambda e, dst=dst: e.tensor_scalar(out=W[:, 10, :n], in0=W[:, dst, :n], scalar1=MAGIC, scalar2=None,
                                                                 op0=ALU.add), r, [k])
                            V(lambda e, dst=dst: e.tensor_scalar(out=W[:, 10, :n], in0=W[:, 10, :n], scalar1=-MAGIC, scalar2=None,
                                                                 op0=ALU.add), r, [k])
                            V(lambda e, dst=dst: e.tensor_tensor(out=W[:, dst, :n], in0=W[:, dst, :n], in1=W[:, 10, :n],
                                                                 op=ALU.subtract), r, [k])
                            A(lambda e, dst=dst: e.activation(out=W[:, dst, :n], in_=W[:, dst, :n], func=AF.Sin, scale=TWO_PI), r, [k])
                        if o_mag is not None:
                            V(lambda e: e.tensor_copy(out=o_mag, in_=W[:, 7, :n]), r, [k, pref + "o"])
                        V(lambda e: e.tensor_tensor(out=o_re, in0=W[:, 7, :n], in1=W[:, 9, :n], op=ALU.mult), r, [k, pref + "o"])
                        V(lambda e: e.tensor_tensor(out=o_im, in0=W[:, 7, :n], in1=W[:, 8, :n], op=ALU.mult), r, [k, pref + "o"])

                    expi(1, None, W[:, 0, :n], W[:, 1, :n])
                    for (kk, om, ore, oim) in mults:
                        expi(kk, om, ore, oim)
                    lam_block.expi = expi
                    V(lambda e: e.tensor_tensor(out=W[:, 7, :n], in0=a_re, in1=a_re, op=ALU.mult), r + [akey], [k])
                    V(lambda e: e.tensor_tensor(out=W[:, 8, :n], in0=a_im, in1=a_im, op=ALU.mult), r + [akey], [k])
                    V(lambda e: e.tensor_tensor(out=W[:, 7, :n], in0=W[:, 7, :n], in1=W[:, 8, :n], op=ALU.add), r, [k])
                    V(lambda e: e.reciprocal(out=W[:, 7, :n], in_=W[:, 7, :n]), r, [k])
                    V(lambda e: e.tensor_scalar(out=W[:, 8, :n], in0=W[:, 0, :n], scalar1=-1.0, scalar2=None, op0=ALU.add), r, [k])
                    V(lambda e: e.tensor_tensor(out=W[:, 9, :n], in0=W[:, 8, :n], in1=a_re, op=ALU.mult), r + [akey], [k])
                    V(lambda e: e.tensor_tensor(out=W[:, 10, :n], in0=W[:, 1, :n], in1=a_im, op=ALU.mult), r + [akey], [k])
                    V(lambda e: e.tensor_tensor(out=W[:, 9, :n], in0=W[:, 9, :n], in1=W[:, 10, :n], op=ALU.add), r, [k])
                    V(lambda e: e.tensor_tensor(out=W[:, 2, :n], in0=W[:, 9, :n], in1=W[:, 7, :n], op=ALU.mult), r, [k])
                    V(lambda e: e.tensor_tensor(out=W[:, 9, :n], in0=W[:, 1, :n], in1=a_re, op=ALU.mult), r + [akey], [k])
                    V(lambda e: e.tensor_tensor(out=W[:, 10, :n], in0=W[:, 8, :n], in1=a_im, op=ALU.mult), r + [akey], [k])
                    V(lambda e: e.tensor_tensor(out=W[:, 9, :n], in0=W[:, 9, :n], in1=W[:, 10, :n], op=ALU.subtract), r, [k])
                    V(lambda e: e.tensor_tensor(out=W[:, 3, :n], in0=W[:, 9, :n], in1=W[:, 7, :n], op=ALU.mult), r, [k])

                def cmul(o_re, o_im, a_re, a_im, b_re, b_im, t1_, t2_, rk, wk, neg_im=False):
                    V(lambda e: e.tensor_tensor(out=t1_, in0=a_re, in1=b_re, op=ALU.mult), rk, wk)
                    V(lambda e: e.tensor_tensor(out=t2_, in0=a_im, in1=b_im, op=ALU.mult), rk, wk)
                    V(lambda e: e.tensor_tensor(out=o_re, in0=t1_, in1=t2_, op=ALU.subtract), rk, wk)
                    V(lambda e: e.tensor_tensor(out=t1_, in0=a_re, in1=b_im, op=ALU.mult), rk, wk)
                    V(lambda e: e.tensor_tensor(out=t2_, in0=a_im, in1=b_re, op=ALU.mult), rk, wk)
                    if neg_im:
                        V(lambda e: e.scalar_tensor_tensor(out=o_im, in0=t1_, scalar=-1.0, in1=t2_, op0=ALU.mult, op1=ALU.subtract), rk, wk)
                    else:
                        V(lambda e: e.tensor_tensor(out=o_im, in0=t1_, in1=t2_, op=ALU.add), rk, wk)

                Bb = T(t1, "Bb", [128, 2, 256])
                tA = T(t1, "tA", [128, 2, 256])
                tB = T(t1, "tB", [128, 2, 256])
                for hb in range(2):
                    DM(lambda e, hb=hb: e.dma_start(out=aL1[:].rearrange("p a (g n) -> p a g n", g=4), in_=aL1_d[:, :, 4 * hb:4 * hb + 4, :]), [], ["aL1"])
                    DM(lambda e, hb=hb: e.dma_start(out=bL1[:].rearrange("p a (g n) -> p a g n", g=4), in_=bL1_d[:, :, 4 * hb:4 * hb + 4, :]), [], ["bL1"])
                    lam_block("L1", "aL1", aL1[:, 0, :], aL1[:, 1, :], aL1[:, 2, :], w1, 256, [])
                    expi1 = lam_block.expi
                    KL1 = ["L1w", "L1o", "bL1"]
                    cmul(Bb[:, 0, :], Bb[:, 1, :], w1[:, 2, :], w1[:, 3, :], bL1[:, 0, :], bL1[:, 1, :],
                         w1[:, 10, :], w1[:, 11, :], KL1, ["L1w", "Bb"])
                    bb_re = Bb[:, 0:1, :].to_broadcast([128, 2, 256])
                    bb_im = Bb[:, 1:2, :].to_broadcast([128, 2, 256])
                    for kq in range(4):
                        for j in range(2):
                            kk_ = 2 * kq + j
                            if kk_ == 0:
                                V(lambda e: e.memset(pw[:, 0, 0, :], 1.0), ["PP"], ["L1o"])
                                V(lambda e: e.memset(pw[:, 1, 0, :], 0.0), ["PP"], ["L1o"])
                            else:
                                expi1(kk_, None, pw[:, 0, j, :], pw[:, 1, j, :])
                        cmul(PP[:, 0], PP[:, 1], pw[:, 0], pw[:, 1], bb_re, bb_im, tA[:], tB[:], KL1 + ["tAB", "Bb"], ["PP", "tAB"])
                        for ri in range(2):
                            for g2 in range(2):
                                V(lambda e, ri=ri, g2=g2, kq=kq, hb=hb: e.tensor_scalar(
                                    out=Ptab[:, 4 * hb:4 * hb + 4, 2 * kq:2 * kq + 2, ri, g2 * 64:(g2 + 1) * 64],
                                    in0=PP[:, ri].rearrange("p k (g n) -> p g k n", g=4),
                                    scalar1=mg2[g2], scalar2=None, op0=ALU.mult), ["PP", "cst"], ["Ptab"])
            S.barrier()
            t1.close()

            with ExitStack() as t2:
                aL2 = T(t2, "aL2", [128, 3, 32])
                bL2 = T(t2, "bL2", [128, 2, 512])
                cL2 = T(t2, "cL2", [128, 2, 512])
                DM(lambda e: e.dma_start(out=aL2[:], in_=aL2_d), w=["aL2"])
                DM(lambda e: e.dma_start(out=bL2[:], in_=bL2_d.rearrange("p a q h -> p a (q h)")), w=["bL2"])
                DM(lambda e: e.dma_start(out=cL2[:], in_=cL2_d.rearrange("p a q h -> p a (q h)")), w=["cL2"])
                w2 = T(t2, "w2", [128, 12, 32])
                pw2 = T(t2, "pw2", [128, 2, 9, 32])
                V(lambda e: e.memset(pw2[:, 0, 0, :], 1.0), [], ["L2o"])
                V(lambda e: e.memset(pw2[:, 1, 0, :], 0.0), [], ["L2o"])
                mults = [(kk, None, pw2[:, 0, kk, :], pw2[:, 1, kk, :]) for kk in range(1, 8)]
                mults.append((8, sml[:, 0, :], pw2[:, 0, 8, :], pw2[:, 1, 8, :]))
                mults.append((2048, None, sml[:, 2, :], sml[:, 3, :]))
                lam_block("L2", "aL2", aL2[:, 0, :], aL2[:, 1, :], aL2[:, 2, :], w2, 32, mults)
                KL2 = ["L2w", "L2o", "bL2", "cL2"]
                V(lambda e: e.tensor_scalar(out=sml[:, 1, :], in0=w2[:, 6, :], scalar1=8.0 / TWO_PI, scalar2=None, op0=ALU.mult), KL2, ["L2o"])
                V(lambda e: e.tensor_scalar(out=w2[:, 10, :], in0=sml[:, 1, :], scalar1=MAGIC, scalar2=None, op0=ALU.add), KL2, ["L2w"])
                V(lambda e: e.tensor_scalar(out=w2[:, 10, :], in0=w2[:, 10, :], scalar1=-MAGIC, scalar2=None, op0=ALU.add), KL2, ["L2w"])
                V(lambda e: e.tensor_tensor(out=sml[:, 1, :], in0=sml[:, 1, :], in1=w2[:, 10, :], op=ALU.subtract), KL2, ["L2o"])
                BB2 = T(t2, "BB2", [128, 2, 512])
                tC = T(t2, "tC", [128, 512])
                tD = T(t2, "tD", [128, 512])
                cf_re = w2[:, 2, :].unsqueeze(2).to_broadcast([128, 32, 16])
                cf_im = w2[:, 3, :].unsqueeze(2).to_broadcast([128, 32, 16])
                v3 = lambda ap: ap.rearrange("p (q h) -> p q h", h=16)
                cmul(v3(BB2[:, 0, :]), v3(BB2[:, 1, :]), cf_re, cf_im, v3(bL2[:, 0, :]), v3(bL2[:, 1, :]),
                     v3(tC[:]), v3(tD[:]), KL2 + ["tCD"], ["BB2", "tCD"])
                BBp = T(t2, "BBp", [128, 32, 2, 32], BF16)
                for ri in range(2):
                    for g2 in range(2):
                        V(lambda e, ri=ri, g2=g2: e.tensor_scalar(
                            out=BBp[:, :, ri, g2 * 16:(g2 + 1) * 16], in0=v3(BB2[:, ri, :]),
                            scalar1=mh2[g2], scalar2=None, op0=ALU.mult), ["BB2", "cst"], ["BBp"])
                CL = T(t2, "CL", [128, 2, 3, 512])
                tE = T(t2, "tE", [128, 3, 512])
                tF = T(t2, "tF", [128, 3, 512])
                v4 = lambda ap: ap.rearrange("p k (q h) -> p k q h", h=16)
                c_re = v3(cL2[:, 0, :]).unsqueeze(1).to_broadcast([128, 3, 32, 16])
                c_im = v3(cL2[:, 1, :]).unsqueeze(1).to_broadcast([128, 3, 32, 16])
                for k3 in range(3):
                    p_re = pw2[:, 0, 3 * k3:3 * k3 + 3, :].unsqueeze(3).to_broadcast([128, 3, 32, 16])
                    p_im = pw2[:, 1, 3 * k3:3 * k3 + 3, :].unsqueeze(3).to_broadcast([128, 3, 32, 16])
                    cmul(v4(CL[:, 0]), v4(CL[:, 1]), c_re, c_im, p_re, p_im, v4(tE[:]), v4(tF[:]), KL2 + ["tEF"], ["CL", "tEF"], neg_im=True)
                    for ri in range(2):
                        for g2 in range(2):
                            V(lambda e, ri=ri, g2=g2, k3=k3: e.tensor_scalar(
                                out=Qtab[:, :, 3 * k3:3 * k3 + 3, ri, g2 * 16:(g2 + 1) * 16],
                                in0=v4(CL[:, ri]).rearrange("p k q h -> p q k h"),
                                scalar1=mh2[g2], scalar2=None, op0=ALU.mult), ["CL", "cst"], ["Qtab"])
                for gb in range(8):
                    b = nbank()
                    pk = "ps%d" % b

                    def kmm(e, gb=gb, b=b):
                        ins = None
                        for qd in range(4):
                            q = 4 * gb + qd
                            for ri in range(2):
                                ins = e.matmul(ps[b][32 * qd:32 * qd + 32, 0:256], lhsT=BBp[:, q, ri, :],
                                               rhs=Qtab[:, q, 0:8, ri, :], start=(ri == 0), stop=(ri == 1),
                                               tile_position=(0, 32 * qd))
                        return ins
                    PE(kmm, ["BBp", "Qtab"], [pk])
                    for qd in range(4):
                        V(lambda e, gb=gb, b=b, qd=qd: e.tensor_scalar(
                            out=Ktab[:, gb, :, 32 * qd:32 * qd + 32],
                            in0=ps[b][:, 0:256].rearrange("p (t c) -> p t c", c=32),
                            scalar1=mqd[qd], scalar2=None, op0=ALU.mult), [pk, "cst"], ["Ktab"])

            dump("zs", zs, [128, 8, 8, NCH], BF16, "zs")
            dump("Ptab", Ptab, [128, 8, 8, 2, 128], BF16, "Ptab")
            dump("Qtab", Qtab, [128, 32, 9, 2, 32], BF16, "Qtab")
            dump("Ktab", Ktab, [128, 8, 8, 128], BF16, "Ktab")
            dump("sml", sml, [128, 8, 32], F32, "L2o")
            S.barrier()

            with ExitStack() as a3:
                Sq = T(a3, "Sq", [128, 4, 2, NCH])
                St = T(a3, "St", [128, 4, 2, NCH])
                Xt = Sq
                Wcs = T(a3, "Wcs", [128, 2, 4, NCH + 1])
                Rt = T(a3, "Rt", [128, NCH])
                tm = T(a3, "tm", [128, 2, 4, NCH + 1])
                Xb = T(a3, "Xb", [128, 4, 2, NCH], BF16)
                yt = T(a3, "yt", [128, 1024])
                y2 = T(a3, "y2", [128, 1024])
                Fall = T(a3, "Fall", [128, 4, 64])
                Gh = T(a3, "Gh", [128, 4, 64])

                def chunk_states(blk, with_carry):
                    q0 = 4 * blk
                    for ql in range(4):
                        for ri in range(2):
                            b = nbank()
                            pk = "ps%d" % b

                            def smm(e, ql=ql, ri=ri, b=b):
                                ins = None
                                for s in range(TCH):
                                    ins = e.matmul(ps[b][:, 0:NCH], lhsT=Ptab[32 * ql:32 * ql + 32, blk, 7 - s, ri, :],
                                                   rhs=zs[32 * ql:32 * ql + 32, blk, s, :], start=(s == 0), stop=(s == TCH - 1),
                                                   tile_position=(32 * ql, 0))
                                return ins
                            PE(smm, ["Ptab", "zs"], [pk])
                            if ri == 0:
                                A(lambda e, ql=ql, ri=ri, b=b: e.activation(out=Sq[:, ql, ri, :], in_=ps[b][:, 0:NCH], func=AF.Copy), [pk], ["Sq"])
                            else:
                                V(lambda e, ql=ql, ri=ri, b=b: e.tensor_copy(out=Sq[:, ql, ri, :], in_=ps[b][:, 0:NCH]), [pk], ["Sq"])
                    if with_carry:
                        V(lambda e: e.tensor_tensor(out=Sq[:, :, :, 1], in0=Sq[:, :, :, 1], in1=Xc[:, q0:q0 + 4, :], op=ALU.add), ["Sq", "Xc"], ["Sq"])
                    fr = sml[:, 1, q0:q0 + 4].unsqueeze(2).to_broadcast([128, 4, NCH + 1])
                    io = iota_c.unsqueeze(1).to_broadcast([128, 4, NCH + 1])
                    V(lambda e: e.tensor_tensor(out=tm[:, 0], in0=fr, in1=io, op=ALU.mult), ["sml", "cst", "tm"], ["tm"])
                    for (j, off) in ((1, 0.0), (0, 0.25)):
                        V(lambda e, off=off: e.tensor_scalar(out=tm[:, 1], in0=tm[:, 0], scalar1=off, scalar2=MAGIC, op0=ALU.add, op1=ALU.add), ["tm"], ["tm"])
                        V(lambda e: e.tensor_scalar(out=tm[:, 1], in0=tm[:, 1], scalar1=-MAGIC, scalar2=None, op0=ALU.add), ["tm"], ["tm"])
                        V(lambda e, off=off: e.scalar_tensor_tensor(out=tm[:, 1], in0=tm[:, 0], scalar=off, in1=tm[:, 1], op0=ALU.add, op1=ALU.subtract), ["tm"], ["tm"])
                        A(lambda e, j=j: e.activation(out=Wcs[:, j], in_=tm[:, 1], func=AF.Sin, scale=TWO_PI), ["tm"], ["Wcs"])
                    cw = Wcs[:, 0, :, 1:NCH + 1]
                    sw = Wcs[:, 1, :, 1:NCH + 1]
                    t_a = tm[:, 0, :, 0:NCH]
                    t_b = tm[:, 1, :, 0:NCH]
                    kk = ["Sq", "Wcs", "tm"]
                    V(lambda e: e.tensor_tensor(out=t_a, in0=Sq[:, :, 0, :], in1=cw, op=ALU.mult), kk, ["tm"])
                    V(lambda e: e.tensor_tensor(out=t_b, in0=Sq[:, :, 1, :], in1=sw, op=ALU.mult), kk, ["tm"])
                    V(lambda e: e.tensor_tensor(out=St[:, :, 0, :], in0=t_a, in1=t_b, op=ALU.add), ["tm"], ["St"])
                    V(lambda e: e.tensor_tensor(out=t_a, in0=Sq[:, :, 1, :], in1=cw, op=ALU.mult), kk, ["tm"])
                    V(lambda e: e.tensor_tensor(out=t_b, in0=Sq[:, :, 0, :], in1=sw, op=ALU.mult), kk, ["tm"])
                    V(lambda e: e.tensor_tensor(out=St[:, :, 1, :], in0=t_a, in1=t_b, op=ALU.subtract), ["tm"], ["St"])
                    for ql in range(4):
                        V(lambda e, ql=ql: e.tensor_copy(out=Rt[:], in_=sml[:, 0, q0 + ql:q0 + ql + 1].to_broadcast([128, NCH])), ["sml", "Rt"], ["Rt"])
                        for ri in range(2):
                            V(lambda e, ri=ri, ql=ql: e.tensor_tensor_scan(out=Xt[:, ql, ri, :], data0=Rt[:], data1=St[:, ql, ri, :],
                                                                           initial=0.0, op0=ALU.mult, op1=ALU.add), ["Rt", "St", "Sq"], ["Sq"])

                for blk in range(8):
                    chunk_states(blk, False)
                    q0 = 4 * blk
                    c258 = Wcs[:, 0, :, NCH]
                    s258 = Wcs[:, 1, :, NCH]
                    xr = Xt[:, :, 0, NCH - 1]
                    xi = Xt[:, :, 1, NCH - 1]
                    ta = tm[:, 0, :, 0]
                    tb = tm[:, 1, :, 0]
                    kk = ["Sq", "Wcs", "tm"]
                    V(lambda e, xr=xr, c258=c258, ta=ta: e.tensor_tensor(out=ta, in0=xr, in1=c258, op=ALU.mult), kk, ["tm"])
                    V(lambda e, xi=xi, s258=s258, tb=tb: e.tensor_tensor(out=tb, in0=xi, in1=s258, op=ALU.mult), kk, ["tm"])
                    V(lambda e, q0=q0, ta=ta, tb=tb: e.tensor_tensor(out=Fst[:, q0:q0 + 4, 0], in0=ta, in1=tb, op=ALU.subtract), ["tm"], ["Fst"])
                    V(lambda e, xr=xr, s258=s258, ta=ta: e.tensor_tensor(out=ta, in0=xr, in1=s258, op=ALU.mult), kk, ["tm"])
                    V(lambda e, xi=xi, c258=c258, tb=tb: e.tensor_tensor(out=tb, in0=xi, in1=c258, op=ALU.mult), kk, ["tm"])
                    V(lambda e, q0=q0, ta=ta, tb=tb: e.tensor_tensor(out=Fst[:, q0:q0 + 4, 1], in0=ta, in1=tb, op=ALU.add), ["tm"], ["Fst"])
                DM(lambda e: e.dma_start(out=cc_in, in_=Fst[:].rearrange("p q r -> p (q r)")), ["Fst"], ["cc_in"])
                S.collective(lambda e: e.collective_compute("AllGather", ALU.bypass, replica_groups=[[0, 1, 2, 3], [4, 5, 6, 7]],
                                                            ins=[cc_in], outs=[cc_out]), ["cc_in"], ["cc_out"])
                DM(lambda e: e.dma_start(out=Fall[:], in_=cc_out.rearrange("(j p) f -> p j f", p=128)), ["cc_out"], ["Fall"])
                for p_ in range(3):
                    V(lambda e, p_=p_: e.tensor_scalar(out=Gh[:, p_, :], in0=Fall[:, 0, :], scalar1=cm[:, p_:p_ + 1], scalar2=None, op0=ALU.mult), ["Fall", "cm"], ["Gh"])
                    for j in range(1, 4):
                        V(lambda e, p_=p_, j=j: e.scalar_tensor_tensor(out=Gh[:, p_, :], in0=Fall[:, j, :], scalar=cm[:, 3 * j + p_:3 * j + p_ + 1],
                                                                       in1=Gh[:, p_, :], op0=ALU.mult, op1=ALU.add), ["Fall", "cm", "Gh"], ["Gh"])
                gv = lambda p_, ri: Gh[:, p_, :].rearrange("p (q r) -> p q r", r=2)[:, :, ri]
                lre = sml[:, 2, :]
                lim = sml[:, 3, :]
                ta = tm[:, 0, 0, 0:32]
                tb = tm[:, 1, 0, 0:32]
                acc_re = Gh[:, 3, 0:32]
                acc_im = Gh[:, 3, 32:64]
                kk = ["Gh", "sml", "tm"]

                def horner(src_re, src_im, add_p, o_re, o_im):
                    V(lambda e: e.tensor_tensor(out=ta, in0=src_re, in1=lre, op=ALU.mult), kk, ["tm"])
                    V(lambda e: e.tensor_tensor(out=tb, in0=src_im, in1=lim, op=ALU.mult), kk, ["tm"])
                    V(lambda e: e.tensor_tensor(out=ta, in0=ta, in1=tb, op=ALU.subtract), ["tm"], ["tm"])
                    V(lambda e: e.tensor_tensor(out=tb, in0=src_re, in1=lim, op=ALU.mult), kk, ["tm"])
                    V(lambda e: e.tensor_tensor(out=o_re, in0=ta, in1=gv(add_p, 0), op=ALU.add), kk, ["Gh", "Xc"])
                    V(lambda e: e.tensor_tensor(out=ta, in0=src_im, in1=lre, op=ALU.mult), kk, ["tm"])
                    V(lambda e: e.tensor_tensor(out=tb, in0=tb, in1=ta, op=ALU.add), ["tm"], ["tm"])
                    V(lambda e: e.tensor_tensor(out=o_im, in0=tb, in1=gv(add_p, 1), op=ALU.add), kk, ["Gh", "Xc"])
                horner(gv(2, 0), gv(2, 1), 1, acc_re, acc_im)
                horner(acc_re, acc_im, 0, Xc[:, :, 0], Xc[:, :, 1])
                dump("Fst", Fst, [128, 32, 2], F32, "Fst")
                dump("Xc", Xc, [128, 32, 2], F32, "Xc")

                for blk in range(8):
                    chunk_states(blk, True)
                    cw = Wcs[:, 0, :, 1:NCH]
                    sw = Wcs[:, 1, :, 1:NCH]
                    xr = Xt[:, :, 0, 0:NCH - 1]
                    xi = Xt[:, :, 1, 0:NCH - 1]
                    t_a = tm[:, 0, :, 0:NCH - 1]
                    t_b = tm[:, 1, :, 0:NCH - 1]
                    kk = ["Sq", "Wcs", "tm"]
                    V(lambda e, xr=xr, cw=cw, t_a=t_a: e.tensor_tensor(out=t_a, in0=xr, in1=cw, op=ALU.mult), kk, ["tm"])
                    V(lambda e, xi=xi, sw=sw, t_b=t_b: e.tensor_tensor(out=t_b, in0=xi, in1=sw, op=ALU.mult), kk, ["tm"])
                    V(lambda e, t_a=t_a, t_b=t_b: e.tensor_tensor(out=Xb[:, :, 0, 1:NCH], in0=t_a, in1=t_b, op=ALU.subtract), ["tm"], ["Xb"])
                    V(lambda e, xr=xr, sw=sw, t_a=t_a: e.tensor_tensor(out=t_a, in0=xr, in1=sw, op=ALU.mult), kk, ["tm"])
                    V(lambda e, xi=xi, cw=cw, t_b=t_b: e.tensor_tensor(out=t_b, in0=xi, in1=cw, op=ALU.mult), kk, ["tm"])
                    V(lambda e, t_a=t_a, t_b=t_b: e.tensor_tensor(out=Xb[:, :, 1, 1:NCH], in0=t_a, in1=t_b, op=ALU.add), ["tm"], ["Xb"])
                    banks = [nbank() for _ in range(4)]
                    pks = ["ps%d" % b for b in banks]

                    def omm(e, blk=blk, banks=banks):
                        ins = None
                        for j in range(4):
                            pb = ps[banks[j]]
                            first = True
                            for tau in range(0, 2 * j + 2):
                                s_lo = max(2 * j, tau)
                                ns = 2 * j + 2 - s_lo
                                o0 = (s_lo - 2 * j) * 256
                                ins = e.matmul(pb[:, o0:o0 + ns * 256], lhsT=Ktab[:, blk, tau, :],
                                               rhs=zs[:, blk, s_lo - tau:s_lo - tau + ns, 2:NCH],
                                               start=first, stop=False)
                                first = False
                            for sl in range(2):
                                s = 2 * j + sl
                                for ql in range(4):
                                    for ri in range(2):
                                        ins = e.matmul(pb[32 * ql:32 * ql + 32, sl * 256:(sl + 1) * 256],
                                                       lhsT=Qtab[:, 4 * blk + ql, s + 1, ri, :], rhs=Xb[:, ql, ri, 2:NCH],
                                                       start=False, stop=(sl == 1 and ql == 3 and ri == 1),
                                                       tile_position=(0, 32 * ql))
                        return ins
                    PE(omm, ["Ktab", "zs", "Qtab", "Xb"], pks)
                    for hf in range(2):
                        for jj in range(2):
                            j = 2 * hf + jj
                            V(lambda e, j=j, jj=jj, blk=blk, banks=banks: e.scalar_tensor_tensor(
                                out=yt[:, jj * 512:(jj + 1) * 512].rearrange("p (s c) -> p s c", s=2),
                                in0=zs[:, blk, 2 * j:2 * j + 2, 2:NCH], scalar=cols[:, 0, blk:blk + 1],
                                in1=ps[banks[j]][:, 0:512].rearrange("p (s c) -> p s c", s=2), op0=ALU.mult, op1=ALU.add),
                              ["zs", "cols", pks[j]], ["yt"])
                        A(lambda e: e.activation(out=y2[:], in_=yt[:], func=AF.Square), ["yt"], ["y2"])
                        V(lambda e: e.tensor_scalar(out=y2[:], in0=y2[:], scalar1=0.044715, scalar2=1.0, op0=ALU.mult, op1=ALU.add), ["y2"], ["y2"])
                        V(lambda e: e.tensor_tensor(out=y2[:], in0=y2[:], in1=yt[:], op=ALU.mult), ["y2", "yt"], ["y2"])
                        A(lambda e: e.activation(out=y2[:], in_=y2[:], func=AF.Sigmoid, scale=1.5957691216057308), ["y2"], ["y2"])
                        V(lambda e, blk=blk, hf=hf: e.tensor_tensor(
                            out=sT[:, blk, :].rearrange("p (c s) -> p s c", s=TCH)[:, 4 * hf:4 * hf + 4, :],
                            in0=yt[:].rearrange("p (s c) -> p s c", s=4),
                            in1=y2[:].rearrange("p (s c) -> p s c", s=4), op=ALU.mult), ["yt", "y2"], ["sT"])
            dump("sT", sT, [128, 8, NT], BF16, "sT")
            DM(lambda e: e.dma_start(out=sTd, in_=sT[:].rearrange("p a t -> p (a t)")), ["sT"], ["sTd"])
            S.barrier()
            ssm_stack.close()


        w_in_v = w_in.rearrange("(k p) n -> p k n", p=128)
        pbk = ExitStack()
        gain = T(pbk, "gain", [128, D])
        XH = T(pbk, "XH", [128, 4, D])
        ubs = [T(pbk, "vb%d" % i, [128, D], BF16) for i in range(2)]
        sqs = [T(pbk, "sqb%d" % i, [128, 4]) for i in range(2)]
        uT = XH[:, 2:4, :].rearrange("p a b -> p (a b)").bitcast(BF16).rearrange("p (k t) -> p k t", t=512)
        UTK = ["xh2", "xh3"]
        zp = T(pbk, "zp", [128, 8, NH + 512], BF16)
        pa = T(pbk, "pa", [128, NH + 512])
        pb_ = T(pbk, "pb", [128, NH + 512])
        dT = T(pbk, "dT", [128, 8, 512], BF16)
        s2T = T(pbk, "s2T", [128, 8, 512], BF16)
        mgT = T(pbk, "mgT", [128, KC, 512], BF16)
        NRING = 6
        vb4 = T(pbk, "vb4", [128, 4, D], BF16)
        lg4 = T(pbk, "lg4", [128, 4, 72])
        mb4 = T(pbk, "mb4", [128, 4, 64], BF16)
        sTb = T(pbk, "sTb", [128, 8, 512], BF16)
        ring = [T(pbk, "ring%d" % i, [128, 2048]) for i in range(NRING)]
        ring_i = [0]
        sg = [T(pbk, "sg%d" % i, [128, 512]) for i in range(3)]
        vT32 = T(pbk, "vT32", [128, KC, 128])
        wr = T(pbk, "wr", [128, KC, 72])
        br = T(pbk, "br", [128, 72])
        lg = T(pbk, "lg", [128, 72])
        rt = T(pbk, "rt", [128, 8, 256])
        rs = T(pbk, "rs", [128, 40])
        base = T(pbk, "base", [128, 64])
        mb = T(pbk, "mb", [128, 64], BF16)
        DM(lambda e: e.dma_start(out=wr[:], in_=wr_d), [], ["wr"])
        DM(lambda e: e.dma_start(out=br[:], in_=br_d), [], ["br"])
        V(lambda e: e.memset(base[:], 0.0), [], ["base"])
        V(lambda e: e.memset(ubs[0][:], 0.0), [], ["vb0"])
        for r in range((NE * CAP + 128) // 128):
            DM(lambda e, r=r: e.dma_start(out=xg[r * 128:(r + 1) * 128, :], in_=ubs[0][:]), ["vb0"], ["xg"])

        def ring_load(src_ap, shape_view):
            i = ring_i[0] % NRING
            ring_i[0] += 1
            key = "ring%d" % i
            dst = ring[i]
            DM(lambda e: e.dma_start(out=shape_view(dst), in_=src_ap), [], [key])
            return dst, key

        def w_piece(w_v, kc0, nkc, c0, ncols):
            dst, key = ring_load(w_v[:, kc0:kc0 + nkc, c0:c0 + ncols],
                                 lambda d: d[:, 0:nkc * ncols].rearrange("p (k n) -> p k n", n=ncols))
            return (lambda kl, c, n: hi(dst[:, kl * ncols + c:kl * ncols + c + n])), key

        nt_i = [0]

        def ntile():
            i = nt_i[0] % 2
            nt_i[0] += 1
            return (XH[:, i, :], ubs[i], sqs[i], "xh%d" % i, "vb%d" % i)

        pool_w_v = pool_w.rearrange("g (k p) n -> p g k n", p=128)
        w_glu_v = w_glu.rearrange("(k p) n -> p k n", p=128)
        w_bp_v = w_bp.rearrange("(k p) n -> p k n", p=128)
        w_bs_v = w_bs.rearrange("(k p) n -> p k n", p=128)
        w_out_v = w_out.rearrange("(k p) n -> p k n", p=128)
        ev_i = [0]

        def evac_copy(dst, src, rk, wk):
            ev_i[0] += 1
            if ev_i[0] % 2 == 0:
                A(lambda e: e.activation(out=dst, in_=src, func=AF.Copy), rk, wk)
            else:
                V(lambda e: e.tensor_copy(out=dst, in_=src), rk, wk)

        def zpool_cols(ncol, col_off, uT_ap, utk):
            for mp in range(4):
                getters = [w_piece(w_in_v, 8 * hk, 8, 256 * mp, 256) for hk in range(2)]
                for mm in range(2):
                    m = 2 * mp + mm
                    b = nbank()
                    pk = "ps%d" % b

                    def zmm(e, mm=mm, b=b):
                        ins = None
                        for kc in range(KC):
                            g_, _ = getters[kc // 8]
                            ins = e.matmul(ps[b][:, 0:ncol], lhsT=g_(kc % 8, mm * 128, 128), rhs=uT_ap[:, kc, 0:ncol],
                                           start=(kc == 0), stop=(kc == KC - 1))
                        return ins
                    PE(zmm, [getters[0][1], getters[1][1]] + utk, [pk])
                    evac_copy(zp[:, m, col_off:col_off + ncol], ps[b][:, 0:ncol], [pk], ["zp"])

        for tb in range(NBLK):
            tok0 = 512 * tb
            DM(lambda e, tok0=tok0: e.dma_start(out=sTb[:], in_=sTd.rearrange("p (a t) -> p a t", a=8)[:, :, tok0:tok0 + 512]), ["sTd"], ["sTb"])
            DM(lambda e: e.dma_start(out=gain[:], in_=gains_d[:, 0, :]), [], ["gains"])
            if tb == 0:
                load_norm_T(ntile(), 0, NH, gain, uT, 0, UTK)
                zpool_cols(NH, 0, uT, UTK)
            else:
                G(lambda e: e.tensor_copy(out=zp[:, :, 0:NH], in_=zp[:, :, 512:512 + NH]), ["zp"], ["zp"])
            for tt in range(4):
                load_norm_T(ntile(), NH + tok0 + 128 * tt, 128, gain, uT, 128 * tt, UTK)
            zpool_cols(512, NH, uT, UTK)
            for m in range(8):
                steps = m // 2 + 1
                srcs = [zp[:, m, :], pa[:], pb_[:], pa[:], pb_[:]]
                keys = ["zp", "pa", "pb", "pa", "pb"]
                for sidx in range(steps):
                    sh = 1 << sidx
                    lo = 2 * sh - 1
                    G(lambda e, sidx=sidx, sh=sh, lo=lo, srcs=srcs: e.tensor_tensor(
                        out=srcs[sidx + 1][:, lo:NH + 512], in0=srcs[sidx][:, lo:NH + 512], in1=srcs[sidx][:, lo - sh:NH + 512 - sh],
                        op=ALU.add), [keys[sidx]], [keys[sidx + 1]])
                V(lambda e, m=m, steps=steps, srcs=srcs: e.scalar_tensor_tensor(
                    out=dT[:, m, :], in0=srcs[steps][:, NH:NH + 512], scalar=1.0 / (1 << steps), in1=zp[:, m, NH:NH + 512],
                    op0=ALU.mult, op1=ALU.subtract), [keys[steps], "zp"], ["dT"])
            for gi in range(4):
                dst, key = ring_load(pool_w_v[:, gi, :, :], lambda d: d[:, 0:512].rearrange("p (k n) -> p k n", n=256))
                bks = [nbank(), nbank()]

                def pmm(e, gi=gi, dst=dst, bks=bks):
                    ins = None
                    for mo in range(2):
                        for kc in range(2):
                            ins = e.matmul(ps[bks[mo]][:, 0:512], lhsT=hi(dst[:, kc * 256 + mo * 128:kc * 256 + mo * 128 + 128]),
                                           rhs=dT[:, 2 * gi + kc, :], start=(kc == 0), stop=(kc == 1))
                    return ins
                PE(pmm, [key, "dT"], ["ps%d" % bks[0], "ps%d" % bks[1]])
                for mo in range(2):
                    m = 2 * gi + mo
                    A(lambda e, m=m, mo=mo, bks=bks: e.activation(out=dT[:, m, :], in_=ps[bks[mo]][:, 0:512], func=AF.Copy,
                                                                   scale=cols[:, 1, m:m + 1]), ["ps%d" % bks[mo], "cols"], ["dT"])
            for mp in range(4):
                g_, key = w_piece(w_glu_v, 0, 8, 256 * mp, 256)
                for mm in range(2):
                    mo = 2 * mp + mm
                    b = nbank()
                    pk = "ps%d" % b

                    def gmm(e, mm=mm, b=b, g_=g_):
                        ins = None
                        for kc in range(8):
                            ins = e.matmul(ps[b][:, 0:512], lhsT=g_(kc, mm * 128, 128), rhs=sTb[:, kc, :],
                                           start=(kc == 0), stop=(kc == 7))
                        return ins
                    PE(gmm, [key, "sTb"], [pk])
                    A(lambda e, mo=mo, b=b: e.activation(out=sg[0][:], in_=ps[b][:, 0:512], func=AF.Sigmoid,
                                                         bias=cols[:, 2, mo:mo + 1], scale=1.0), [pk, "cols"], ["sg0"])
                    V(lambda e, mo=mo: e.tensor_tensor(out=s2T[:, mo, :], in0=sTb[:, mo, :], in1=sg[0][:], op=ALU.mult),
                      ["sg0", "sTb"], ["s2T"])
            for jp in range(8):
                bk = [nbank() for _ in range(8)]
                pks = ["ps%d" % b for b in bk]
                gbp, kbp = w_piece(w_bp_v, 0, 8, 256 * jp, 256)

                def m_yp(e, bk=bk, gbp=gbp):
                    ins = None
                    for jj in range(2):
                        for kc in range(8):
                            ins = e.matmul(ps[bk[jj]][:, 0:512], lhsT=gbp(kc, jj * 128, 128), rhs=dT[:, kc, :], start=(kc == 0), stop=(kc == 7))
                    return ins
                PE(m_yp, [kbp, "dT"], pks[0:2])
                gbs, kbs = w_piece(w_bs_v, 0, 8, 256 * jp, 256)

                def m_ys(e, bk=bk, gbs=gbs):
                    ins = None
                    for jj in range(2):
                        for kc in range(8):
                            ins = e.matmul(ps[bk[2 + jj]][:, 0:512], lhsT=gbs(kc, jj * 128, 128), rhs=s2T[:, kc, :], start=(kc == 0), stop=(kc == 7))
                    return ins
                PE(m_ys, [kbs, "s2T"], pks[2:4])
                for gsel in range(2):
                    gg = [w_piece(w_in_v, 8 * hk, 8, 2048 * (gsel + 1) + 256 * jp, 256) for hk in range(2)]

                    def m_g(e, bk=bk, gg=gg, gsel=gsel):
                        ins = None
                        for jj in range(2):
                            for kc in range(KC):
                                ins = e.matmul(ps[bk[4 + 2 * gsel + jj]][:, 0:512], lhsT=gg[kc // 8][0](kc % 8, jj * 128, 128), rhs=uT[:, kc, :],
                                               start=(kc == 0), stop=(kc == KC - 1))
                        return ins
                    PE(m_g, [gg[0][1], gg[1][1]] + UTK, pks[4 + 2 * gsel:6 + 2 * gsel])
                for jj in range(2):
                    j = 2 * jp + jj
                    A(lambda e, bk=bk, jj=jj: e.activation(out=sg[0][:], in_=ps[bk[4 + jj]][:, 0:512], func=AF.Sigmoid), [pks[4 + jj]], ["sg0"])
                    A(lambda e, bk=bk, jj=jj: e.activation(out=sg[1][:], in_=ps[bk[6 + jj]][:, 0:512], func=AF.Sigmoid), [pks[6 + jj]], ["sg1"])
                    V(lambda e, bk=bk, jj=jj: e.tensor_tensor(out=sg[0][:], in0=sg[0][:], in1=ps[bk[jj]][:, 0:512], op=ALU.mult), ["sg0", pks[jj]], ["sg0"])
                    V(lambda e, bk=bk, jj=jj: e.tensor_tensor(out=sg[1][:], in0=sg[1][:], in1=ps[bk[2 + jj]][:, 0:512], op=ALU.mult), ["sg1", pks[2 + jj]], ["sg1"])
                    G(lambda e, j=j: e.tensor_tensor(out=mgT[:, j, :], in0=sg[0][:], in1=sg[1][:], op=ALU.add), ["sg0", "sg1"], ["mgT"])
            if tb == 0:
                dump("zp", zp, [128, 8, NH + 512], BF16, "zp")
                dump("dT", dT, [128, 8, 512], BF16, "dT")
                dump("s2T", s2T, [128, 8, 512], BF16, "s2T")
                dump("mgT", mgT, [128, KC, 512], BF16, "mgT")
            for tt in range(4):
                DM(lambda e, tt=tt: e.dma_start(out=XH[:, tt, :], in_=xs[NH + tok0 + 128 * tt:NH + tok0 + 128 * (tt + 1), :]),
                   [], ["xh%d" % tt])
            DM(lambda e: e.dma_start(out=gain[:], in_=gains_d[:, 1, :]), [], ["gains"])
            for nb in range(4):
                bk = [nbank() for _ in range(4)]
                pks = ["ps%d" % b for b in bk]
                pieces = [w_piece(w_out_v, 4 * kq, 4, 512 * nb, 512) for kq in range(4)]

                def omm2(e, bk=bk, pieces=pieces):
                    ins = None
                    for kq in range(4):
                        for tt in range(4):
                            for kl in range(4):
                                kc = 4 * kq + kl
                                ins = e.matmul(ps[bk[tt]][:, 0:512], lhsT=mgT[:, kc, 128 * tt:128 * (tt + 1)],
                                               rhs=pieces[kq][0](kl, 0, 512), start=(kc == 0), stop=(kc == KC - 1))
                    return ins
                PE(omm2, [p_[1] for p_ in pieces] + ["mgT"], pks)
                for tt in range(4):
                    V(lambda e, tt=tt, nb=nb, bk=bk: e.tensor_tensor(out=XH[:, tt, 512 * nb:512 * (nb + 1)], in0=XH[:, tt, 512 * nb:512 * (nb + 1)],
                                                                     in1=ps[bk[tt]][:, 0:512], op=ALU.add), [pks[tt], "xh%d" % tt], ["xh%d" % tt])
            for tt in range(4):
                ti = 4 * tb + tt
                hk = "xh%d" % tt
                ht = XH[:, tt, :]
                vb = ubs[tt % 2]
                vk = "vb%d" % (tt % 2)
                sq = sqs[tt % 2]
                sk = "sqb%d" % (tt % 2)
                DM(lambda e, ti=ti, ht=ht: e.dma_start(out=hs[128 * ti:128 * (ti + 1), :], in_=ht), [hk], ["hs"])
                A(lambda e, ht=ht, vb=vb, sq=sq: e.activation(out=vb[:], in_=ht, func=AF.Square, accum_out=sq[:, 0:1]), [hk], [vk, sk])
                V(lambda e, sq=sq: e.tensor_scalar(out=sq[:, 1:2], in0=sq[:, 0:1], scalar1=1.0 / D, scalar2=EPS, op0=ALU.mult, op1=ALU.add), [sk], [sk])
                A(lambda e, sq=sq: e.activation(out=sq[:, 1:2], in_=sq[:, 1:2], func=AF.Sqrt), [sk], [sk])
                V(lambda e, sq=sq: e.reciprocal(out=sq[:, 2:3], in_=sq[:, 1:2]), [sk], [sk])
                V(lambda e, ht=ht, sq=sq: e.scalar_tensor_tensor(out=ht, in0=ht, scalar=sq[:, 2:3], in1=gain[:], op0=ALU.mult, op1=ALU.mult),
                  [hk, sk, "gains"], [hk])
                G(lambda e, ht=ht, tt=tt: e.tensor_copy(out=vb4[:, tt, :], in_=ht), [hk], ["vb4_%d" % tt])
                for qd in range(4):
                    b = nbank()
                    pk = "ps%d" % b

                    def vtr(e, qd=qd, b=b, ht=ht):
                        ins = None
                        for j in range(4):
                            kc = 4 * qd + j
                            ins = e.transpose(out=ps[b][:, j * 128:(j + 1) * 128], in_=ht[:, kc * 128:(kc + 1) * 128], identity=ident32)
                        return ins
                    PE(vtr, [hk, "cst"], [pk])
                    evac_copy(vT32[:, 4 * qd:4 * qd + 4, :], ps[b][:, 0:512].rearrange("p (k t) -> p k t", t=128), [pk], ["vT32"])
                b = nbank()
                pk = "ps%d" % b

                def rmm(e, b=b):
                    ins = None
                    for kc in range(KC):
                        ins = e.matmul(ps[b][:, 0:72], lhsT=vT32[:, kc, :], rhs=wr[:, kc, :], start=(kc == 0), stop=(kc == KC - 1))
                    return ins
                PE(rmm, ["vT32", "wr"], [pk])
                V(lambda e, b=b, tt=tt: e.tensor_tensor(out=lg4[:, tt, :], in0=ps[b][:, 0:72], in1=br[:], op=ALU.add), [pk, "br"], ["lg4"])
            R = lambda i: rt[:, i, :].rearrange("p (t x) -> p t x", t=4)
            R8 = lambda i: rt[:, i, 0:32].rearrange("p (t x) -> p t x", t=4)
            C4 = lambda i: rs[:, 4 * i:4 * i + 4]
            B8 = lambda ap: ap.unsqueeze(2).to_broadcast([128, 4, 8])
            B64 = lambda ap: ap.unsqueeze(2).to_broadcast([128, 4, 64])
            K_ = ["rt", "rs", "lg4"]
            W_ = ["rt", "rs"]
            t0_ = 4 * tb
            V(lambda e: e.reduce_max(out=C4(0), in_=lg4[:, :, 0:8], axis=AX.X), K_, W_)
            V(lambda e: e.tensor_tensor(out=R8(0), in0=lg4[:, :, 0:8], in1=B8(C4(0)), op=ALU.is_equal), K_, W_)
            V(lambda e: e.tensor_tensor(out=R8(1), in0=lg4[:, :, 0:8], in1=B8(C4(0)), op=ALU.subtract), K_, W_)
            A(lambda e: e.activation(out=R8(1), in_=R8(1), func=AF.Exp), K_, W_)
            V(lambda e: e.reduce_sum(out=C4(1), in_=R8(1), axis=AX.X), K_, W_)
            V(lambda e: e.reciprocal(out=C4(2), in_=C4(1)), K_, W_)
            V(lambda e: e.tensor_scalar(out=R8(1), in0=R8(0), scalar1=1e30, scalar2=-1e30, op0=ALU.mult, op1=ALU.add), K_, W_)
            V(lambda e: e.tensor_tensor(out=rt[:, 2, :].rearrange("p (t g x) -> p t g x", t=4, g=8),
                                        in0=lg4[:, :, 8:72].rearrange("p t (g x) -> p t g x", g=8),
                                        in1=R8(1).unsqueeze(3).to_broadcast([128, 4, 8, 8]), op=ALU.add), K_, W_)
            V(lambda e: e.reduce_max(out=C4(3), in_=R(2), axis=AX.X), K_, W_)
            V(lambda e: e.tensor_tensor(out=R(3), in0=R(2), in1=B64(C4(3)), op=ALU.is_equal), K_, W_)
            V(lambda e: e.tensor_scalar(out=R(4), in0=R(3), scalar1=-1e30, scalar2=None, op0=ALU.mult), K_, W_)
            V(lambda e: e.tensor_tensor(out=R(4), in0=R(4), in1=R(2), op=ALU.add), K_, W_)
            V(lambda e: e.reduce_max(out=C4(4), in_=R(4), axis=AX.X), K_, W_)
            V(lambda e: e.tensor_tensor(out=R(5), in0=R(4), in1=B64(C4(4)), op=ALU.is_equal), K_, W_)
            V(lambda e: e.tensor_tensor(out=C4(5), in0=C4(4), in1=C4(3), op=ALU.subtract), K_, W_)
            A(lambda e: e.activation(out=C4(5), in_=C4(5), func=AF.Exp), K_, W_)
            V(lambda e: e.tensor_scalar(out=C4(5), in0=C4(5), scalar1=1.0, scalar2=None, op0=ALU.add), K_, W_)
            V(lambda e: e.reciprocal(out=C4(6), in_=C4(5)), K_, W_)
            V(lambda e: e.tensor_tensor(out=gw[:, t0_:t0_ + 4, 0], in0=C4(6), in1=C4(2), op=ALU.mult), K_, W_ + ["gw"])
            V(lambda e: e.tensor_tensor(out=gw[:, t0_:t0_ + 4, 1], in0=C4(2), in1=gw[:, t0_:t0_ + 4, 0], op=ALU.subtract), K_ + ["gw"], W_ + ["gw"])
            V(lambda e: e.tensor_tensor(out=mb4[:], in0=R(3), in1=R(5), op=ALU.add), K_, ["mb4"])
            b = nbank()
            pk = "ps%d" % b

            def cmm(e, b=b):
                ins = None
                for tt in range(4):
                    ins = e.matmul(ps[b][:, 64 * tt:64 * tt + 64], lhsT=trib, rhs=mb4[:, tt, :], start=True, stop=(tt == 0))
                    for t2 in range(tt):
                        ins = e.matmul(ps[b][:, 64 * tt:64 * tt + 64], lhsT=onesb, rhs=mb4[:, t2, :], start=False, stop=(t2 == tt - 1))
                for tt in range(4):
                    ins = e.matmul(ps[b][:, 256:320], lhsT=onesb, rhs=mb4[:, tt, :], start=(tt == 0), stop=(tt == 3))
                return ins
            PE(cmm, ["mb4", "cstb"], [pk])
            V(lambda e, b=b: e.tensor_tensor(out=R(6), in0=ps[b][:, 0:256].rearrange("p (t x) -> p t x", t=4),
                                             in1=base[:].unsqueeze(1).to_broadcast([128, 4, 64]), op=ALU.add), [pk, "base"] + K_, W_)
            V(lambda e, b=b: e.tensor_tensor(out=base[:], in0=base[:], in1=ps[b][:, 256:320], op=ALU.add), [pk, "base"] + K_, ["base"])
            for k_ in range(2):
                oh = R(3) if k_ == 0 else R(5)
                V(lambda e, oh=oh: e.tensor_tensor(out=R(7), in0=oh, in1=R(6), op=ALU.mult), K_, W_)
                V(lambda e: e.reduce_sum(out=C4(7), in_=R(7), axis=AX.X), K_, W_)
                V(lambda e: e.tensor_scalar(out=C4(7), in0=C4(7), scalar1=float(CAP - 1), scalar2=None, op0=ALU.min), K_, W_)
                V(lambda e, oh=oh: e.tensor_tensor(out=R(7), in0=oh, in1=iota_e.unsqueeze(1).to_broadcast([128, 4, 64]), op=ALU.mult), K_ + ["cst"], W_)
                V(lambda e: e.reduce_sum(out=C4(8), in_=R(7), axis=AX.X), K_, W_)
                V(lambda e: e.tensor_scalar(out=C4(8), in0=C4(8), scalar1=float(CAP), scalar2=None, op0=ALU.mult), K_, W_)
                V(lambda e: e.tensor_tensor(out=C4(8), in0=C4(8), in1=C4(7), op=ALU.add), K_, W_)
                V(lambda e, k_=k_: e.tensor_copy(out=gidx[:, t0_:t0_ + 4, k_], in_=C4(8)), K_, ["gidx"])
                for tt in range(4):
                    ti = t0_ + tt
                    S.dma(lambda e, ti=ti, k_=k_, tt=tt: e.indirect_dma_start(
                        out=xg, out_offset=bass.IndirectOffsetOnAxis(ap=gidx[:, ti, k_:k_ + 1], axis=0), in_=vb4[:, tt, :], in_offset=None),
                        ["gidx", "vb4_%d" % tt, "xg"], ["xg%d" % (ti * 2 + k_)], q="pool")
        dump("gw", gw, [128, 16, 2], F32, "gw")
        S.barrier()
        pbk.close()

        if stage >= 3:
            pc = ExitStack()
            NR2 = 8
            ring2 = [T(pc, "wrg%d" % i, [128, 4096]) for i in range(NR2)]
            r2_i = [0]
            Xe = [T(pc, "Xe%d" % i, [128, D], BF16) for i in range(2)]
            XeT = [T(pc, "XeT%d" % i, [128, KC, 128], BF16) for i in range(2)]
            hg = T(pc, "hg", [128, 512])
            hh = T(pc, "hh", [128, 512], BF16)
            hT = T(pc, "hT", [128, 4, 128], BF16)
            Ye = [T(pc, "Ye%d" % i, [128, D]) for i in range(2)]

            def piece2(src_ap, nk, ncols):
                i = r2_i[0] % NR2
                r2_i[0] += 1
                key = "wrg%d" % i
                dst = ring2[i]
                DM(lambda e: e.dma_start(out=dst[:, 0:nk * ncols].rearrange("p (k n) -> p k n", n=ncols), in_=src_ap), [], [key])
                return (lambda kl, c, n: hi(dst[:, kl * ncols + c:kl * ncols + c + n])), key

            for ex in range(NE):
                xe = Xe[ex % 2]
                xk = "Xe%d" % (ex % 2)
                xT = XeT[ex % 2]
                xtk = "XeT%d" % (ex % 2)
                DM(lambda e, ex=ex, xe=xe: e.dma_start(out=xe[:], in_=xg[ex * CAP:(ex + 1) * CAP, :]),
                   ["xg"] + ["xg%d" % i for i in range(32)], [xk])
                wg_v = w_gate[ex].rearrange("(p k) f -> p k f", k=KC)
                wu_v = w_up[ex].rearrange("(p k) f -> p k f", k=KC)
                wd_v = w_down[ex].rearrange("(p k) n -> p k n", k=4)
                pg_ = [piece2(wg_v[:, 8 * h_:8 * h_ + 8, :], 8, 512) for h_ in range(2)]
                pu_ = [piece2(wu_v[:, 8 * h_:8 * h_ + 8, :], 8, 512) for h_ in range(2)]
                pd_ = [piece2(wd_v[:, 2 * h_:2 * h_ + 2, :], 2, 2048) for h_ in range(2)]
                for half in range(2):
                    b = nbank()
                    pk = "ps%d" % b
                    psb = ps[b][:].bitcast(BF16)

                    def xtr(e, half=half, psb=psb, xe=xe):
                        ins = None
                        for j in range(8):
                            kc = half * 8 + j
                            ins = e.transpose(out=psb[:, j * 128:(j + 1) * 128], in_=xe[:, kc:D:KC], identity=identb)
                        return ins
                    PE(xtr, [xk, "cstb"], [pk])
                    evac_copy(xT[:, half * 8:half * 8 + 8, :], psb[:, 0:1024].rearrange("p (k t) -> p k t", t=128), [pk], [xtk])
                bg, bu = nbank(), nbank()

                def gu(e, bg=bg, bu=bu, xT=xT, pg_=pg_, pu_=pu_):
                    ins = None
                    for kc in range(KC):
                        ins = e.matmul(ps[bg][:, 0:512], lhsT=xT[:, kc, :], rhs=pg_[kc // 8][0](kc % 8, 0, 512), start=(kc == 0), stop=(kc == KC - 1))
                    for kc in range(KC):
                        ins = e.matmul(ps[bu][:, 0:512], lhsT=xT[:, kc, :], rhs=pu_[kc // 8][0](kc % 8, 0, 512), start=(kc == 0), stop=(kc == KC - 1))
                    return ins
                PE(gu, [xtk] + [p_[1] for p_ in pg_ + pu_], ["ps%d" % bg, "ps%d" % bu])
                A(lambda e, bg=bg: e.activation(out=hg[:], in_=ps[bg][:, 0:512], func=AF.Silu), ["ps%d" % bg], ["hg"])
                V(lambda e, bu=bu: e.tensor_tensor(out=hh[:], in0=hg[:], in1=ps[bu][:, 0:512], op=ALU.mult), ["hg", "ps%d" % bu], ["hh"])
                b = nbank()
                pk = "ps%d" % b
                psb = ps[b][:].bitcast(BF16)

                def htr(e, psb=psb):
                    ins = None
                    for j in range(4):
                        ins = e.transpose(out=psb[:, j * 128:(j + 1) * 128], in_=hh[:, j:512:4], identity=identb)
                    return ins
                PE(htr, ["hh", "cstb"], [pk])
                evac_copy(hT[:], psb[:, 0:512].rearrange("p (k t) -> p k t", t=128), [pk], ["hT"])
                ye = Ye[ex % 2]
                yk = "Ye%d" % (ex % 2)
                for nb in range(4):
                    b = nbank()
                    pk = "ps%d" % b

                    def dmm(e, nb=nb, b=b, pd_=pd_):
                        ins = None
                        for kc in range(4):
                            ins = e.matmul(ps[b][:, 0:512], lhsT=hT[:, kc, :], rhs=pd_[kc // 2][0](kc % 2, nb * 512, 512),
                                           start=(kc == 0), stop=(kc == 3))
                        return ins
                    PE(dmm, ["hT", pd_[0][1], pd_[1][1]], [pk])
                    evac_copy(ye[:, nb * 512:(nb + 1) * 512], ps[b][:, 0:512], [pk], [yk])
                DM(lambda e, ex=ex, ye=ye: e.dma_start(out=ysc[ex * CAP:(ex + 1) * CAP, :], in_=ye[:]), [yk], ["ysc"])
            S.barrier()
            pc.close()

        pd = ExitStack()
        gfin = T(pd, "gfin", [128, D])
        DM(lambda e: e.dma_start(out=gfin[:], in_=gains_d[:, 2, :]), [], ["gfin"])
        hts = [T(pd, "hD%d" % i, [128, D]) for i in range(2)]
        g1s = [T(pd, "g1_%d" % i, [128, D]) for i in range(2)]
        g2s = [T(pd, "g2_%d" % i, [128, D]) for i in range(2)]
        jnk = T(pd, "jnk", [128, D], BF16)
        sqd = [T(pd, "sqd%d" % i, [128, 4]) for i in range(2)]
        for ti in range(4 * NBLK):
            i = ti % 2
            ht, g1, g2, sq = hts[i], g1s[i], g2s[i], sqd[i]
            hk, k1, k2, sk = "hD%d" % i, "g1_%d" % i, "g2_%d" % i, "sqd%d" % i
            DM(lambda e, ti=ti, ht=ht: e.dma_start(out=ht[:], in_=hs[128 * ti:128 * (ti + 1), :]), ["hs"], [hk])
            if stage >= 3:
                S.dma(lambda e, ti=ti, g1=g1: e.indirect_dma_start(out=g1[:], out_offset=None, in_=ysc,
                      in_offset=bass.IndirectOffsetOnAxis(ap=gidx[:, ti, 0:1], axis=0)), ["ysc", "gi"], [k1], q="pool")
                S.dma(lambda e, ti=ti, g2=g2: e.indirect_dma_start(out=g2[:], out_offset=None, in_=ysc,
                      in_offset=bass.IndirectOffsetOnAxis(ap=gidx[:, ti, 1:2], axis=0)), ["ysc", "gi"], [k2], q="pool")
                V(lambda e, ti=ti, ht=ht, g1=g1: e.scalar_tensor_tensor(out=ht[:], in0=g1[:], scalar=gw[:, ti, 0:1], in1=ht[:],
                                                                        op0=ALU.mult, op1=ALU.add), [hk, k1, "gw"], [hk])
                V(lambda e, ti=ti, ht=ht, g2=g2: e.scalar_tensor_tensor(out=ht[:], in0=g2[:], scalar=gw[:, ti, 1:2], in1=ht[:],
                                                                        op0=ALU.mult, op1=ALU.add), [hk, k2, "gw"], [hk])
            A(lambda e, ht=ht, sq=sq: e.activation(out=jnk[:], in_=ht[:], func=AF.Square, accum_out=sq[:, 0:1]), [hk], ["jnk", sk])
            V(lambda e, sq=sq: e.tensor_scalar(out=sq[:, 1:2], in0=sq[:, 0:1], scalar1=1.0 / D, scalar2=EPS, op0=ALU.mult, op1=ALU.add), [sk], [sk])
            A(lambda e, sq=sq: e.activation(out=sq[:, 1:2], in_=sq[:, 1:2], func=AF.Sqrt), [sk], [sk])
            V(lambda e, sq=sq: e.reciprocal(out=sq[:, 2:3], in_=sq[:, 1:2]), [sk], [sk])
            V(lambda e, ht=ht, sq=sq: e.scalar_tensor_tensor(out=ht[:], in0=ht[:], scalar=sq[:, 2:3], in1=gfin[:], op0=ALU.mult, op1=ALU.mult),
              [hk, sk, "gfin"], [hk])
            DM(lambda e, ti=ti, ht=ht: e.dma_start(out=out_d[128 * ti:128 * (ti + 1), :], in_=ht[:]), [hk], ["out"], is_out=True)
        S.barrier()
        pd.close()
        S.emit()
    return nc


def build_rest(nc, st, S, L):
    pass


def _consts():
    c = np.zeros((128, 1024), np.float32)
    c[:, 0:128] = np.eye(128, dtype=np.float32)
    k = np.arange(128)
    c[:, 128:256] = (k[:, None] < k[None, :]).astype(np.float32)
    c[:, 256:384] = 1.0
    c[:, 384:384 + 259] = np.arange(259, dtype=np.float32)[None, :]
    c[:, 648:712] = np.arange(64, dtype=np.float32)[None, :]
    for j in range(2):
        c[:, 712 + j] = ((k // 16) % 2 == j)
        c[:, 718 + j] = (k // 64 == j)
    for j in range(4):
        c[:, 714 + j] = (k // 32 == j)
    c[:, 720] = k
    return c


def _prep(inp, stage=3):
    f = lambda a: np.ascontiguousarray(np.asarray(a, dtype=np.float32))
    x = f(inp["x"]); meta = f(inp["meta"])
    a_re = f(inp["ssm_a_re"])[0]; a_im = f(inp["ssm_a_im"])[0]; ldt = f(inp["ssm_log_dt"])[0]
    b_re = f(inp["ssm_b_re"])[0]; b_im = f(inp["ssm_b_im"])[0]
    c_re = f(inp["ssm_c_re"])[0]; c_im = f(inp["ssm_c_im"])[0]
    shared = {}
    shared["w_in"] = f(inp["w_in"])[0]
    shared["pool_w"] = f(inp["pool_w"])[0]
    shared["w_glu"] = f(inp["w_glu"])[0]
    shared["w_bp"] = f(inp["w_branch_pool"])[0]
    shared["w_bs"] = f(inp["w_branch_ssm"])[0]
    shared["w_out"] = f(inp["w_out"])[0]
    if stage >= 3:
        shared["w_gate"] = f(inp["w_gate"])[0]
        shared["w_up"] = f(inp["w_up"])[0]
        shared["w_down"] = f(inp["w_down"])[0]
    g = np.stack([f(inp["norm_mix"])[0], f(inp["norm_ffn"])[0], f(inp["norm_final"])], 0)
    shared["gains"] = np.ascontiguousarray(np.broadcast_to(g[None], (128, 3, D)))
    cols = np.stack([f(inp["ssm_d"])[0].reshape(8, 128).T, f(inp["pool_scale"])[0].reshape(8, 128).T,
                     f(inp["b_glu"])[0].reshape(8, 128).T], 1)
    shared["cols"] = np.ascontiguousarray(cols)
    wr = np.concatenate([f(inp["w_router_group"])[0], f(inp["w_router_expert"])[0]], 1)
    shared["wr"] = np.ascontiguousarray(wr.reshape(KC, 128, 72).transpose(1, 0, 2))
    br = np.concatenate([f(inp["b_router_group"])[0], f(inp["b_router_expert"])[0]], 0)
    shared["br"] = np.ascontiguousarray(np.broadcast_to(br[None], (128, 72)))
    def L1(a):
        t = a.reshape(8, 8, 64)
        t = np.broadcast_to(t[:, :, None, :], (8, 8, 16, 64))
        return t.transpose(1, 2, 0, 3).reshape(128, 8, 64)
    ldt_b = np.broadcast_to(ldt[:, None], (64, 64))
    shared["aL1"] = np.ascontiguousarray(np.stack([L1(a_re), L1(a_im), L1(ldt_b)], 1))
    def B1(b):
        t = b.reshape(8, 8, 64, 16)
        return t.transpose(1, 3, 0, 2).reshape(128, 8, 64)
    shared["bL1"] = np.ascontiguousarray(np.stack([B1(b_re), B1(b_im)], 1))
    def L2(a):
        return a.reshape(32, 2, 64).transpose(1, 2, 0).reshape(128, 32)
    shared["aL2"] = np.ascontiguousarray(np.stack([L2(a_re), L2(a_im), L2(ldt_b)], 1))
    def B2(b):
        return b.reshape(32, 2, 64, 16).transpose(1, 2, 0, 3).reshape(128, 32, 16)
    shared["bL2"] = np.ascontiguousarray(np.stack([B2(b_re), B2(b_im)], 1))
    def C2(c):
        return c.reshape(32, 2, 16, 64).transpose(1, 3, 0, 2).reshape(128, 32, 16)
    shared["cL2"] = np.ascontiguousarray(np.stack([C2(c_re), C2(c_im)], 1))
    shared["cst"] = _consts()
    maps = []
    for c in range(8):
        b, k = c // 4, c % 4
        halo = meta if k == 0 else x[b, NT * k - NH:NT * k]
        m = dict(shared)
        m["xs"] = np.ascontiguousarray(np.concatenate([halo, x[b, NT * k:NT * (k + 1)]], 0))
        m["hm"] = np.full((128, 1), 1.0 if k == 0 else 0.0, np.float32)
        cmv = np.zeros((4, 3), np.float32)
        for j in range(k):
            cmv[j, k - 1 - j] = 1.0
        m["cm"] = np.ascontiguousarray(np.broadcast_to(cmv.reshape(1, 12), (128, 12)))
        maps.append(m)
    return maps


_NC_CACHE = {}


def kernel(**inputs):
    if "nc" not in _NC_CACHE:
        _NC_CACHE["nc"] = build(False, 3)
    nc = _NC_CACHE["nc"]
    maps = _prep(inputs, 3)
    res = run_bass_kernel_spmd(nc, maps, core_ids=list(range(8)))
    out = np.empty((2, 8192, D), np.float32)
    for c in range(8):
        b, k = c // 4, c % 4
        out[b, NT * k:NT * (k + 1)] = res.results[c]["out"]
    return out
```
